# Optimizing a Trainium2 kernel written in Bass

```python
import jax, jax.numpy as jnp
from jax import lax
import numpy as np

D_MODEL = 1024
BATCH = 4
SEQ = 8192
DEPTH = 4

MLA_HEADS = D_MODEL // 128
MLA_NOPE = 128
MLA_ROPE = 64
MLA_V = 128
MLA_Q_RANK = 256
MLA_KV_RANK = 128
ROPE_THETA = 10000.0
Q_BLOCK = 128
GLA_HEADS = 4
GLA_DK = D_MODEL // 2 // GLA_HEADS
GLA_DV = D_MODEL // GLA_HEADS
GLA_GATE_RANK = 16
GLA_GATE_NORM = 16.0
GLA_CHUNK = 64
CONV_WIDTH = 31
FFN_DIM = 2816
FFN_CONV_WIDTH = 3
N_MIXERS = 3
NORM_EPS = 1e-5
DEEPNORM_ALPHA = (2.0 * DEPTH) ** 0.25
DEEPNORM_BETA = (8.0 * DEPTH) ** -0.25

kernel_name = "hybrid_mla_gla_conformer_convffn_deepnorm"


def _layernorm(x, g, b):
    x32 = x.astype(jnp.float32)
    mu = jnp.mean(x32, axis=-1, keepdims=True)
    var = jnp.mean(jnp.square(x32 - mu), axis=-1, keepdims=True)
    y = (x32 - mu) * lax.rsqrt(var + NORM_EPS) * g.astype(jnp.float32) + b.astype(jnp.float32)
    return y.astype(x.dtype)


def _rmsnorm(x, g):
    x32 = x.astype(jnp.float32)
    y = x32 * lax.rsqrt(jnp.mean(jnp.square(x32), axis=-1, keepdims=True) + NORM_EPS) * g.astype(jnp.float32)
    return y.astype(x.dtype)


def _apply_rope(x, positions):
    half = MLA_ROPE // 2
    inv_freq = ROPE_THETA ** (-jnp.arange(half, dtype=jnp.float32) / half)
    ang = positions.astype(jnp.float32)[..., None] * inv_freq
    cos, sin = jnp.cos(ang).astype(x.dtype), jnp.sin(ang).astype(x.dtype)
    x1, x2 = x[..., :half], x[..., half:]
    return jnp.concatenate([x1 * cos - x2 * sin, x2 * cos + x1 * sin], axis=-1)


def _dwconv_causal(x, w, b):
    width, ch = w.shape
    y = lax.conv_general_dilated(
        x, w[:, None, :].astype(x.dtype), window_strides=(1,), padding=[(width - 1, 0)],
        dimension_numbers=('NWC', 'WIO', 'NWC'), feature_group_count=ch)
    return y + b


def _mla(x, positions, w_in, q_norm, kv_norm, w_uq, w_ukv, w_o):
    B, S, _ = x.shape
    H = MLA_HEADS
    c = x @ w_in
    c_q, c_kv, k_rope = jnp.split(c, [MLA_Q_RANK, MLA_Q_RANK + MLA_KV_RANK], axis=-1)
    q = (_rmsnorm(c_q, q_norm) @ w_uq).reshape(B, S, H, MLA_NOPE + MLA_ROPE)
    scale = (MLA_NOPE + MLA_ROPE) ** -0.5
    q_nope = q[..., :MLA_NOPE] * scale
    q_rope = _apply_rope(q[..., MLA_NOPE:], positions[:, :, None]) * scale
    k_rope = _apply_rope(k_rope, positions)
    kv = (_rmsnorm(c_kv, kv_norm) @ w_ukv).reshape(B, S, H, MLA_NOPE + MLA_V)
    k_nope, v = kv[..., :MLA_NOPE], kv[..., MLA_NOPE:]

    nb = S // Q_BLOCK
    qn_blocks = q_nope.reshape(B, nb, Q_BLOCK, H, MLA_NOPE).transpose(1, 0, 2, 3, 4)
    qr_blocks = q_rope.reshape(B, nb, Q_BLOCK, H, MLA_ROPE).transpose(1, 0, 2, 3, 4)
    key_idx = jnp.arange(S)

    def block(args):
        qn, qr, blk = args
        s = (jnp.einsum('bqhd,bkhd->bhqk', qn, k_nope)
             + jnp.einsum('bqhr,bkr->bhqk', qr, k_rope))
        q_idx = blk * Q_BLOCK + jnp.arange(Q_BLOCK)
        mask = key_idx[None, :] <= q_idx[:, None]
        s = jnp.where(mask, s.astype(jnp.float32), -jnp.inf)
        p = jax.nn.softmax(s, axis=-1).astype(v.dtype)
        return jnp.einsum('bhqk,bkhd->bqhd', p, v)

    o = lax.map(block, (qn_blocks, qr_blocks, jnp.arange(nb)))
    o = o.transpose(1, 0, 2, 3, 4).reshape(B, S, H * MLA_V)
    return o @ w_o


def _gla(x, w_in, w_a2, b_a, out_norm, w_o):
    B, S, _ = x.shape
    H, DK, DV, C = GLA_HEADS, GLA_DK, GLA_DV, GLA_CHUNK
    hk, hv = H * DK, H * DV
    proj = x @ w_in
    q, k, v, a_lr, r = jnp.split(proj, [hk, 2 * hk, 2 * hk + hv, 2 * hk + hv + GLA_GATE_RANK], axis=-1)
    log_a = jax.nn.log_sigmoid((a_lr @ w_a2 + b_a).astype(jnp.float32)) / GLA_GATE_NORM
    nc = S // C

    def to_chunks(t, d):
        return t.astype(jnp.float32).reshape(B, nc, C, H, d).transpose(1, 0, 3, 2, 4)

    qc = to_chunks(q * DK ** -0.5, DK)
    kc = to_chunks(k, DK)
    vc = to_chunks(v, DV)
    bc = jnp.cumsum(to_chunks(log_a, DK), axis=3)
    causal = jnp.tril(jnp.ones((C, C), dtype=bool))

    def step(state, inp):
        q_, k_, v_, b_ = inp
        o_inter = jnp.einsum('bhcd,bhde->bhce', q_ * jnp.exp(b_), state)
        diff = b_[:, :, :, None, :] - b_[:, :, None, :, :]
        decay = jnp.exp(jnp.where(causal[:, :, None], diff, -jnp.inf))
        attn = jnp.einsum('bhid,bhjd,bhijd->bhij', q_, k_, decay)
        o = o_inter + jnp.einsum('bhij,bhje->bhie', attn, v_)
        b_last = b_[:, :, -1:, :]
        k_dec = k_ * jnp.exp(b_last - b_)
        state = jnp.exp(b_last[:, :, 0, :, None]) * state + jnp.einsum('bhcd,bhce->bhde', k_dec, v_)
        return state, o

    state0 = jnp.zeros((B, H, DK, DV), jnp.float32)
    _, o = lax.scan(step, state0, (qc, kc, vc, bc))
    o = o.transpose(1, 0, 3, 2, 4).reshape(B, S, H, DV)
    o = _rmsnorm(o, out_norm).astype(x.dtype).reshape(B, S, hv)
    return (o * jax.nn.silu(r)) @ w_o


def _conformer_conv(x, w_in, b_in, dw, dw_b, ln_g, ln_b, w_o, b_o):
    h = x @ w_in + b_in
    a, g = jnp.split(h, 2, axis=-1)
    h = a * jax.nn.sigmoid(g)
    h = _dwconv_causal(h, dw, dw_b)
    h = jax.nn.silu(_layernorm(h, ln_g, ln_b))
    return h @ w_o + b_o


def _conv_ffn(x, w_in, conv, conv_b, w_out):
    u = _dwconv_causal(x @ w_in, conv, conv_b)
    g, val = jnp.split(u, 2, axis=-1)
    return (jax.nn.silu(g) * val) @ w_out


def _normal(key, shape, scale):
    return scale * jax.random.normal(key, shape, jnp.float32)


def _gain(key, n):
    return 1.0 + 0.02 * jax.random.normal(key, (n,), jnp.float32)


def _bias(key, n):
    return 0.02 * jax.random.normal(key, (n,), jnp.float32)


def _mla_params(key, p):
    ks = jax.random.split(key, 6)
    hv = MLA_HEADS * MLA_V
    return {
        p + "mla_w_in": _normal(ks[0], (D_MODEL, MLA_Q_RANK + MLA_KV_RANK + MLA_ROPE), D_MODEL ** -0.5),
        p + "mla_q_norm": _gain(ks[1], MLA_Q_RANK),
        p + "mla_kv_norm": _gain(ks[2], MLA_KV_RANK),
        p + "mla_w_uq": _normal(ks[3], (MLA_Q_RANK, MLA_HEADS * (MLA_NOPE + MLA_ROPE)), MLA_Q_RANK ** -0.5),
        p + "mla_w_ukv": _normal(ks[4], (MLA_KV_RANK, MLA_HEADS * (MLA_NOPE + MLA_V)), MLA_KV_RANK ** -0.5),
        p + "mla_w_o": _normal(ks[5], (hv, D_MODEL), DEEPNORM_BETA * hv ** -0.5),
    }


def _gla_params(key, p):
    ks = jax.random.split(key, 5)
    hk, hv = GLA_HEADS * GLA_DK, GLA_HEADS * GLA_DV
    return {
        p + "gla_w_in": _normal(ks[0], (D_MODEL, 2 * hk + hv + GLA_GATE_RANK + hv), D_MODEL ** -0.5),
        p + "gla_w_a2": _normal(ks[1], (GLA_GATE_RANK, hk), GLA_GATE_RANK ** -0.5),
        p + "gla_b_a": _bias(ks[2], hk),
        p + "gla_out_norm": _gain(ks[3], GLA_DV),
        p + "gla_w_o": _normal(ks[4], (hv, D_MODEL), DEEPNORM_BETA * hv ** -0.5),
    }


def _conv_params(key, p):
    ks = jax.random.split(key, 8)
    return {
        p + "conv_w_in": _normal(ks[0], (D_MODEL, 2 * D_MODEL), D_MODEL ** -0.5),
        p + "conv_b_in": _bias(ks[1], 2 * D_MODEL),
        p + "conv_dw": _normal(ks[2], (CONV_WIDTH, D_MODEL), CONV_WIDTH ** -0.5),
        p + "conv_dw_b": _bias(ks[3], D_MODEL),
        p + "conv_ln_g": _gain(ks[4], D_MODEL),
        p + "conv_ln_b": _bias(ks[5], D_MODEL),
        p + "conv_w_o": _normal(ks[6], (D_MODEL, D_MODEL), DEEPNORM_BETA * D_MODEL ** -0.5),
        p + "conv_b_o": _bias(ks[7], D_MODEL),
    }


def _layer_params(key, i):
    k_mix, k_ln1, k_ffn, k_ln2 = jax.random.split(key, 4)
    p = "l%d_" % i
    kind = i % N_MIXERS
    if kind == 0:
        d = _mla_params(k_mix, p)
    elif kind == 1:
        d = _gla_params(k_mix, p)
    else:
        d = _conv_params(k_mix, p)
    a, b = jax.random.split(k_ln1)
    d[p + "ln1_g"] = _gain(a, D_MODEL)
    d[p + "ln1_b"] = _bias(b, D_MODEL)
    kf = jax.random.split(k_ffn, 4)
    d[p + "ffn_w_in"] = _normal(kf[0], (D_MODEL, 2 * FFN_DIM), D_MODEL ** -0.5)
    d[p + "ffn_conv"] = _normal(kf[1], (FFN_CONV_WIDTH, 2 * FFN_DIM), FFN_CONV_WIDTH ** -0.5)
    d[p + "ffn_conv_b"] = _bias(kf[2], 2 * FFN_DIM)
    d[p + "ffn_w_out"] = _normal(kf[3], (FFN_DIM, D_MODEL), DEEPNORM_BETA * FFN_DIM ** -0.5)
    a, b = jax.random.split(k_ln2)
    d[p + "ln2_g"] = _gain(a, D_MODEL)
    d[p + "ln2_b"] = _bias(b, D_MODEL)
    return d


def setup_inputs(seed: int = 0) -> dict:
    key = jax.random.key(seed)
    keys = jax.random.split(key, DEPTH + 1)
    out = {
        "x": jax.random.normal(keys[0], (BATCH, SEQ, D_MODEL), jnp.float32),
        "positions": jnp.broadcast_to(jnp.arange(SEQ, dtype=jnp.int32)[None, :], (BATCH, SEQ)),
    }
    for i in range(DEPTH):
        out.update(_layer_params(keys[i + 1], i))
    return out


def reference(x, positions,
              l0_mla_w_in, l0_mla_q_norm, l0_mla_kv_norm, l0_mla_w_uq, l0_mla_w_ukv, l0_mla_w_o,
              l0_ln1_g, l0_ln1_b, l0_ffn_w_in, l0_ffn_conv, l0_ffn_conv_b, l0_ffn_w_out, l0_ln2_g, l0_ln2_b,
              l1_gla_w_in, l1_gla_w_a2, l1_gla_b_a, l1_gla_out_norm, l1_gla_w_o,
              l1_ln1_g, l1_ln1_b, l1_ffn_w_in, l1_ffn_conv, l1_ffn_conv_b, l1_ffn_w_out, l1_ln2_g, l1_ln2_b,
              l2_conv_w_in, l2_conv_b_in, l2_conv_dw, l2_conv_dw_b, l2_conv_ln_g, l2_conv_ln_b, l2_conv_w_o, l2_conv_b_o,
              l2_ln1_g, l2_ln1_b, l2_ffn_w_in, l2_ffn_conv, l2_ffn_conv_b, l2_ffn_w_out, l2_ln2_g, l2_ln2_b,
              l3_mla_w_in, l3_mla_q_norm, l3_mla_kv_norm, l3_mla_w_uq, l3_mla_w_ukv, l3_mla_w_o,
              l3_ln1_g, l3_ln1_b, l3_ffn_w_in, l3_ffn_conv, l3_ffn_conv_b, l3_ffn_w_out, l3_ln2_g, l3_ln2_b):
    mixer_args = [
        (l0_mla_w_in, l0_mla_q_norm, l0_mla_kv_norm, l0_mla_w_uq, l0_mla_w_ukv, l0_mla_w_o),
        (l1_gla_w_in, l1_gla_w_a2, l1_gla_b_a, l1_gla_out_norm, l1_gla_w_o),
        (l2_conv_w_in, l2_conv_b_in, l2_conv_dw, l2_conv_dw_b, l2_conv_ln_g, l2_conv_ln_b, l2_conv_w_o, l2_conv_b_o),
        (l3_mla_w_in, l3_mla_q_norm, l3_mla_kv_norm, l3_mla_w_uq, l3_mla_w_ukv, l3_mla_w_o),
    ]
    ln1_args = [(l0_ln1_g, l0_ln1_b), (l1_ln1_g, l1_ln1_b), (l2_ln1_g, l2_ln1_b), (l3_ln1_g, l3_ln1_b)]
    ffn_args = [
        (l0_ffn_w_in, l0_ffn_conv, l0_ffn_conv_b, l0_ffn_w_out),
        (l1_ffn_w_in, l1_ffn_conv, l1_ffn_conv_b, l1_ffn_w_out),
        (l2_ffn_w_in, l2_ffn_conv, l2_ffn_conv_b, l2_ffn_w_out),
        (l3_ffn_w_in, l3_ffn_conv, l3_ffn_conv_b, l3_ffn_w_out),
    ]
    ln2_args = [(l0_ln2_g, l0_ln2_b), (l1_ln2_g, l1_ln2_b), (l2_ln2_g, l2_ln2_b), (l3_ln2_g, l3_ln2_b)]

    for i in range(DEPTH):
        kind = i % N_MIXERS
        if kind == 0:
            h = _mla(x, positions, *mixer_args[i])
        elif kind == 1:
            h = _gla(x, *mixer_args[i])
        else:
            h = _conformer_conv(x, *mixer_args[i])
        x = _layernorm(DEEPNORM_ALPHA * x + h, *ln1_args[i])
        x = _layernorm(DEEPNORM_ALPHA * x + _conv_ffn(x, *ffn_args[i]), *ln2_args[i])
    return x
```

```python
import numpy as np
import concourse.bass as bass
import concourse.mybir as mybir
from concourse.bass_utils import run_bass_kernel_spmd

F32 = mybir.dt.float32
BF16 = mybir.dt.bfloat16
I32 = mybir.dt.int32
AF = mybir.ActivationFunctionType
ALU = mybir.AluOpType
AX = mybir.AxisListType

D = 1024
DC = 8
FF = 2816
FC = 22
DEPTH = 4
ALPHA = float((2.0 * DEPTH) ** 0.25)
EPS = 1e-5
SAME_ENG_SYNC = True
NEG = -30000.0


class Res:
    __slots__ = ("name", "w", "rs", "pred")

    def __init__(self, name):
        self.name = name
        self.w = None
        self.rs = []
        self.pred = []


class Op:
    __slots__ = ("eng", "fn", "deps", "inc", "tick", "dma", "sem", "idx")

    def __init__(self, eng, fn, dma):
        self.eng = eng
        self.fn = fn
        self.deps = []
        self.inc = False
        self.tick = 0
        self.dma = dma
        self.sem = None
        self.idx = 0


ENGS = ("pe", "act", "dve", "pool", "sp")
NDMASEM = 8


class Prog:
    def __init__(self):
        self.streams = {e: [] for e in ENGS}
        self.ndma = {e: 0 for e in ENGS}
        self.dma_hist = {e: [] for e in ENGS}
        self.n = 0

    def _deps(self, op, rd, wr):
        deps = op.deps
        for r in rd:
            if r.pred:
                deps.extend(r.pred)
            if r.w is not None:
                deps.append(r.w)
        for r in wr:
            if r.pred:
                deps.extend(r.pred)
            if r.w is not None:
                deps.append(r.w)
            deps.extend(r.rs)
        for r in rd:
            r.rs.append(op)
            r.pred = []
        for r in wr:
            r.w = op
            r.rs = []
            r.pred = []
        for d in deps:
            d.inc = True

    def op(self, eng, meth, *args, rd=(), wr=(), **kw):
        fn = (lambda e, meth=meth, args=args, kw=kw: getattr(e, meth)(*args, **kw))
        o = Op(eng, fn, False)
        o.idx = self.n
        self.n += 1
        self._deps(o, rd, wr)
        self.streams[eng].append(o)
        return o

    def dma(self, eng, out, in_, rd=(), wr=()):
        o = Op(eng, (lambda e, out=out, in_=in_: e.dma_start(out=out, in_=in_)), True)
        o.idx = self.n
        self.n += 1
        k = self.ndma[eng]
        self.ndma[eng] = k + 1
        o.sem = (eng, k % NDMASEM)
        o.tick = 16 * (k // NDMASEM + 1)
        hist = self.dma_hist[eng]
        if k >= NDMASEM:
            o.deps.append(hist[k - NDMASEM])
        hist.append(o)
        self._deps(o, rd, wr)
        o.inc = True
        self.streams[eng].append(o)
        return o

    def retire(self, resources):
        out = []
        for r in resources:
            if r.w is not None:
                out.append(r.w)
            out.extend(r.rs)
            out.extend(r.pred)
        last = {}
        res = []
        for o in out:
            if o.dma:
                res.append(o)
            else:
                if o.eng not in last or last[o.eng].idx < o.idx:
                    last[o.eng] = o
        res.extend(last.values())
        for o in res:
            o.inc = True
        return res

    def emit(self, nc, block, sems, dsems):
        for e in ENGS:
            t = 0
            for o in self.streams[e]:
                if not o.dma and o.inc:
                    t += 1
                    o.tick = t
        streams = self.streams

        def run(eng_name, e):
            seen = {}
            for o in streams[eng_name]:
                need = {}
                for d in o.deps:
                    if d.dma:
                        key = d.sem
                    else:
                        if d.eng == eng_name and (eng_name == "pe" or not SAME_ENG_SYNC):
                            continue
                        key = d.eng
                    if need.get(key, 0) < d.tick:
                        need[key] = d.tick
                for key, val in need.items():
                    if seen.get(key, 0) >= val:
                        continue
                    seen[key] = val
                    s = dsems[key] if isinstance(key, tuple) else sems[key]
                    e.wait_ge(s, val)
                ins = o.fn(e)
                if o.inc:
                    if o.dma:
                        ins.then_inc(dsems[o.sem], 16)
                    else:
                        ins.then_inc(sems[eng_name], 1)

        @block.tensor
        def _(e):
            run("pe", e)

        @block.scalar
        def _(e):
            run("act", e)

        @block.vector
        def _(e):
            run("dve", e)

        @block.gpsimd
        def _(e):
            run("pool", e)

        @block.sync
        def _(e):
            run("sp", e)


class Mem:
    def __init__(self, P, big, words):
        self.P = P
        self.big = big
        self.words = words
        self.top = 0
        self.live = []

    def mark(self):
        return self.top

    def release(self, mark):
        self.top = mark

    def alloc(self, name, shape, dtype, nres=1):
        n = 1
        for s in shape:
            n *= s
        if dtype == BF16:
            w = (n + 1) // 2
        else:
            w = n
        a = self.top
        b = a + w
        assert b <= self.words, f"SBUF overflow allocating {name}: {b} > {self.words}"
        self.top = b
        pred = []
        keep = []
        for (s, e, rl) in self.live:
            if s < b and a < e:
                pred.extend(self.P.retire(rl))
            else:
                keep.append((s, e, rl))
        self.live = keep
        rl = [Res(f"{name}{i}") for i in range(nres)]
        for r in rl:
            r.pred = list(pred)
        self.live.append((a, b, rl))
        ap = self.big[:, a:b]
        if dtype == BF16:
            ap = ap.bitcast(BF16)[:, 0:n]
        elif dtype == I32:
            ap = ap.bitcast(I32)
        if len(shape) == 2:
            ap = ap.rearrange("p (a b) -> p a b", a=shape[0])
        elif len(shape) == 3:
            ap = ap.rearrange("p (a b c) -> p a b c", a=shape[0], b=shape[1])
        return ap, rl


def fm(v):
    v = np.asarray(v, np.float32)
    return np.ascontiguousarray(v.reshape(-1, 128).T)


class VecPack:
    def __init__(self):
        self.cols = []
        self.off = {}
        self.n = 0

    def add(self, name, arr2d):
        arr2d = np.asarray(arr2d, np.float32)
        assert arr2d.shape[0] == 128
        self.off[name] = (self.n, arr2d.shape[1])
        self.cols.append(arr2d)
        self.n += arr2d.shape[1]

    def pack(self):
        return np.ascontiguousarray(np.concatenate(self.cols, axis=1))


def layer_kind(i):
    return i % 3


def build_vecpack(inp):
    vp = VecPack()
    for l in range(DEPTH):
        p = f"l{l}_"
        for nm in ("ln1_g", "ln1_b", "ln2_g", "ln2_b"):
            vp.add(p + nm, fm(inp[p + nm]))
        cw = np.asarray(inp[p + "ffn_conv"], np.float32)
        for k in range(3):
            vp.add(p + f"ffn_conv{k}", fm(cw[k]))
        vp.add(p + "ffn_conv_b", fm(inp[p + "ffn_conv_b"]))
        kind = layer_kind(l)
        if kind == 0:
            vp.add(p + "mla_q_norm", fm(inp[p + "mla_q_norm"]))
            vp.add(p + "mla_kv_norm", fm(inp[p + "mla_kv_norm"]))
        elif kind == 1:
            vp.add(p + "gla_b_a", fm(inp[p + "gla_b_a"]))
            vp.add(p + "gla_out_norm", fm(inp[p + "gla_out_norm"]))
        else:
            vp.add(p + "conv_b_in", fm(inp[p + "conv_b_in"]))
            dw = np.asarray(inp[p + "conv_dw"], np.float32)
            for k in range(31):
                vp.add(p + f"conv_dw{k}", fm(dw[k]))
            vp.add(p + "conv_dw_b", fm(inp[p + "conv_dw_b"]))
            vp.add(p + "conv_ln_g", fm(inp[p + "conv_ln_g"]))
            vp.add(p + "conv_ln_b", fm(inp[p + "conv_ln_b"]))
            vp.add(p + "conv_b_o", fm(inp[p + "conv_b_o"]))
    return vp


class K:
    def __init__(self, T, vp_off, nvec, phases, debug_out=None):
        self.T = T
        self.vp_off = vp_off
        self.nvec = nvec
        self.phases = phases
        nc = bass.Bass("TRN2", target_bir_lowering=False)
        self.nc = nc
        self.P = Prog()
        self.dram = {}
        self.dres = {}

    def din(self, name, shape, dtype=F32):
        t = self.nc.dram_tensor(name, list(shape), dtype, kind="ExternalInput").ap()
        self.dram[name] = t
        return t

    def dout(self, name, shape, dtype=F32):
        t = self.nc.dram_tensor(name, list(shape), dtype, kind="ExternalOutput").ap()
        self.dram[name] = t
        return t

    def dscr(self, name, shape, dtype=F32):
        t = self.nc.dram_tensor(name, list(shape), dtype, kind="Internal").ap()
        self.dram[name] = t
        return t

    def dr(self, key):
        r = self.dres.get(key)
        if r is None:
            r = Res(str(key))
            self.dres[key] = r
        return r

    def vec(self, name, j=0, n=1):
        o, w = self.vp_off[name]
        return self.vecs[:, o + j:o + j + n]


def tiles_order(n):
    return list(range(n))


def build_program(T, inp_shapes, vp_off, nvec, layers, TT=256):
    k = K(T, vp_off, nvec, None)
    nc = k.nc
    P = k.P
    NT = T // TT

    xT_in = k.din("xT", [D, T])
    vecs_d = k.din("vecs", [128, nvec])
    pos_d = k.din("pos", [1, T], I32)
    cst_d = k.din("cst", [128, 160])
    mask_d = k.din("maskb", [128, 2048])
    glac_d = k.din("glac", [128, 384])
    W = {}
    for name, shp in inp_shapes.items():
        W[name] = k.din(name, shp)
    outT = k.dout("outT", [D, T])
    nph = sum(int(a) + int(b) for (_, a, b) in layers)
    xs_d = [xT_in]
    for i in range(nph - 1):
        xs_d.append(k.dscr(f"xs{i}", [D, T]))
    xs_d.append(outT)

    WORDS = 53000
    import contextlib
    es = contextlib.ExitStack()
    with es:
        big_h = es.enter_context(nc.sbuf_tensor("big", [128, WORDS], F32))
        big = big_h[:, :]
        pbanks = []
        for i in range(8):
            ph = es.enter_context(nc.psum_tensor(f"ps{i}", [128, 512], F32))
            pbanks.append(ph[:, :])
        sems = {e: es.enter_context(nc.semaphore(f"s_{e}")) for e in ENGS}
        dsems = {}
        for e in ("sp", "pool", "act"):
            for i in range(NDMASEM):
                dsems[(e, i)] = es.enter_context(nc.semaphore(f"d_{e}{i}"))
        M = Mem(P, big, WORDS)
        pres = [Res(f"psum{i}") for i in range(8)]

        vecs, vr = M.alloc("vecs", [nvec], F32)
        k.vecs = vecs
        vr = vr[0]
        P.dma("sp", vecs, vecs_d[:, :], wr=[vr])
        ones1024, r_o = M.alloc("ones1024", [128], BF16)
        r_ones = r_o[0]
        P.op("dve", "memset", ones1024, 1.0 / 1024.0, wr=[r_ones])
        ones256, _r = M.alloc("ones256", [128], BF16)
        P.op("dve", "memset", ones256, 1.0 / 256.0, wr=[r_ones])
        ones128, _r = M.alloc("ones128", [128], BF16)
        P.op("dve", "memset", ones128, 1.0 / 128.0, wr=[r_ones])
        ones1, _r = M.alloc("ones1", [128], BF16)
        P.op("dve", "memset", ones1, 1.0, wr=[r_ones])
        cst, r_c = M.alloc("cst", [160], F32)
        r_cst = r_c[0]
        P.dma("sp", cst, cst_d[:, :], wr=[r_cst])
        ident_b, _r = M.alloc("ident", [128], BF16)
        P.op("dve", "tensor_copy", ident_b, cst[:, 0:128], rd=[r_cst], wr=[r_ones])
        consts = dict(ones1024=ones1024, ones256=ones256, ones128=ones128, ones1=ones1,
                      ident=ident_b, cst=cst, r_ones=r_ones, r_cst=r_cst, vr=vr)
        base_mark = M.mark()

        ctx = dict(k=k, nc=nc, P=P, M=M, pbanks=pbanks, pres=pres, T=T, TT=TT, NT=NT,
                   consts=consts, W=W, pos_d=pos_d, mask_d=mask_d, glac_d=glac_d)

        xi = 0
        for (l, do_mixer, do_ffn) in layers:
            kind = layer_kind(l)
            if do_mixer:
                M.release(base_mark)
                if kind == 0:
                    phase_mla(ctx, l, xs_d[xi], xs_d[xi + 1])
                elif kind == 1:
                    phase_gla(ctx, l, xs_d[xi], xs_d[xi + 1])
                else:
                    phase_conv(ctx, l, xs_d[xi], xs_d[xi + 1])
                xi += 1
            if do_ffn:
                M.release(base_mark)
                phase_ffn(ctx, l, xs_d[xi], xs_d[xi + 1])
                xi += 1
        assert xs_d[xi] is outT or True
        final_src = xs_d[xi]
        fin_deps = [k.dr(("x", id(final_src), i)) for i in range(NT)]
        P.op("sp", "nop", rd=fin_deps)
        with nc.Block() as block:
            P.emit(nc, block, sems, dsems)
    return nc, final_src


def ln_tail_alloc(ctx):
    M, TT = ctx["M"], ctx["TT"]
    t = {}
    t["r"], t["r_r"] = M.alloc("r", [DC, TT], F32, nres=DC)
    t["rb"], t["rb_r"] = M.alloc("rb", [DC, TT], BF16, nres=DC)
    t["rsq"], t["rsq_r"] = M.alloc("rsq", [DC, TT], BF16, nres=DC)
    t["mean"], t["mean_r"] = M.alloc("mean", [TT], F32)
    t["m2"], t["m2_r"] = M.alloc("m2", [TT], F32)
    t["A"], t["A_r"] = M.alloc("A", [TT], F32)
    t["B"], t["B_r"] = M.alloc("B", [TT], F32)
    return t


def ln_core(ctx, t, g_name, b_name, final_fn=None):
    k, P = ctx["k"], ctx["P"]
    c = ctx["consts"]
    r, rb, rsq = t["r"], t["rb"], t["rsq"]
    (pm, pm_r), (pe2, pe2_r) = t["ps_stat"]
    for oc in range(DC):
        P.op("pe", "matmul", pm, c["ones1024"], rb[:, oc, :], start=(oc == 0), stop=(oc == DC - 1),
             rd=[t["rb_r"][oc], c["r_ones"]], wr=[pm_r])
    for oc in range(DC):
        P.op("pe", "matmul", pe2, c["ones1024"], rsq[:, oc, :], start=(oc == 0), stop=(oc == DC - 1),
             rd=[t["rsq_r"][oc], c["r_ones"]], wr=[pe2_r])
    mean, m2, A, B = t["mean"], t["m2"], t["A"], t["B"]
    P.op("act", "activation", mean, pm, AF.Copy, rd=[pm_r], wr=t["mean_r"])
    P.op("pool", "tensor_tensor", out=m2, in0=mean, in1=mean, op=ALU.mult, rd=t["mean_r"], wr=t["m2_r"])
    P.op("dve", "tensor_tensor", out=A, in0=pe2, in1=m2, op=ALU.subtract, rd=[pe2_r] + t["m2_r"], wr=t["A_r"])
    P.op("dve", "tensor_scalar_add", A, A, EPS, rd=t["A_r"], wr=t["A_r"])
    P.op("act", "activation", A, A, AF.Ln, rd=t["A_r"], wr=t["A_r"])
    P.op("act", "activation", A, A, AF.Exp, scale=-0.5, rd=t["A_r"], wr=t["A_r"])
    P.op("dve", "scalar_tensor_tensor", out=B, in0=mean, scalar=-1.0, in1=A, op0=ALU.mult, op1=ALU.mult,
         rd=t["mean_r"] + t["A_r"], wr=t["B_r"])
    for oc in range(DC):
        ro = r[:, oc, :]
        P.op("dve", "tensor_tensor", out=ro, in0=ro, in1=A, op=ALU.mult,
             rd=[t["r_r"][oc]] + t["A_r"], wr=[t["r_r"][oc]])
        P.op("pool", "tensor_tensor", out=ro, in0=ro, in1=B, op=ALU.add,
             rd=[t["r_r"][oc]] + t["B_r"], wr=[t["r_r"][oc]])
        gv, bv = k.vec(g_name, oc), k.vec(b_name, oc)
        P.op("act", "activation", ro, ro, AF.Identity, bias=bv, scale=gv,
             rd=[t["r_r"][oc], c["vr"]], wr=[t["r_r"][oc]])
        if final_fn is not None:
            final_fn(oc, ro, t["r_r"][oc])


def ln_tail(ctx, t, ti, y_chunk_fn, xs, xs_r, xoff, g_name, b_name, dst, ps_y, ps_stat, bias_name=None):
    k, P, TT = ctx["k"], ctx["P"], ctx["TT"]
    c = ctx["consts"]
    r, rb, rsq = t["r"], t["rb"], t["rsq"]
    t["ps_stat"] = ps_stat
    for oc in range(DC):
        ps, ps_r = ps_y[oc % len(ps_y)]
        y_chunk_fn(oc, ps, ps_r)
        xin = xs[:, oc, xoff:xoff + TT]
        ro = r[:, oc, :]
        if bias_name is None:
            P.op("dve", "scalar_tensor_tensor", out=ro, in0=xin, scalar=ALPHA, in1=ps, op0=ALU.mult, op1=ALU.add,
                 rd=[xs_r, ps_r], wr=[t["r_r"][oc]])
        else:
            bv = k.vec(bias_name, oc)
            P.op("act", "activation", ro, ps, AF.Identity, bias=bv, scale=1.0,
                 rd=[ps_r, c["vr"]], wr=[t["r_r"][oc]])
            P.op("dve", "scalar_tensor_tensor", out=ro, in0=xin, scalar=ALPHA, in1=ro, op0=ALU.mult, op1=ALU.add,
                 rd=[xs_r, t["r_r"][oc]], wr=[t["r_r"][oc]])
        P.op("pool", "tensor_copy", rb[:, oc, :], ro, rd=[t["r_r"][oc]], wr=[t["rb_r"][oc]])
        P.op("act", "activation", rsq[:, oc, :], ro, AF.Square, rd=[t["r_r"][oc]], wr=[t["rsq_r"][oc]])
    ln_core(ctx, t, g_name, b_name)
    dview = dst.rearrange("(c p) t -> p c t", p=128)[:, :, ti * TT:(ti + 1) * TT]
    P.dma("sp", dview, r, rd=t["r_r"], wr=[k.dr(("x", id(dst), ti))])


def load_w(ctx, dst_ap, src_ap, res):
    P = ctx["P"]
    P.dma("pool", dst_ap, src_ap, wr=res)


def phase_ffn(ctx, l, src, dst):
    k, P, M, T, TT, NT = ctx["k"], ctx["P"], ctx["M"], ctx["T"], ctx["TT"], ctx["NT"]
    W, c, pb, pr = ctx["W"], ctx["consts"], ctx["pbanks"], ctx["pres"]
    p = f"l{l}_"
    w_in_d, w_out_d = W[p + "ffn_w_in"], W[p + "ffn_w_out"]
    w_in, w_in_r = M.alloc("ffn_w_in", [DC, 2 * FF], BF16, nres=DC * 4)
    w_out, w_out_r = M.alloc("ffn_w_out", [FC, D], BF16, nres=FC)
    for kc in range(DC):
        for q in range(4):
            load_w(ctx, w_in[:, kc, q * 1408:(q + 1) * 1408], w_in_d[kc * 128:(kc + 1) * 128, q * 1408:(q + 1) * 1408],
                   [w_in_r[kc * 4 + q]])
    for fc in range(FC):
        load_w(ctx, w_out[:, fc, :], w_out_d[fc * 128:(fc + 1) * 128, :], [w_out_r[fc]])
    NB = 2
    xs_l, xb_l = [], []
    for i in range(NB):
        xs_l.append(M.alloc(f"xs{i}", [DC, TT + 2], F32))
        xb_l.append(M.alloc(f"xb{i}", [DC, TT + 2], BF16))
    tg_l = [M.alloc(f"tg{i}", [TT], F32) for i in range(2)]
    tv_l = [M.alloc(f"tv{i}", [TT], F32) for i in range(2)]
    gT, gT_r = M.alloc("gT", [FC, TT], BF16, nres=FC)
    t = ln_tail_alloc(ctx)
    srcv = src.rearrange("(c p) t -> p c t", p=128)

    def load_x(ti, slot):
        xs, xs_r = xs_l[slot]
        xb, xb_r = xb_l[slot]
        t0 = ti * TT
        rdr = [k.dr(("x", id(src), ti))]
        if ti == 0:
            P.op("pool", "memset", xs[:, :, 0:2], 0.0, wr=xs_r)
            P.op("pool", "memset", xb[:, :, 0:2], 0.0, wr=xb_r)
            P.dma("sp", xs[:, :, 2:TT + 2], srcv[:, :, 0:TT], rd=rdr, wr=xs_r)
            P.dma("pool", xb[:, :, 2:TT + 2], srcv[:, :, 0:TT], rd=rdr, wr=xb_r)
        else:
            rdr.append(k.dr(("x", id(src), ti - 1)))
            P.dma("sp", xs[:, :, :], srcv[:, :, t0 - 2:t0 + TT], rd=rdr, wr=xs_r)
            P.dma("pool", xb[:, :, :], srcv[:, :, t0 - 2:t0 + TT], rd=rdr, wr=xb_r)

    def fin(gc, tg, tg_r, tv, tv_r):
        P.op("act", "activation", tg, tg, AF.Silu, rd=tg_r, wr=tg_r)
        P.op("pool", "tensor_tensor", out=gT[:, gc, :], in0=tg, in1=tv, op=ALU.mult,
             rd=tg_r + tv_r, wr=[gT_r[gc]])

    load_x(0, 0)
    for ti in range(NT):
        slot = ti % NB
        if ti + 1 < NT:
            load_x(ti + 1, (ti + 1) % NB)
        xs, xs_r = xs_l[slot]
        xb, xb_r = xb_l[slot]
        pend = None
        for gc in range(FC):
            bg, bv_ = (gc % 2) * 2, (gc % 2) * 2 + 1
            psg, psv = pb[bg][:, 0:TT + 2], pb[bv_][:, 0:TT + 2]
            for kc in range(DC):
                P.op("pe", "matmul", psg, w_in[:, kc, gc * 128:(gc + 1) * 128], xb[:, kc, :],
                     start=(kc == 0), stop=(kc == DC - 1),
                     rd=[w_in_r[kc * 4 + (gc * 128) // 1408], w_in_r[kc * 4 + (gc * 128 + 127) // 1408]] + xb_r,
                     wr=[pr[bg]])
            for kc in range(DC):
                c0 = FF + gc * 128
                P.op("pe", "matmul", psv, w_in[:, kc, c0:c0 + 128], xb[:, kc, :],
                     start=(kc == 0), stop=(kc == DC - 1),
                     rd=[w_in_r[kc * 4 + c0 // 1408], w_in_r[kc * 4 + (c0 + 127) // 1408]] + xb_r, wr=[pr[bv_]])
            tg, tg_r = tg_l[gc % 2]
            tv, tv_r = tv_l[gc % 2]
            for (ps, psr, tt, ttr, col) in ((psg, pr[bg], tg, tg_r, gc), (psv, pr[bv_], tv, tv_r, FC + gc)):
                w0, w1, w2 = (k.vec(p + f"ffn_conv{j}", col) for j in range(3))
                cb = k.vec(p + "ffn_conv_b", col)
                P.op("act", "activation", tt, ps[:, 0:TT], AF.Identity, bias=cb, scale=w0,
                     rd=[psr, c["vr"]], wr=ttr)
                P.op("dve", "scalar_tensor_tensor", out=tt, in0=ps[:, 1:TT + 1], scalar=w1, in1=tt,
                     op0=ALU.mult, op1=ALU.add, rd=[psr, c["vr"]] + ttr, wr=ttr)
                P.op("dve", "scalar_tensor_tensor", out=tt, in0=ps[:, 2:TT + 2], scalar=w2, in1=tt,
                     op0=ALU.mult, op1=ALU.add, rd=[psr, c["vr"]] + ttr, wr=ttr)
            if pend is not None:
                fin(*pend)
            pend = (gc, tg, tg_r, tv, tv_r)
        fin(*pend)

        def ychunk(oc, ps, ps_r):
            for fc in range(FC):
                P.op("pe", "matmul", ps, w_out[:, fc, oc * 128:(oc + 1) * 128], gT[:, fc, :],
                     start=(fc == 0), stop=(fc == FC - 1), rd=[w_out_r[fc], gT_r[fc]], wr=[ps_r])
        ps_y = [(pb[4][:, 0:TT], pr[4]), (pb[5][:, 0:TT], pr[5])]
        ps_stat = [(pb[6][:, 0:TT], pr[6]), (pb[7][:, 0:TT], pr[7])]
        ln_tail(ctx, t, ti, ychunk, xs, xs_r[0], 2, p + "ln2_g", p + "ln2_b", dst, ps_y, ps_stat)


def phase_mla(ctx, l, src, dst):
    k, P, M, T, TT = ctx["k"], ctx["P"], ctx["M"], ctx["T"], ctx["TT"]
    W, c, pb, pr = ctx["W"], ctx["consts"], ctx["pbanks"], ctx["pres"]
    p = f"l{l}_"
    TQ = 512
    NQ = T // TQ
    NKT = T // 128
    SC = float(192 ** -0.5)
    PI = float(np.pi)
    vr = c["vr"]
    w_in_d, w_uq_d, w_ukv_d, w_o_d = W[p + "mla_w_in"], W[p + "mla_w_uq"], W[p + "mla_w_ukv"], W[p + "mla_w_o"]
    w_in, w_in_r = M.alloc("m_w_in", [DC, 512], BF16)
    for kc in range(DC):
        load_w(ctx, w_in[:, kc, 0:448], w_in_d[kc * 128:(kc + 1) * 128, :], w_in_r)
    P.op("dve", "tensor_scalar_mul", w_in[:, :, 448:480], w_in[:, :, 416:448], -1.0, rd=w_in_r, wr=w_in_r)
    P.op("dve", "tensor_copy", w_in[:, :, 480:512], w_in[:, :, 384:416], rd=w_in_r, wr=w_in_r)
    w_uq, w_uq_r = M.alloc("m_w_uq", [2, 2048], BF16)
    for c2 in range(2):
        load_w(ctx, w_uq[:, c2, 0:1536], w_uq_d[c2 * 128:(c2 + 1) * 128, :], w_uq_r)
    for c2 in range(2):
        srcv_ = w_uq[:, c2, 0:1536].rearrange("p (h d) -> p h d", h=8)
        dstv_ = w_uq[:, c2, 1536:2048].rearrange("p (h d) -> p h d", h=8)
        P.op("dve", "tensor_scalar_mul", dstv_[:, :, 0:32], srcv_[:, :, 160:192], -1.0, rd=w_uq_r, wr=w_uq_r)
        P.op("dve", "tensor_copy", dstv_[:, :, 32:64], srcv_[:, :, 128:160], rd=w_uq_r, wr=w_uq_r)
    w_ukv, w_ukv_r = M.alloc("m_w_ukv", [2048], BF16)
    load_w(ctx, w_ukv[:, :], w_ukv_d[:, :], w_ukv_r)
    w_o, w_o_r = M.alloc("m_w_o", [8, D], BF16)
    for h in range(8):
        load_w(ctx, w_o[:, h, :], w_o_d[h * 128:(h + 1) * 128, :], w_o_r)
    maskb, maskb_r = M.alloc("m_mask", [4, 512], BF16)
    load_w(ctx, maskb[:, :, :], ctx["mask_d"].rearrange("p (a b) -> p a b", a=4), maskb_r)
    wukT, wukT_r = M.alloc("m_wukT", [8, 128], BF16)
    for h in range(8):
        bk = 6 + h % 2
        pst = pb[bk].bitcast(BF16)[:, 0:128]
        P.op("pe", "transpose", pst, w_ukv[:, h * 256:h * 256 + 128], c["ident"], rd=w_ukv_r + [c["r_ones"]], wr=[pr[bk]])
        P.op("dve", "tensor_copy", wukT[:, h, :], pst, rd=[pr[bk]], wr=wukT_r)
    negpi, negpi_r = M.alloc("m_negpi", [2], F32)
    P.op("dve", "memset", negpi, -PI, wr=negpi_r)
    latT, latT_r = M.alloc("m_latT", [T], BF16, nres=NQ)
    krT, krT_r = M.alloc("m_krT", [T], BF16, nres=NQ)
    lat_tok, lat_tok_r = M.alloc("m_lat_tok", [NKT, 128], BF16, nres=NQ)
    xb_l = [M.alloc(f"m_xb{i}", [DC, TQ], BF16) for i in range(1)]
    xs, xs_r = M.alloc("m_xs", [DC, TT], F32)
    cqf, cqf_r = M.alloc("m_cqf", [2, TQ], F32, nres=2)
    sq, sq_r = M.alloc("m_sq", [2, TQ], BF16, nres=2)
    cqn, cqn_r = M.alloc("m_cqn", [2, TQ], BF16, nres=2)
    rstd, rstd_r = M.alloc("m_rstd", [TQ], F32)
    ckvf, ckvf_r = M.alloc("m_ckvf", [TQ], F32)
    sqkv, sqkv_r = M.alloc("m_sqkv", [TQ], BF16)
    posi, posi_r = M.alloc("m_posi", [TQ], I32)
    posf, posf_r = M.alloc("m_posf", [TQ], F32)
    a1, a1_r = M.alloc("m_a1", [TQ], F32)
    a0, a0_r = M.alloc("m_a0", [TQ], F32)
    a2, a2_r = M.alloc("m_a2", [TQ], F32)
    ang, ang_r = posf, posf_r
    cosf, cosf_r = M.alloc("m_cos", [TQ], F32)
    sinf, sinf_r = M.alloc("m_sin", [TQ], F32)
    coss, coss_r = M.alloc("m_coss", [TQ], F32)
    sins, sins_r = M.alloc("m_sins", [TQ], F32)
    tm1, tm1_r = M.alloc("m_tm1", [TQ], F32)
    tm2, tm2_r = M.alloc("m_tm2", [TQ], F32)
    qn_l = [M.alloc(f"m_qn{i}", [TQ], BF16) for i in range(2)]
    qt, qt_r = M.alloc("m_qt", [8, TQ], BF16, nres=8)
    qr, qr_r = M.alloc("m_qr", [8, TQ], BF16, nres=8)
    PT_l = [M.alloc(f"m_PT{i}", [TQ], BF16) for i in range(3)]
    rL, rL_r = M.alloc("m_rL", [TQ], F32)
    On_l = [M.alloc(f"m_On{i}", [TQ], BF16) for i in range(2)]
    oT, oT_r = M.alloc("m_oT", [8, TQ], BF16, nres=8)
    t = ln_tail_alloc(ctx)
    srcv = src.rearrange("(c p) t -> p c t", p=128)
    invf = c["cst"][0:64, 128:129]

    def load_xb(qb):
        xb, xb_r = xb_l[0]
        rdr = [k.dr(("x", id(src), qb * 2)), k.dr(("x", id(src), qb * 2 + 1))]
        P.dma("pool", xb[:, :, :], srcv[:, :, qb * TQ:(qb + 1) * TQ], rd=rdr, wr=xb_r)

    def rstd_from(ps, psr):
        P.op("dve", "tensor_scalar_add", rstd, ps, EPS, rd=[psr], wr=rstd_r)
        P.op("act", "activation", rstd, rstd, AF.Ln, rd=rstd_r, wr=rstd_r)
        P.op("act", "activation", rstd, rstd, AF.Exp, scale=-0.5, rd=rstd_r, wr=rstd_r)

    def proj(ps, psr, wcols, xb, xb_r, m=128):
        for kc in range(DC):
            P.op("pe", "matmul", ps[0:m, :], w_in[:, kc, wcols:wcols + m], xb[:, kc, :],
                 start=(kc == 0), stop=(kc == DC - 1), rd=w_in_r + xb_r, wr=[psr])

    load_xb(0)
    for qb in range(NQ):
        xb, xb_r = xb_l[0]
        if qb > 0:
            load_xb(qb)
        blk = slice(qb * TQ, (qb + 1) * TQ)
        p6, p7 = pb[6], pb[7]
        for c2 in range(2):
            ps, psr = (p6, pr[6]) if c2 == 0 else (p7, pr[7])
            proj(ps, psr, c2 * 128, xb, xb_r)
            P.op("act", "activation", cqf[:, c2, :], ps, AF.Copy, rd=[psr], wr=[cqf_r[c2]])
            P.op("act", "activation", sq[:, c2, :], cqf[:, c2, :], AF.Square, rd=[cqf_r[c2]], wr=[sq_r[c2]])
        for c2 in range(2):
            P.op("pe", "matmul", p6, c["ones256"], sq[:, c2, :], start=(c2 == 0), stop=(c2 == 1),
                 rd=[sq_r[c2], c["r_ones"]], wr=[pr[6]])
        rstd_from(p6, pr[6])
        for c2 in range(2):
            P.op("dve", "scalar_tensor_tensor", out=cqn[:, c2, :], in0=cqf[:, c2, :], scalar=k.vec(p + "mla_q_norm", c2),
                 in1=rstd, op0=ALU.mult, op1=ALU.mult, rd=[cqf_r[c2], vr] + rstd_r, wr=[cqn_r[c2]])
        proj(p7, pr[7], 256, xb, xb_r)
        P.op("act", "activation", ckvf, p7, AF.Copy, rd=[pr[7]], wr=ckvf_r)
        P.op("act", "activation", sqkv, ckvf, AF.Square, rd=ckvf_r, wr=sqkv_r)
        P.op("pe", "matmul", p6, c["ones128"], sqkv, start=True, stop=True, rd=sqkv_r + [c["r_ones"]], wr=[pr[6]])
        rstd_from(p6, pr[6])
        P.op("dve", "scalar_tensor_tensor", out=latT[:, blk], in0=ckvf, scalar=k.vec(p + "mla_kv_norm", 0),
             in1=rstd, op0=ALU.mult, op1=ALU.mult, rd=ckvf_r + [vr] + rstd_r, wr=[latT_r[qb]])
        P.dma("sp", posi[0:64, :], ctx["pos_d"][0:1, blk].partition_broadcast(64), wr=posi_r)
        P.op("dve", "tensor_copy", posf[0:64, :], posi[0:64, :], rd=posi_r, wr=posf_r)
        P.op("dve", "tensor_scalar_mul", ang[0:64, :], posf[0:64, :], invf, rd=posf_r + [c["r_cst"]], wr=ang_r)
        MAGIC = 12582912.0
        C1 = 6.28125
        C2 = float(2.0 * np.pi - 6.28125)
        for (dstt, dstr, off) in ((sinf, sinf_r, 0.0), (cosf, cosf_r, 0.5 * PI)):
            if off != 0.0:
                P.op("dve", "tensor_scalar_add", a0[0:64, :], ang[0:64, :], off, rd=ang_r, wr=a0_r)
                aa, aa_r = a0, a0_r
            else:
                aa, aa_r = ang, ang_r
            P.op("dve", "tensor_scalar", out=a1[0:64, :], in0=aa[0:64, :], scalar1=float(1.0 / (2.0 * np.pi)),
                 scalar2=MAGIC, op0=ALU.mult, op1=ALU.add, rd=aa_r, wr=a1_r)
            P.op("dve", "tensor_scalar_add", a1[0:64, :], a1[0:64, :], -MAGIC, rd=a1_r, wr=a1_r)
            P.op("dve", "scalar_tensor_tensor", out=a2[0:64, :], in0=a1[0:64, :], scalar=-C1, in1=aa[0:64, :],
                 op0=ALU.mult, op1=ALU.add, rd=a1_r + aa_r, wr=a2_r)
            P.op("dve", "scalar_tensor_tensor", out=a2[0:64, :], in0=a1[0:64, :], scalar=-C2, in1=a2[0:64, :],
                 op0=ALU.mult, op1=ALU.add, rd=a1_r + a2_r, wr=a2_r)
            P.op("dve", "tensor_scalar", out=a2[0:64, :], in0=a2[0:64, :], scalar1=-3.1415925, scalar2=3.1415925,
                 op0=ALU.max, op1=ALU.min, rd=a2_r, wr=a2_r)
            P.op("act", "activation", dstt[0:64, :], a2[0:64, :], AF.Sin, rd=a2_r, wr=dstr)
        P.op("pool", "tensor_scalar_mul", coss[0:64, :], cosf[0:64, :], SC, rd=cosf_r, wr=coss_r)
        P.op("pool", "tensor_scalar_mul", sins[0:64, :], sinf[0:64, :], SC, rd=sinf_r, wr=sins_r)
        proj(p6, pr[6], 384, xb, xb_r, m=64)
        proj(p7, pr[7], 448, xb, xb_r, m=64)
        P.op("dve", "tensor_tensor", out=tm1[0:64, :], in0=p6[0:64, :], in1=cosf[0:64, :], op=ALU.mult,
             rd=[pr[6]] + cosf_r, wr=tm1_r)
        P.op("dve", "tensor_tensor", out=tm2[0:64, :], in0=p7[0:64, :], in1=sinf[0:64, :], op=ALU.mult,
             rd=[pr[7]] + sinf_r, wr=tm2_r)
        P.op("pool", "tensor_tensor", out=krT[0:64, blk], in0=tm1[0:64, :], in1=tm2[0:64, :], op=ALU.add,
             rd=tm1_r + tm2_r, wr=[krT_r[qb]])
        for j in range(4):
            bk = 6 + j % 2
            pst = pb[bk].bitcast(BF16)[:, 0:128]
            P.op("pe", "transpose", pst, latT[:, qb * TQ + j * 128:qb * TQ + (j + 1) * 128], c["ident"],
                 rd=[latT_r[qb], c["r_ones"]], wr=[pr[bk]])
            P.op("dve", "tensor_copy", lat_tok[:, qb * 4 + j, :], pst, rd=[pr[bk]], wr=[lat_tok_r[qb]])
        for h in range(8):
            qn, qn_r = qn_l[h % 2]
            for c2 in range(2):
                P.op("pe", "matmul", p6, w_uq[:, c2, h * 192:h * 192 + 128], cqn[:, c2, :], start=(c2 == 0),
                     stop=(c2 == 1), rd=w_uq_r + [cqn_r[c2]], wr=[pr[6]])
            P.op("act", "activation", qn, p6, AF.Copy, rd=[pr[6]], wr=qn_r)
            P.op("pe", "matmul", p7, wukT[:, h, :], qn, start=True, stop=True, rd=wukT_r + qn_r, wr=[pr[7]])
            P.op("act", "activation", qt[:, h, :], p7, AF.Identity, scale=SC, rd=[pr[7]], wr=[qt_r[h]])
            for c2 in range(2):
                P.op("pe", "matmul", p6[0:64, :], w_uq[:, c2, h * 192 + 128:h * 192 + 192], cqn[:, c2, :],
                     start=(c2 == 0), stop=(c2 == 1), rd=w_uq_r + [cqn_r[c2]], wr=[pr[6]])
            for c2 in range(2):
                P.op("pe", "matmul", p7[0:64, :], w_uq[:, c2, 1536 + h * 64:1536 + (h + 1) * 64], cqn[:, c2, :],
                     start=(c2 == 0), stop=(c2 == 1), rd=w_uq_r + [cqn_r[c2]], wr=[pr[7]])
            P.op("dve", "tensor_tensor", out=tm1[0:64, :], in0=p6[0:64, :], in1=coss[0:64, :], op=ALU.mult,
                 rd=[pr[6]] + coss_r, wr=tm1_r)
            P.op("dve", "tensor_tensor", out=tm2[0:64, :], in0=p7[0:64, :], in1=sins[0:64, :], op=ALU.mult,
                 rd=[pr[7]] + sins_r, wr=tm2_r)
            P.op("pool", "tensor_tensor", out=qr[0:64, h, :], in0=tm1[0:64, :], in1=tm2[0:64, :], op=ALU.add,
                 rd=tm1_r + tm2_r, wr=[qr_r[h]])
        nkt = 4 * (qb + 1)
        kv_rd = lambda kt: [latT_r[kt // 4], krT_r[kt // 4]]
        it = 0
        for h in range(8):
            Ob, Lb = 2 + h % 2, 4 + h % 2
            O, L = pb[Ob], pb[Lb]
            for kt in range(nkt):
                Sb = it % 2
                S = pb[Sb]
                PT, PT_r = PT_l[it % 3]
                it += 1
                diag = kt >= 4 * qb
                ks = slice(kt * 128, (kt + 1) * 128)
                P.op("pe", "matmul", S, latT[:, ks], qt[:, h, :], start=True, stop=False,
                     rd=[latT_r[kt // 4], qt_r[h]], wr=[pr[Sb]])
                P.op("pe", "matmul", S, krT[0:64, ks], qr[0:64, h, :], start=False, stop=(not diag),
                     rd=[krT_r[kt // 4], qr_r[h]], wr=[pr[Sb]])
                if diag:
                    P.op("pe", "matmul", S, c["ident"], maskb[:, kt - 4 * qb, :], start=False, stop=True,
                         rd=maskb_r + [c["r_ones"]], wr=[pr[Sb]])
                P.op("act", "activation", PT, S, AF.Exp, rd=[pr[Sb]], wr=PT_r)
                P.op("pe", "matmul", O, lat_tok[:, kt, :], PT, start=(kt == 0), stop=(kt == nkt - 1),
                     rd=[lat_tok_r[kt // 4]] + PT_r, wr=[pr[Ob]])
                P.op("pe", "matmul", L, c["ones1"], PT, start=(kt == 0), stop=(kt == nkt - 1),
                     rd=[c["r_ones"]] + PT_r, wr=[pr[Lb]])
            On, On_r = On_l[h % 2]
            P.op("dve", "reciprocal", rL, L, rd=[pr[Lb]], wr=rL_r)
            P.op("dve", "tensor_tensor", out=On, in0=O, in1=rL, op=ALU.mult, rd=[pr[Ob]] + rL_r, wr=On_r)
            ob = 6 + h % 2
            P.op("pe", "matmul", pb[ob], w_ukv[:, h * 256 + 128:h * 256 + 256], On, start=True, stop=True,
                 rd=w_ukv_r + On_r, wr=[pr[ob]])
            P.op("dve", "tensor_copy", oT[:, h, :], pb[ob], rd=[pr[ob]], wr=[oT_r[h]])
        for j in range(TQ // TT):
            def ychunk(oc, ps, ps_r, j=j):
                for h in range(8):
                    P.op("pe", "matmul", ps, w_o[:, h, oc * 128:(oc + 1) * 128], oT[:, h, j * TT:(j + 1) * TT],
                         start=(h == 0), stop=(h == 7), rd=w_o_r + [oT_r[h]], wr=[ps_r])
            ps_y = [(pb[6][:, 0:TT], pr[6]), (pb[7][:, 0:TT], pr[7])]
            ps_stat = [(pb[0][:, 0:TT], pr[0]), (pb[1][:, 0:TT], pr[1])]
            tix = qb * (TQ // TT) + j
            P.dma("sp", xs[:, :, :], srcv[:, :, tix * TT:(tix + 1) * TT], rd=[k.dr(("x", id(src), tix))], wr=xs_r)
            ln_tail(ctx, t, tix, ychunk, xs, xs_r[0], 0, p + "ln1_g", p + "ln1_b", dst, ps_y, ps_stat)


def phase_gla(ctx, l, src, dst):
    k, P, M, T, TT, NT = ctx["k"], ctx["P"], ctx["M"], ctx["T"], ctx["TT"], ctx["NT"]
    W, c, pb, pr = ctx["W"], ctx["consts"], ctx["pbanks"], ctx["pres"]
    p = f"l{l}_"
    vr = c["vr"]
    w_in_d, w_a2_d, b_a_d, w_o_d = W[p + "gla_w_in"], W[p + "gla_w_a2"], W[p + "gla_b_a"], W[p + "gla_w_o"]
    NW = 3088
    w_in, w_in_r = M.alloc("g_w_in", [DC, NW], BF16, nres=DC)
    for kc in range(DC):
        load_w(ctx, w_in[:, kc, :], w_in_d[kc * 128:(kc + 1) * 128, :], [w_in_r[kc]])
    w_o, w_o_r = M.alloc("g_w_o", [DC, D], BF16, nres=DC)
    for kc in range(DC):
        load_w(ctx, w_o[:, kc, :], w_o_d[kc * 128:(kc + 1) * 128, :], [w_o_r[kc]])
    w_a2b, w_a2b_r = M.alloc("g_w_a2b", [512], BF16)
    load_w(ctx, w_a2b[0:16, :], w_a2_d[:, :], w_a2b_r)
    load_w(ctx, w_a2b[16:17, :], b_a_d[:, :], w_a2b_r)
    gc3, gc3_r = M.alloc("g_c3", [3, 128], BF16)
    load_w(ctx, gc3[:, :, :], ctx["glac_d"].rearrange("p (a b) -> p a b", a=3), gc3_r)
    triU, triR = gc3[:, 0, :], gc3[:, 1, :]
    maskA, maskA_r = M.alloc("g_maskA", [128], F32)
    P.dma("sp", maskA, ctx["glac_d"][:, 256:384], wr=maskA_r)
    onec, onec_r = M.alloc("g_onec", [2], F32)
    P.op("dve", "memset", onec, 1.0, wr=onec_r)
    alr1, alr1_r = M.alloc("g_alr1", [128], BF16)
    P.op("dve", "memset", alr1[0:32, :], 1.0, wr=alr1_r)
    Sf, Sf_r = M.alloc("g_Sf", [4, 256], F32, nres=4)
    Sb, Sb_r = M.alloc("g_Sb", [4, 256], BF16, nres=4)
    for h in range(4):
        P.op("dve", "memset", Sf[:, h, :], 0.0, wr=[Sf_r[h]])
        P.op("dve", "memset", Sb[:, h, :], 0.0, wr=[Sb_r[h]])
    xs_l = [M.alloc(f"g_xs{i}", [DC, TT], F32) for i in range(2)]
    xb_l = [M.alloc(f"g_xb{i}", [DC, TT], BF16) for i in range(2)]
    Lf, Lf_r = M.alloc("g_Lf", [512], F32)
    Lb, Lb_r = M.alloc("g_Lb", [512], BF16)
    Ef, Ef_r = M.alloc("g_Ef", [4, 128], F32)
    Emf, Emf_r = M.alloc("g_Emf", [4, 128], F32)
    Erf, Erf_r = M.alloc("g_Erf", [512], F32)
    qtl, qtl_r = M.alloc("g_qtl", [4, 128], BF16)
    ktl, ktl_r = M.alloc("g_ktl", [4, 128], BF16)
    kdec, kdec_r = M.alloc("g_kdec", [512], BF16)
    v_bf, v_bf_r = M.alloc("g_vbf", [1024], BF16, nres=2)
    attn, attn_r = M.alloc("g_attn", [4, 128], BF16, nres=4)
    osq, osq_r = M.alloc("g_osq", [2, 4, 128], BF16, nres=2)
    rso, rso_r = M.alloc("g_rso", [4, 128], F32)
    sil, sil_r = M.alloc("g_sil", [8, 128], F32, nres=2)
    tmp_l = [M.alloc(f"g_tmp{i}", [128], F32) for i in range(2)]
    gT, gT_r = M.alloc("g_gT", [DC, TT], BF16, nres=DC)
    t = ln_tail_alloc(ctx)
    srcv = src.rearrange("(c p) t -> p c t", p=128)
    QS = float(128 ** -0.5)

    def load_x(ti, slot):
        xs, xs_r = xs_l[slot]
        xb, xb_r = xb_l[slot]
        rdr = [k.dr(("x", id(src), ti))]
        P.dma("sp", xs[:, :, :], srcv[:, :, ti * TT:(ti + 1) * TT], rd=rdr, wr=xs_r)
        P.dma("pool", xb[:, :, :], srcv[:, :, ti * TT:(ti + 1) * TT], rd=rdr, wr=xb_r)

    def fm_proj(ps, psr, col0, m, xb, xb_r, tsl):
        for kc in range(DC):
            P.op("pe", "matmul", ps, w_in[:, kc, col0:col0 + m], xb[:, kc, tsl], start=(kc == 0), stop=(kc == DC - 1),
                 rd=[w_in_r[kc]] + xb_r, wr=[psr])

    def tm_proj(ps, psr, col0, n, xb, xb_r, tsl):
        for kc in range(DC):
            P.op("pe", "matmul", ps, xb[:, kc, tsl], w_in[:, kc, col0:col0 + n], start=(kc == 0), stop=(kc == DC - 1),
                 rd=[w_in_r[kc]] + xb_r, wr=[psr])

    def hs(h):
        return slice(h * 128, (h + 1) * 128)

    load_x(0, 0)
    for ti in range(NT):
        slot = ti % 2
        if ti + 1 < NT:
            load_x(ti + 1, (ti + 1) % 2)
        xs, xs_r = xs_l[slot]
        xb, xb_r = xb_l[slot]
        for sub in range(TT // 128):
            tsl = slice(sub * 128, (sub + 1) * 128)
            for h in range(4):
                fm_proj(pb[0][:, hs(h)], pr[0], h * 128, 128, xb, xb_r, tsl)
            for h in range(4):
                fm_proj(pb[1][:, hs(h)], pr[1], 512 + h * 128, 128, xb, xb_r, tsl)
            fm_proj(pb[7][0:16, 0:128], pr[7], 2048, 16, xb, xb_r, tsl)
            tm_proj(pb[2], pr[2], 512, 512, xb, xb_r, tsl)
            tm_proj(pb[3], pr[3], 1024, 512, xb, xb_r, tsl)
            tm_proj(pb[4], pr[4], 1536, 512, xb, xb_r, tsl)
            P.op("act", "activation", alr1[0:16, :], pb[7][0:16, 0:128], AF.Copy, rd=[pr[7]], wr=alr1_r)
            P.op("pe", "matmul", pb[5], alr1[0:17, :], w_a2b[0:17, :], start=True, stop=True,
                 rd=alr1_r + w_a2b_r, wr=[pr[5]])
            P.op("act", "activation", Lf, pb[5], AF.Exp, scale=-1.0, rd=[pr[5]], wr=Lf_r)
            P.op("act", "activation", Lb, Lf, AF.Ln, bias=onec[:, 0:1], scale=1.0, rd=Lf_r + onec_r, wr=Lb_r)
            for h in range(4):
                P.op("pe", "matmul", pb[6][:, hs(h)], Lb[:, hs(h)], triU, start=True, stop=True,
                     rd=Lb_r + gc3_r, wr=[pr[6]])
            P.op("pe", "matmul", pb[7], triR, Lb, start=True, stop=True, rd=Lb_r + gc3_r, wr=[pr[7]])
            Ef2 = Ef.rearrange("p a b -> p (a b)")
            Emf2 = Emf.rearrange("p a b -> p (a b)")
            P.op("act", "activation", Ef2, pb[6], AF.Exp, rd=[pr[6]], wr=Ef_r)
            P.op("act", "activation", Emf2, pb[6], AF.Exp, scale=-1.0, rd=[pr[6]], wr=Emf_r)
            P.op("act", "activation", Erf, pb[7], AF.Exp, rd=[pr[7]], wr=Erf_r)
            P.op("dve", "scalar_tensor_tensor", out=qtl.rearrange("p a b -> p (a b)"), in0=pb[0], scalar=QS, in1=Ef2,
                 op0=ALU.mult, op1=ALU.mult, rd=[pr[0]] + Ef_r, wr=qtl_r)
            P.op("dve", "tensor_tensor", out=ktl.rearrange("p a b -> p (a b)"), in0=pb[1], in1=Emf2, op=ALU.mult,
                 rd=[pr[1]] + Emf_r, wr=ktl_r)
            P.op("dve", "tensor_tensor", out=kdec, in0=pb[2], in1=Erf, op=ALU.mult, rd=[pr[2]] + Erf_r, wr=kdec_r)
            P.op("act", "activation", v_bf[:, 0:512], pb[3], AF.Copy, rd=[pr[3]], wr=[v_bf_r[0]])
            P.op("dve", "tensor_copy", v_bf[:, 512:1024], pb[4], rd=[pr[4]], wr=[v_bf_r[1]])
            for h in range(4):
                P.op("pe", "matmul", pb[2][:, hs(h)], ktl[:, h, :], qtl[:, h, :], start=True, stop=True,
                     rd=ktl_r + qtl_r, wr=[pr[2]])
                P.op("dve", "tensor_tensor", out=attn[:, h, :], in0=pb[2][:, hs(h)], in1=maskA, op=ALU.mult,
                     rd=[pr[2]] + maskA_r, wr=[attn_r[h]])
            dsi = 0
            for h in range(4):
                vr_h = [v_bf_r[(h * 256) // 512]]
                for eh in range(2):
                    P.op("pe", "matmul", pb[3 + eh][:, hs(h)], v_bf[:, h * 256 + eh * 128:h * 256 + (eh + 1) * 128],
                         attn[:, h, :], start=True, stop=False, rd=vr_h + [attn_r[h]], wr=[pr[3 + eh]])
                for ci in range(2):
                    cs = slice(ci * 64, (ci + 1) * 64)
                    for eh in range(2):
                        P.op("pe", "matmul", pb[3 + eh][:, h * 128 + ci * 64:h * 128 + (ci + 1) * 64],
                             Sb[:, h, eh * 128:(eh + 1) * 128], qtl[:, h, cs], start=False, stop=(ci == 1),
                             skip_group_check=True, rd=[Sb_r[h]] + qtl_r, wr=[pr[3 + eh]])
                    db = 5 if dsi % 2 == 0 else 7
                    dsi += 1
                    dps = pb[db][:, 0:256]
                    P.op("pe", "matmul", dps, kdec[cs, hs(h)], v_bf[cs, h * 256:(h + 1) * 256], start=True, stop=True,
                         rd=kdec_r + vr_h, wr=[pr[db]])
                    P.op("dve", "scalar_tensor_tensor", out=Sf[:, h, :], in0=Sf[:, h, :],
                         scalar=Ef[:, h, ci * 64 + 63:ci * 64 + 64], in1=dps, op0=ALU.mult, op1=ALU.add,
                         rd=[Sf_r[h], pr[db]] + Ef_r, wr=[Sf_r[h]])
                    P.op("pool", "tensor_copy", Sb[:, h, :], Sf[:, h, :], rd=[Sf_r[h]], wr=[Sb_r[h]])
            for eh in range(2):
                P.op("act", "activation", osq[:, eh, :, :].rearrange("p a b -> p (a b)"), pb[3 + eh], AF.Square,
                     rd=[pr[3 + eh]], wr=[osq_r[eh]])
            for h in range(4):
                for eh in range(2):
                    P.op("pe", "matmul", pb[6][:, hs(h)], c["ones256"], osq[:, eh, h, :], start=(eh == 0), stop=(eh == 1),
                         rd=[osq_r[eh], c["r_ones"]], wr=[pr[6]])
            rso2 = rso.rearrange("p a b -> p (a b)")
            P.op("dve", "tensor_scalar_add", rso2, pb[6], EPS, rd=[pr[6]], wr=rso_r)
            P.op("act", "activation", rso2, rso2, AF.Ln, rd=rso_r, wr=rso_r)
            P.op("act", "activation", rso2, rso2, AF.Exp, scale=-0.5, rd=rso_r, wr=rso_r)
            for cidx in range(8):
                bk = cidx // 4
                fm_proj(pb[bk][:, hs(cidx % 4)], pr[bk], 2064 + cidx * 128, 128, xb, xb_r, tsl)
            for bk in range(2):
                P.op("act", "activation", sil[:, bk * 4:(bk + 1) * 4, :].rearrange("p a b -> p (a b)"), pb[bk], AF.Silu,
                     rd=[pr[bk]], wr=[sil_r[bk]])
            for h in range(4):
                for eh in range(2):
                    cidx = h * 2 + eh
                    tmp, tmp_r = tmp_l[cidx % 2]
                    P.op("dve", "scalar_tensor_tensor", out=tmp, in0=pb[3 + eh][:, hs(h)],
                         scalar=k.vec(p + "gla_out_norm", eh), in1=rso[:, h, :], op0=ALU.mult, op1=ALU.mult,
                         rd=[pr[3 + eh], vr] + rso_r, wr=tmp_r)
                    P.op("pool", "tensor_tensor", out=gT[:, cidx, tsl], in0=tmp, in1=sil[:, cidx, :], op=ALU.mult,
                         rd=tmp_r + [sil_r[cidx // 4]], wr=[gT_r[cidx]])

        def ychunk(oc, ps, ps_r):
            for cc in range(DC):
                P.op("pe", "matmul", ps, w_o[:, cc, oc * 128:(oc + 1) * 128], gT[:, cc, :],
                     start=(cc == 0), stop=(cc == DC - 1), rd=[w_o_r[cc], gT_r[cc]], wr=[ps_r])
        ps_y = [(pb[0][:, 0:TT], pr[0]), (pb[1][:, 0:TT], pr[1])]
        ps_stat = [(pb[2][:, 0:TT], pr[2]), (pb[5][:, 0:TT], pr[5])]
        ln_tail(ctx, t, ti, ychunk, xs, xs_r[0], 0, p + "ln1_g", p + "ln1_b", dst, ps_y, ps_stat)


def phase_conv(ctx, l, src, dst):
    k, P, M, T, TT, NT = ctx["k"], ctx["P"], ctx["M"], ctx["T"], ctx["TT"], ctx["NT"]
    W, c, pb, pr = ctx["W"], ctx["consts"], ctx["pbanks"], ctx["pres"]
    p = f"l{l}_"
    H = 30
    w_in_d, w_o_d = W[p + "conv_w_in"], W[p + "conv_w_o"]
    w_in, w_in_r = M.alloc("cv_w_in", [DC, 2 * D], BF16, nres=DC)
    w_o, w_o_r = M.alloc("cv_w_o", [DC, D], BF16, nres=DC)
    for kc in range(DC):
        load_w(ctx, w_in[:, kc, :], w_in_d[kc * 128:(kc + 1) * 128, :], [w_in_r[kc]])
    for kc in range(DC):
        load_w(ctx, w_o[:, kc, :], w_o_d[kc * 128:(kc + 1) * 128, :], [w_o_r[kc]])
    NB = 2
    xs_l = [M.alloc(f"cxs{i}", [DC, TT], F32) for i in range(NB)]
    xb_l = [M.alloc(f"cxb{i}", [DC, TT + H], BF16) for i in range(NB)]
    sg_l = [M.alloc(f"csg{i}", [TT + H], F32) for i in range(2)]
    u_l = [M.alloc(f"cu{i}", [TT + H], F32) for i in range(2)]
    aD_l = [M.alloc(f"caD{i}", [TT], F32) for i in range(2)]
    aP_l = [M.alloc(f"caP{i}", [TT], F32) for i in range(2)]
    tm_l = [M.alloc(f"ctm{i}", [TT], F32) for i in range(2)]
    gs, gs_r = M.alloc("cgs", [DC, TT], BF16, nres=DC)
    t2 = ln_tail_alloc(ctx)
    t = ln_tail_alloc(ctx)
    srcv = src.rearrange("(c p) t -> p c t", p=128)

    def load_x(ti, slot):
        xs, xs_r = xs_l[slot]
        xb, xb_r = xb_l[slot]
        t0 = ti * TT
        rdr = [k.dr(("x", id(src), ti))]
        P.dma("sp", xs[:, :, :], srcv[:, :, t0:t0 + TT], rd=rdr, wr=xs_r)
        if ti == 0:
            P.op("pool", "memset", xb[:, :, 0:H], 0.0, wr=xb_r)
            P.dma("pool", xb[:, :, H:TT + H], srcv[:, :, 0:TT], rd=rdr, wr=xb_r)
        else:
            rdr.append(k.dr(("x", id(src), ti - 1)))
            P.dma("pool", xb[:, :, :], srcv[:, :, t0 - H:t0 + TT], rd=rdr, wr=xb_r)

    load_x(0, 0)
    for ti in range(NT):
        slot = ti % NB
        if ti + 1 < NT:
            load_x(ti + 1, (ti + 1) % NB)
        xs, xs_r = xs_l[slot]
        xb, xb_r = xb_l[slot]
        for cc in range(DC):
            ba, bg = (cc % 2) * 2, (cc % 2) * 2 + 1
            psa, psg = pb[ba][:, 0:TT + H], pb[bg][:, 0:TT + H]
            for kc in range(DC):
                P.op("pe", "matmul", psa, w_in[:, kc, cc * 128:(cc + 1) * 128], xb[:, kc, :],
                     start=(kc == 0), stop=(kc == DC - 1), rd=[w_in_r[kc]] + xb_r, wr=[pr[ba]])
            for kc in range(DC):
                P.op("pe", "matmul", psg, w_in[:, kc, D + cc * 128:D + (cc + 1) * 128], xb[:, kc, :],
                     start=(kc == 0), stop=(kc == DC - 1), rd=[w_in_r[kc]] + xb_r, wr=[pr[bg]])
            sg, sg_r = sg_l[cc % 2]
            u, u_r = u_l[cc % 2]
            aD, aD_r = aD_l[cc % 2]
            aP, aP_r = aP_l[cc % 2]
            P.op("act", "activation", sg, psg, AF.Sigmoid, bias=k.vec(p + "conv_b_in", DC + cc), scale=1.0,
                 rd=[pr[bg], c["vr"]], wr=sg_r)
            P.op("dve", "scalar_tensor_tensor", out=u, in0=psa, scalar=k.vec(p + "conv_b_in", cc), in1=sg,
                 op0=ALU.add, op1=ALU.mult, rd=[pr[ba], c["vr"]] + sg_r, wr=u_r)
            if ti == 0:
                P.op("pool", "memset", u[:, 0:H], 0.0, rd=u_r, wr=u_r)
            P.op("act", "activation", aD, u[:, 0:TT], AF.Identity, bias=k.vec(p + "conv_dw_b", cc),
                 scale=k.vec(p + "conv_dw0", cc), rd=u_r + [c["vr"]], wr=aD_r)
            ND = 18
            for j in range(1, ND + 1):
                P.op("dve", "scalar_tensor_tensor", out=aD, in0=u[:, j:j + TT], scalar=k.vec(p + f"conv_dw{j}", cc),
                     in1=aD, op0=ALU.mult, op1=ALU.add, rd=u_r + aD_r + [c["vr"]], wr=aD_r)
            P.op("act", "activation", aP, u[:, ND + 1:ND + 1 + TT], AF.Identity, scale=k.vec(p + f"conv_dw{ND + 1}", cc),
                 rd=u_r + [c["vr"]], wr=aP_r)
            for j in range(ND + 2, 31):
                tm, tm_r = tm_l[j % 2]
                P.op("act", "activation", tm, u[:, j:j + TT], AF.Identity, scale=k.vec(p + f"conv_dw{j}", cc),
                     rd=u_r + [c["vr"]], wr=tm_r)
                P.op("pool", "tensor_tensor", out=aP, in0=aP, in1=tm, op=ALU.add, rd=aP_r + tm_r, wr=aP_r)
            ho = t2["r"][:, cc, :]
            P.op("dve", "tensor_tensor", out=ho, in0=aD, in1=aP, op=ALU.add, rd=aD_r + aP_r, wr=[t2["r_r"][cc]])
            P.op("pool", "tensor_copy", t2["rb"][:, cc, :], ho, rd=[t2["r_r"][cc]], wr=[t2["rb_r"][cc]])
            P.op("act", "activation", t2["rsq"][:, cc, :], ho, AF.Square, rd=[t2["r_r"][cc]], wr=[t2["rsq_r"][cc]])
        t2["ps_stat"] = [(pb[4][:, 0:TT], pr[4]), (pb[5][:, 0:TT], pr[5])]

        def fin(oc, ro, rres):
            P.op("act", "activation", gs[:, oc, :], ro, AF.Silu, rd=[rres], wr=[gs_r[oc]])
        ln_core(ctx, t2, p + "conv_ln_g", p + "conv_ln_b", fin)

        def ychunk(oc, ps, ps_r):
            for cc in range(DC):
                P.op("pe", "matmul", ps, w_o[:, cc, oc * 128:(oc + 1) * 128], gs[:, cc, :],
                     start=(cc == 0), stop=(cc == DC - 1), rd=[w_o_r[cc], gs_r[cc]], wr=[ps_r])
        ps_y = [(pb[6][:, 0:TT], pr[6]), (pb[7][:, 0:TT], pr[7])]
        ps_stat = [(pb[4][:, 0:TT], pr[4]), (pb[5][:, 0:TT], pr[5])]
        ln_tail(ctx, t, ti, ychunk, xs, xs_r[0], 0, p + "ln1_g", p + "ln1_b", dst, ps_y, ps_stat,
                bias_name=p + "conv_b_o")


WNAMES = {
    0: ["mla_w_in", "mla_w_uq", "mla_w_ukv", "mla_w_o"],
    1: ["gla_w_in", "gla_w_a2", "gla_b_a", "gla_w_o"],
    2: ["conv_w_in", "conv_w_o"],
}


def make_consts():
    cst = np.zeros((128, 160), np.float32)
    maskb = np.zeros((128, 2048), np.float32)
    cst[:, 0:128] = np.eye(128, dtype=np.float32)
    half = 32
    inv = (10000.0 ** (-np.arange(half, dtype=np.float32) / half)).astype(np.float32)
    cst[0:32, 128] = inv
    cst[32:64, 128] = inv
    cst[64:96, 128] = inv
    cst[96:128, 128] = inv
    qq = np.arange(512)[None, :]
    for d in range(4):
        kk = (d * 128 + np.arange(128))[:, None]
        maskb[:, d * 512:(d + 1) * 512] = np.where(kk <= qq, 0.0, NEG)
    return cst, maskb


def make_glac():
    g = np.zeros((128, 384), np.float32)
    j = np.arange(128)[:, None]
    i = np.arange(128)[None, :]
    same = (j // 64) == (i // 64)
    g[:, 0:128] = np.where(same & (j <= i), -1.0 / 16.0, 0.0)
    g[:, 128:256] = np.where(same & (j > i), -1.0 / 16.0, 0.0)
    g[:, 256:384] = np.where(same & (j <= i), 1.0, 0.0)
    return g


def kernel(**inputs):
    inp = {kk: np.asarray(v) for kk, v in inputs.items()}
    x = inp["x"]
    B, S, _ = x.shape
    vp = build_vecpack(inp)
    vecs = vp.pack()
    layers = [(l, True, True) for l in range(DEPTH)]
    shapes = {}
    wmaps = {}
    for l in range(DEPTH):
        p = f"l{l}_"
        names = WNAMES[layer_kind(l)] + ["ffn_w_in", "ffn_w_out"]
        for nm in names:
            a = np.ascontiguousarray(inp[p + nm], dtype=np.float32)
            if a.ndim == 1:
                a = a.reshape(1, -1)
            shapes[p + nm] = a.shape
            wmaps[p + nm] = a
    nc, _ = build_program(S, shapes, vp.off, vp.n, layers)
    cst, maskb = make_consts()
    in_maps = []
    for b in range(B):
        m = dict(wmaps)
        m["xT"] = np.ascontiguousarray(x[b].T)
        m["vecs"] = vecs
        m["pos"] = np.ascontiguousarray(inp["positions"][b].reshape(1, S).astype(np.int32))
        m["cst"] = cst
        m["maskb"] = maskb
        m["glac"] = make_glac()
        in_maps.append(m)
    res = run_bass_kernel_spmd(nc, in_maps, core_ids=list(range(B)))
    out = np.stack([np.ascontiguousarray(res.results[b]["outT"].T) for b in range(B)], axis=0)
    return out.astype(np.float32)
```

```python
import numpy as np
import concourse.bass as bass
import concourse.mybir as mybir
from concourse.bass_utils import run_bass_kernel_spmd

F32 = mybir.dt.float32
BF16 = mybir.dt.bfloat16
I32 = mybir.dt.int32
AF = mybir.ActivationFunctionType
ALU = mybir.AluOpType
AX = mybir.AxisListType

D = 1024
DC = 8
FF = 2816
FC = 22
DEPTH = 4
ALPHA = float((2.0 * DEPTH) ** 0.25)
EPS = 1e-5
SAME_ENG_SYNC = False
NEG = -30000.0


class Res:
    __slots__ = ("name", "w", "rs", "pred")

    def __init__(self, name):
        self.name = name
        self.w = None
        self.rs = []
        self.pred = []


class Op:
    __slots__ = ("eng", "fn", "deps", "inc", "tick", "dma", "sem", "idx")

    def __init__(self, eng, fn, dma):
        self.eng = eng
        self.fn = fn
        self.deps = []
        self.inc = False
        self.tick = 0
        self.dma = dma
        self.sem = None
        self.idx = 0


ENGS = ("pe", "act", "dve", "pool", "sp")
NDMASEM = 8


class Prog:
    def __init__(self):
        self.streams = {e: [] for e in ENGS}
        self.ndma = {e: 0 for e in ENGS}
        self.dma_hist = {e: [] for e in ENGS}
        self.n = 0

    def _deps(self, op, rd, wr):
        deps = op.deps
        for r in rd:
            if r.pred:
                deps.extend(r.pred)
            if r.w is not None:
                deps.append(r.w)
        for r in wr:
            if r.pred:
                deps.extend(r.pred)
            if r.w is not None:
                deps.append(r.w)
            deps.extend(r.rs)
        for r in rd:
            r.rs.append(op)
            r.pred = []
        for r in wr:
            r.w = op
            r.rs = []
            r.pred = []
        for d in deps:
            if d.dma or d.eng != op.eng or (SAME_ENG_SYNC and op.eng != "pe"):
                d.inc = True

    def op(self, eng, meth, *args, rd=(), wr=(), **kw):
        fn = (lambda e, meth=meth, args=args, kw=kw: getattr(e, meth)(*args, **kw))
        o = Op(eng, fn, False)
        o.idx = self.n
        self.n += 1
        self._deps(o, rd, wr)
        self.streams[eng].append(o)
        return o

    def dma(self, eng, out, in_, rd=(), wr=()):
        o = Op(eng, (lambda e, out=out, in_=in_: e.dma_start(out=out, in_=in_)), True)
        o.idx = self.n
        self.n += 1
        k = self.ndma[eng]
        self.ndma[eng] = k + 1
        o.sem = (eng, k % NDMASEM)
        o.tick = 16 * (k // NDMASEM + 1)
        hist = self.dma_hist[eng]
        if k >= NDMASEM:
            o.deps.append(hist[k - NDMASEM])
        hist.append(o)
        self._deps(o, rd, wr)
        o.inc = True
        self.streams[eng].append(o)
        return o

    def retire(self, resources):
        out = []
        for r in resources:
            if r.w is not None:
                out.append(r.w)
            out.extend(r.rs)
            out.extend(r.pred)
        last = {}
        res = []
        for o in out:
            if o.dma:
                res.append(o)
            else:
                if o.eng not in last or last[o.eng].idx < o.idx:
                    last[o.eng] = o
        res.extend(last.values())
        for o in res:
            o.inc = True
        return res

    def emit(self, nc, block, sems, dsems):
        for e in ENGS:
            t = 0
            for o in self.streams[e]:
                if not o.dma and o.inc:
                    t += 1
                    o.tick = t
        streams = self.streams

        def run(eng_name, e):
            seen = {}
            for o in streams[eng_name]:
                need = {}
                for d in o.deps:
                    if d.dma:
                        key = d.sem
                    else:
                        if d.eng == eng_name and (eng_name == "pe" or not SAME_ENG_SYNC):
                            continue
                        key = d.eng
                    if need.get(key, 0) < d.tick:
                        need[key] = d.tick
                for key, val in need.items():
                    if seen.get(key, 0) >= val:
                        continue
                    seen[key] = val
                    s = dsems[key] if isinstance(key, tuple) else sems[key]
                    e.wait_ge(s, val)
                ins = o.fn(e)
                if o.inc:
                    if o.dma:
                        ins.then_inc(dsems[o.sem], 16)
                    else:
                        ins.then_inc(sems[eng_name], 1)

        @block.tensor
        def _(e):
            run("pe", e)

        @block.scalar
        def _(e):
            run("act", e)

        @block.vector
        def _(e):
            run("dve", e)

        @block.gpsimd
        def _(e):
            run("pool", e)

        @block.sync
        def _(e):
            run("sp", e)


class Mem:
    def __init__(self, P, big, words):
        self.P = P
        self.big = big
        self.words = words
        self.top = 0
        self.live = []

    def mark(self):
        return self.top

    def release(self, mark):
        self.top = mark

    def alloc(self, name, shape, dtype, nres=1):
        n = 1
        for s in shape:
            n *= s
        if dtype == BF16:
            w = (n + 1) // 2
        else:
            w = n
        a = self.top
        b = a + w
        assert b <= self.words, f"SBUF overflow allocating {name}: {b} > {self.words}"
        self.top = b
        pred = []
        keep = []
        for (s, e, rl) in self.live:
            if s < b and a < e:
                pred.extend(self.P.retire(rl))
            else:
                keep.append((s, e, rl))
        self.live = keep
        rl = [Res(f"{name}{i}") for i in range(nres)]
        for r in rl:
            r.pred = list(pred)
        self.live.append((a, b, rl))
        ap = self.big[:, a:b]
        if dtype == BF16:
            ap = ap.bitcast(BF16)[:, 0:n]
        elif dtype == I32:
            ap = ap.bitcast(I32)
        if len(shape) == 2:
            ap = ap.rearrange("p (a b) -> p a b", a=shape[0])
        elif len(shape) == 3:
            ap = ap.rearrange("p (a b c) -> p a b c", a=shape[0], b=shape[1])
        return ap, rl


def fm(v):
    v = np.asarray(v, np.float32)
    return np.ascontiguousarray(v.reshape(-1, 128).T)


class VecPack:
    def __init__(self):
        self.cols = []
        self.off = {}
        self.n = 0

    def add(self, name, arr2d):
        arr2d = np.asarray(arr2d, np.float32)
        assert arr2d.shape[0] == 128
        self.off[name] = (self.n, arr2d.shape[1])
        self.cols.append(arr2d)
        self.n += arr2d.shape[1]

    def pack(self):
        return np.ascontiguousarray(np.concatenate(self.cols, axis=1))


def layer_kind(i):
    return i % 3


def build_vecpack(inp):
    vp = VecPack()
    for l in range(DEPTH):
        p = f"l{l}_"
        for nm in ("ln1_g", "ln1_b", "ln2_g", "ln2_b"):
            vp.add(p + nm, fm(inp[p + nm]))
        cw = np.asarray(inp[p + "ffn_conv"], np.float32)
        for k in range(3):
            vp.add(p + f"ffn_conv{k}", fm(cw[k]))
        vp.add(p + "ffn_conv_b", fm(inp[p + "ffn_conv_b"]))
        kind = layer_kind(l)
        if kind == 0:
            vp.add(p + "mla_q_norm", fm(inp[p + "mla_q_norm"]))
            vp.add(p + "mla_kv_norm", fm(inp[p + "mla_kv_norm"]))
        elif kind == 1:
            vp.add(p + "gla_b_a", fm(inp[p + "gla_b_a"]))
            vp.add(p + "gla_out_norm", fm(inp[p + "gla_out_norm"]))
        else:
            vp.add(p + "conv_b_in", fm(inp[p + "conv_b_in"]))
            dw = np.asarray(inp[p + "conv_dw"], np.float32)
            for k in range(31):
                vp.add(p + f"conv_dw{k}", fm(dw[k]))
            vp.add(p + "conv_dw_b", fm(inp[p + "conv_dw_b"]))
            vp.add(p + "conv_ln_g", fm(inp[p + "conv_ln_g"]))
            vp.add(p + "conv_ln_b", fm(inp[p + "conv_ln_b"]))
            vp.add(p + "conv_b_o", fm(inp[p + "conv_b_o"]))
    return vp


class K:
    def __init__(self, T, vp_off, nvec, phases, debug_out=None):
        self.T = T
        self.vp_off = vp_off
        self.nvec = nvec
        self.phases = phases
        nc = bass.Bass("TRN2", target_bir_lowering=False)
        self.nc = nc
        self.P = Prog()
        self.dram = {}
        self.dres = {}

    def din(self, name, shape, dtype=F32):
        t = self.nc.dram_tensor(name, list(shape), dtype, kind="ExternalInput").ap()
        self.dram[name] = t
        return t

    def dout(self, name, shape, dtype=F32):
        t = self.nc.dram_tensor(name, list(shape), dtype, kind="ExternalOutput").ap()
        self.dram[name] = t
        return t

    def dscr(self, name, shape, dtype=F32):
        t = self.nc.dram_tensor(name, list(shape), dtype, kind="Internal").ap()
        self.dram[name] = t
        return t

    def dr(self, key):
        r = self.dres.get(key)
        if r is None:
            r = Res(str(key))
            self.dres[key] = r
        return r

    def vec(self, name, j=0, n=1):
        o, w = self.vp_off[name]
        return self.vecs[:, o + j:o + j + n]


def tiles_order(n):
    return list(range(n))


def build_program(T, inp_shapes, vp_off, nvec, layers, TT=256):
    k = K(T, vp_off, nvec, None)
    nc = k.nc
    P = k.P
    NT = T // TT

    xT_in = k.din("xT", [D, T])
    vecs_d = k.din("vecs", [128, nvec])
    pos_d = k.din("pos", [1, T], I32)
    cst_d = k.din("cst", [128, 160])
    mask_d = k.din("maskb", [128, 2048])
    glac_d = k.din("glac", [128, 384])
    W = {}
    for name, shp in inp_shapes.items():
        W[name] = k.din(name, shp)
    outT = k.dout("outT", [D, T])
    nph = sum(int(a) + int(b) for (_, a, b) in layers)
    xs_d = [xT_in]
    for i in range(nph - 1):
        xs_d.append(k.dscr(f"xs{i}", [D, T]))
    xs_d.append(outT)

    WORDS = 53000
    import contextlib
    es = contextlib.ExitStack()
    with es:
        big_h = es.enter_context(nc.sbuf_tensor("big", [128, WORDS], F32))
        big = big_h[:, :]
        pbanks = []
        for i in range(8):
            ph = es.enter_context(nc.psum_tensor(f"ps{i}", [128, 512], F32))
            pbanks.append(ph[:, :])
        sems = {e: es.enter_context(nc.semaphore(f"s_{e}")) for e in ENGS}
        dsems = {}
        for e in ("sp", "pool", "act"):
            for i in range(NDMASEM):
                dsems[(e, i)] = es.enter_context(nc.semaphore(f"d_{e}{i}"))
        M = Mem(P, big, WORDS)
        pres = [Res(f"psum{i}") for i in range(8)]

        vecs, vr = M.alloc("vecs", [nvec], F32)
        k.vecs = vecs
        vr = vr[0]
        P.dma("sp", vecs, vecs_d[:, :], wr=[vr])
        ones1024, r_o = M.alloc("ones1024", [128], BF16)
        r_ones = r_o[0]
        P.op("dve", "memset", ones1024, 1.0 / 1024.0, wr=[r_ones])
        ones256, _r = M.alloc("ones256", [128], BF16)
        P.op("dve", "memset", ones256, 1.0 / 256.0, wr=[r_ones])
        ones128, _r = M.alloc("ones128", [128], BF16)
        P.op("dve", "memset", ones128, 1.0 / 128.0, wr=[r_ones])
        ones1, _r = M.alloc("ones1", [128], BF16)
        P.op("dve", "memset", ones1, 1.0, wr=[r_ones])
        cst, r_c = M.alloc("cst", [160], F32)
        r_cst = r_c[0]
        P.dma("sp", cst, cst_d[:, :], wr=[r_cst])
        ident_b, _r = M.alloc("ident", [128], BF16)
        P.op("dve", "tensor_copy", ident_b, cst[:, 0:128], rd=[r_cst], wr=[r_ones])
        consts = dict(ones1024=ones1024, ones256=ones256, ones128=ones128, ones1=ones1,
                      ident=ident_b, cst=cst, r_ones=r_ones, r_cst=r_cst, vr=vr)
        base_mark = M.mark()

        ctx = dict(k=k, nc=nc, P=P, M=M, pbanks=pbanks, pres=pres, T=T, TT=TT, NT=NT,
                   consts=consts, W=W, pos_d=pos_d, mask_d=mask_d, glac_d=glac_d)

        xi = 0
        for (l, do_mixer, do_ffn) in layers:
            kind = layer_kind(l)
            if do_mixer:
                M.release(base_mark)
                if kind == 0:
                    phase_mla(ctx, l, xs_d[xi], xs_d[xi + 1])
                elif kind == 1:
                    phase_gla(ctx, l, xs_d[xi], xs_d[xi + 1])
                else:
                    phase_conv(ctx, l, xs_d[xi], xs_d[xi + 1])
                xi += 1
            if do_ffn:
                M.release(base_mark)
                phase_ffn(ctx, l, xs_d[xi], xs_d[xi + 1])
                xi += 1
        assert xs_d[xi] is outT or True
        final_src = xs_d[xi]
        fin_deps = [k.dr(("x", id(final_src), i)) for i in range(NT)]
        P.op("sp", "nop", rd=fin_deps)
        with nc.Block() as block:
            P.emit(nc, block, sems, dsems)
    return nc, final_src


def ln_tail_alloc(ctx):
    M, TT = ctx["M"], ctx["TT"]
    t = {}
    t["r"], t["r_r"] = M.alloc("r", [DC, TT], F32, nres=DC)
    t["rb"], t["rb_r"] = M.alloc("rb", [DC, TT], BF16, nres=DC)
    t["rsq"], t["rsq_r"] = M.alloc("rsq", [DC, TT], BF16, nres=DC)
    t["mean"], t["mean_r"] = M.alloc("mean", [TT], F32)
    t["m2"], t["m2_r"] = M.alloc("m2", [TT], F32)
    t["A"], t["A_r"] = M.alloc("A", [TT], F32)
    t["B"], t["B_r"] = M.alloc("B", [TT], F32)
    return t


def ln_core(ctx, t, g_name, b_name, final_fn=None):
    k, P = ctx["k"], ctx["P"]
    c = ctx["consts"]
    r, rb, rsq = t["r"], t["rb"], t["rsq"]
    (pm, pm_r), (pe2, pe2_r) = t["ps_stat"]
    for oc in range(DC):
        P.op("pe", "matmul", pm, c["ones1024"], rb[:, oc, :], start=(oc == 0), stop=(oc == DC - 1),
             rd=[t["rb_r"][oc], c["r_ones"]], wr=[pm_r])
    for oc in range(DC):
        P.op("pe", "matmul", pe2, c["ones1024"], rsq[:, oc, :], start=(oc == 0), stop=(oc == DC - 1),
             rd=[t["rsq_r"][oc], c["r_ones"]], wr=[pe2_r])
    mean, m2, A, B = t["mean"], t["m2"], t["A"], t["B"]
    P.op("act", "activation", mean, pm, AF.Copy, rd=[pm_r], wr=t["mean_r"])
    P.op("pool", "tensor_tensor", out=m2, in0=mean, in1=mean, op=ALU.mult, rd=t["mean_r"], wr=t["m2_r"])
    P.op("dve", "tensor_tensor", out=A, in0=pe2, in1=m2, op=ALU.subtract, rd=[pe2_r] + t["m2_r"], wr=t["A_r"])
    P.op("dve", "tensor_scalar_add", A, A, EPS, rd=t["A_r"], wr=t["A_r"])
    P.op("act", "activation", A, A, AF.Ln, rd=t["A_r"], wr=t["A_r"])
    P.op("act", "activation", A, A, AF.Exp, scale=-0.5, rd=t["A_r"], wr=t["A_r"])
    P.op("dve", "scalar_tensor_tensor", out=B, in0=mean, scalar=-1.0, in1=A, op0=ALU.mult, op1=ALU.mult,
         rd=t["mean_r"] + t["A_r"], wr=t["B_r"])
    for oc in range(DC):
        ro = r[:, oc, :]
        P.op("dve", "tensor_tensor", out=ro, in0=ro, in1=A, op=ALU.mult,
             rd=[t["r_r"][oc]] + t["A_r"], wr=[t["r_r"][oc]])
        P.op("pool", "tensor_tensor", out=ro, in0=ro, in1=B, op=ALU.add,
             rd=[t["r_r"][oc]] + t["B_r"], wr=[t["r_r"][oc]])
        gv, bv = k.vec(g_name, oc), k.vec(b_name, oc)
        P.op("act", "activation", ro, ro, AF.Identity, bias=bv, scale=gv,
             rd=[t["r_r"][oc], c["vr"]], wr=[t["r_r"][oc]])
        if final_fn is not None:
            final_fn(oc, ro, t["r_r"][oc])


def ln_tail(ctx, t, ti, y_chunk_fn, xs, xs_r, xoff, g_name, b_name, dst, ps_y, ps_stat, bias_name=None):
    k, P, TT = ctx["k"], ctx["P"], ctx["TT"]
    c = ctx["consts"]
    r, rb, rsq = t["r"], t["rb"], t["rsq"]
    t["ps_stat"] = ps_stat
    for oc in range(DC):
        ps, ps_r = ps_y[oc % len(ps_y)]
        y_chunk_fn(oc, ps, ps_r)
        xin = xs[:, oc, xoff:xoff + TT]
        ro = r[:, oc, :]
        if bias_name is None:
            P.op("dve", "scalar_tensor_tensor", out=ro, in0=xin, scalar=ALPHA, in1=ps, op0=ALU.mult, op1=ALU.add,
                 rd=[xs_r, ps_r], wr=[t["r_r"][oc]])
        else:
            bv = k.vec(bias_name, oc)
            P.op("act", "activation", ro, ps, AF.Identity, bias=bv, scale=1.0,
                 rd=[ps_r, c["vr"]], wr=[t["r_r"][oc]])
            P.op("dve", "scalar_tensor_tensor", out=ro, in0=xin, scalar=ALPHA, in1=ro, op0=ALU.mult, op1=ALU.add,
                 rd=[xs_r, t["r_r"][oc]], wr=[t["r_r"][oc]])
        P.op("pool", "tensor_copy", rb[:, oc, :], ro, rd=[t["r_r"][oc]], wr=[t["rb_r"][oc]])
        P.op("act", "activation", rsq[:, oc, :], ro, AF.Square, rd=[t["r_r"][oc]], wr=[t["rsq_r"][oc]])
    ln_core(ctx, t, g_name, b_name)
    dview = dst.rearrange("(c p) t -> p c t", p=128)[:, :, ti * TT:(ti + 1) * TT]
    P.dma("sp", dview, r, rd=t["r_r"], wr=[k.dr(("x", id(dst), ti))])


def load_w(ctx, dst_ap, src_ap, res):
    P = ctx["P"]
    P.dma("pool", dst_ap, src_ap, wr=res)


def phase_ffn(ctx, l, src, dst):
    k, P, M, T, TT, NT = ctx["k"], ctx["P"], ctx["M"], ctx["T"], ctx["TT"], ctx["NT"]
    W, c, pb, pr = ctx["W"], ctx["consts"], ctx["pbanks"], ctx["pres"]
    p = f"l{l}_"
    w_in_d, w_out_d = W[p + "ffn_w_in"], W[p + "ffn_w_out"]
    w_in, w_in_r = M.alloc("ffn_w_in", [DC, 2 * FF], BF16, nres=DC * 4)
    w_out, w_out_r = M.alloc("ffn_w_out", [FC, D], BF16, nres=FC)
    for kc in range(DC):
        for q in range(4):
            load_w(ctx, w_in[:, kc, q * 1408:(q + 1) * 1408], w_in_d[kc * 128:(kc + 1) * 128, q * 1408:(q + 1) * 1408],
                   [w_in_r[kc * 4 + q]])
    for fc in range(FC):
        load_w(ctx, w_out[:, fc, :], w_out_d[fc * 128:(fc + 1) * 128, :], [w_out_r[fc]])
    NB = 2
    xs_l, xb_l = [], []
    for i in range(NB):
        xs_l.append(M.alloc(f"xs{i}", [DC, TT + 2], F32))
        xb_l.append(M.alloc(f"xb{i}", [DC, TT + 2], BF16))
    tg_l = [M.alloc(f"tg{i}", [TT], F32) for i in range(2)]
    tv_l = [M.alloc(f"tv{i}", [TT], F32) for i in range(2)]
    gT, gT_r = M.alloc("gT", [FC, TT], BF16, nres=FC)
    t = ln_tail_alloc(ctx)
    srcv = src.rearrange("(c p) t -> p c t", p=128)

    def load_x(ti, slot):
        xs, xs_r = xs_l[slot]
        xb, xb_r = xb_l[slot]
        t0 = ti * TT
        rdr = [k.dr(("x", id(src), ti))]
        if ti == 0:
            P.op("pool", "memset", xs[:, :, 0:2], 0.0, wr=xs_r)
            P.op("pool", "memset", xb[:, :, 0:2], 0.0, wr=xb_r)
            P.dma("sp", xs[:, :, 2:TT + 2], srcv[:, :, 0:TT], rd=rdr, wr=xs_r)
            P.dma("pool", xb[:, :, 2:TT + 2], srcv[:, :, 0:TT], rd=rdr, wr=xb_r)
        else:
            rdr.append(k.dr(("x", id(src), ti - 1)))
            P.dma("sp", xs[:, :, :], srcv[:, :, t0 - 2:t0 + TT], rd=rdr, wr=xs_r)
            P.dma("pool", xb[:, :, :], srcv[:, :, t0 - 2:t0 + TT], rd=rdr, wr=xb_r)

    def fin(gc, tg, tg_r, tv, tv_r):
        P.op("act", "activation", tg, tg, AF.Silu, rd=tg_r, wr=tg_r)
        P.op("pool", "tensor_tensor", out=gT[:, gc, :], in0=tg, in1=tv, op=ALU.mult,
             rd=tg_r + tv_r, wr=[gT_r[gc]])

    load_x(0, 0)
    for ti in range(NT):
        slot = ti % NB
        if ti + 1 < NT:
            load_x(ti + 1, (ti + 1) % NB)
        xs, xs_r = xs_l[slot]
        xb, xb_r = xb_l[slot]
        pend = None
        for gc in range(FC):
            bg, bv_ = (gc % 2) * 2, (gc % 2) * 2 + 1
            psg, psv = pb[bg][:, 0:TT + 2], pb[bv_][:, 0:TT + 2]
            for kc in range(DC):
                P.op("pe", "matmul", psg, w_in[:, kc, gc * 128:(gc + 1) * 128], xb[:, kc, :],
                     start=(kc == 0), stop=(kc == DC - 1),
                     rd=[w_in_r[kc * 4 + (gc * 128) // 1408], w_in_r[kc * 4 + (gc * 128 + 127) // 1408]] + xb_r,
                     wr=[pr[bg]])
            for kc in range(DC):
                c0 = FF + gc * 128
                P.op("pe", "matmul", psv, w_in[:, kc, c0:c0 + 128], xb[:, kc, :],
                     start=(kc == 0), stop=(kc == DC - 1),
                     rd=[w_in_r[kc * 4 + c0 // 1408], w_in_r[kc * 4 + (c0 + 127) // 1408]] + xb_r, wr=[pr[bv_]])
            tg, tg_r = tg_l[gc % 2]
            tv, tv_r = tv_l[gc % 2]
            for (ps, psr, tt, ttr, col) in ((psg, pr[bg], tg, tg_r, gc), (psv, pr[bv_], tv, tv_r, FC + gc)):
                w0, w1, w2 = (k.vec(p + f"ffn_conv{j}", col) for j in range(3))
                cb = k.vec(p + "ffn_conv_b", col)
                P.op("act", "activation", tt, ps[:, 0:TT], AF.Identity, bias=cb, scale=w0,
                     rd=[psr, c["vr"]], wr=ttr)
                P.op("dve", "scalar_tensor_tensor", out=tt, in0=ps[:, 1:TT + 1], scalar=w1, in1=tt,
                     op0=ALU.mult, op1=ALU.add, rd=[psr, c["vr"]] + ttr, wr=ttr)
                P.op("dve", "scalar_tensor_tensor", out=tt, in0=ps[:, 2:TT + 2], scalar=w2, in1=tt,
                     op0=ALU.mult, op1=ALU.add, rd=[psr, c["vr"]] + ttr, wr=ttr)
            if pend is not None:
                fin(*pend)
            pend = (gc, tg, tg_r, tv, tv_r)
        fin(*pend)

        def ychunk(oc, ps, ps_r):
            for fc in range(FC):
                P.op("pe", "matmul", ps, w_out[:, fc, oc * 128:(oc + 1) * 128], gT[:, fc, :],
                     start=(fc == 0), stop=(fc == FC - 1), rd=[w_out_r[fc], gT_r[fc]], wr=[ps_r])
        ps_y = [(pb[4][:, 0:TT], pr[4]), (pb[5][:, 0:TT], pr[5])]
        ps_stat = [(pb[6][:, 0:TT], pr[6]), (pb[7][:, 0:TT], pr[7])]
        ln_tail(ctx, t, ti, ychunk, xs, xs_r[0], 2, p + "ln2_g", p + "ln2_b", dst, ps_y, ps_stat)


def phase_mla(ctx, l, src, dst):
    k, P, M, T, TT = ctx["k"], ctx["P"], ctx["M"], ctx["T"], ctx["TT"]
    W, c, pb, pr = ctx["W"], ctx["consts"], ctx["pbanks"], ctx["pres"]
    p = f"l{l}_"
    TQ = 512
    NQ = T // TQ
    NKT = T // 128
    SC = float(192 ** -0.5)
    PI = float(np.pi)
    vr = c["vr"]
    w_in_d, w_uq_d, w_ukv_d, w_o_d = W[p + "mla_w_in"], W[p + "mla_w_uq"], W[p + "mla_w_ukv"], W[p + "mla_w_o"]
    w_in, w_in_r = M.alloc("m_w_in", [DC, 512], BF16)
    for kc in range(DC):
        load_w(ctx, w_in[:, kc, 0:448], w_in_d[kc * 128:(kc + 1) * 128, :], w_in_r)
    P.op("dve", "tensor_scalar_mul", w_in[:, :, 448:480], w_in[:, :, 416:448], -1.0, rd=w_in_r, wr=w_in_r)
    P.op("dve", "tensor_copy", w_in[:, :, 480:512], w_in[:, :, 384:416], rd=w_in_r, wr=w_in_r)
    w_uq, w_uq_r = M.alloc("m_w_uq", [2, 2048], BF16)
    for c2 in range(2):
        load_w(ctx, w_uq[:, c2, 0:1536], w_uq_d[c2 * 128:(c2 + 1) * 128, :], w_uq_r)
    for c2 in range(2):
        srcv_ = w_uq[:, c2, 0:1536].rearrange("p (h d) -> p h d", h=8)
        dstv_ = w_uq[:, c2, 1536:2048].rearrange("p (h d) -> p h d", h=8)
        P.op("dve", "tensor_scalar_mul", dstv_[:, :, 0:32], srcv_[:, :, 160:192], -1.0, rd=w_uq_r, wr=w_uq_r)
        P.op("dve", "tensor_copy", dstv_[:, :, 32:64], srcv_[:, :, 128:160], rd=w_uq_r, wr=w_uq_r)
    w_ukv, w_ukv_r = M.alloc("m_w_ukv", [2048], BF16)
    load_w(ctx, w_ukv[:, :], w_ukv_d[:, :], w_ukv_r)
    w_o, w_o_r = M.alloc("m_w_o", [8, D], BF16)
    for h in range(8):
        load_w(ctx, w_o[:, h, :], w_o_d[h * 128:(h + 1) * 128, :], w_o_r)
    maskb, maskb_r = M.alloc("m_mask", [4, 512], BF16)
    load_w(ctx, maskb[:, :, :], ctx["mask_d"].rearrange("p (a b) -> p a b", a=4), maskb_r)
    wukT, wukT_r = M.alloc("m_wukT", [8, 128], BF16)
    for h in range(8):
        bk = 6 + h % 2
        pst = pb[bk].bitcast(BF16)[:, 0:128]
        P.op("pe", "transpose", pst, w_ukv[:, h * 256:h * 256 + 128], c["ident"], rd=w_ukv_r + [c["r_ones"]], wr=[pr[bk]])
        P.op("dve", "tensor_copy", wukT[:, h, :], pst, rd=[pr[bk]], wr=wukT_r)
    negpi, negpi_r = M.alloc("m_negpi", [2], F32)
    P.op("dve", "memset", negpi, -PI, wr=negpi_r)
    latT, latT_r = M.alloc("m_latT", [T], BF16, nres=NQ)
    krT, krT_r = M.alloc("m_krT", [T], BF16, nres=NQ)
    lat_tok, lat_tok_r = M.alloc("m_lat_tok", [NKT, 128], BF16, nres=NQ)
    xb_l = [M.alloc(f"m_xb{i}", [DC, TQ], BF16) for i in range(1)]
    xs_l = [M.alloc(f"m_xs{i}", [DC, TT], F32) for i in range(TQ // TT)]
    cqf, cqf_r = M.alloc("m_cqf", [2, TQ], F32, nres=2)
    sq, sq_r = M.alloc("m_sq", [2, TQ], BF16, nres=2)
    cqn, cqn_r = M.alloc("m_cqn", [2, TQ], BF16, nres=2)
    rstd, rstd_r = M.alloc("m_rstd", [TQ], F32)
    ckvf, ckvf_r = cqf[:, 0, :], [cqf_r[0]]
    sqkv, sqkv_r = M.alloc("m_sqkv", [TQ], BF16)
    posi, posi_r = M.alloc("m_posi", [TQ], I32)
    posf, posf_r = M.alloc("m_posf", [TQ], F32)
    a2, a2_r = M.alloc("m_a2", [TQ], F32)
    ang, ang_r = posf, posf_r
    cosf, cosf_r = M.alloc("m_cos", [TQ], F32)
    sinf, sinf_r = M.alloc("m_sin", [TQ], F32)
    coss, coss_r = M.alloc("m_coss", [TQ], F32)
    sins, sins_r = M.alloc("m_sins", [TQ], F32)
    tm1, tm1_r = M.alloc("m_tm1", [TQ], F32)
    tm2, tm2_r = M.alloc("m_tm2", [TQ], F32)
    tmq_l = [(tm1, tm1_r), (tm2, tm2_r), M.alloc("m_tm3", [TQ], F32), M.alloc("m_tm4", [TQ], F32)]
    a0, a0_r = tmq_l[2]
    a1, a1_r = tmq_l[3]
    qn_l = [M.alloc(f"m_qn{i}", [TQ], BF16) for i in range(2)]
    qt, qt_r = M.alloc("m_qt", [8, TQ], BF16, nres=8)
    qr, qr_r = M.alloc("m_qr", [8, TQ], BF16, nres=8)
    PT_l = [M.alloc(f"m_PT{i}", [TQ], BF16) for i in range(3)]
    rL, rL_r = M.alloc("m_rL", [TQ], F32)
    On_l = [M.alloc(f"m_On{i}", [TQ], BF16) for i in range(2)]
    oT, oT_r = M.alloc("m_oT", [8, TQ], BF16, nres=8)
    t = ln_tail_alloc(ctx)
    srcv = src.rearrange("(c p) t -> p c t", p=128)
    invf = c["cst"][0:64, 128:129]

    def load_xb(qb):
        xb, xb_r = xb_l[0]
        rdr = [k.dr(("x", id(src), qb * 2)), k.dr(("x", id(src), qb * 2 + 1))]
        P.dma("pool", xb[:, :, :], srcv[:, :, qb * TQ:(qb + 1) * TQ], rd=rdr, wr=xb_r)

    def rstd_from(ps, psr):
        P.op("dve", "tensor_scalar_add", rstd, ps, EPS, rd=[psr], wr=rstd_r)
        P.op("act", "activation", rstd, rstd, AF.Ln, rd=rstd_r, wr=rstd_r)
        P.op("act", "activation", rstd, rstd, AF.Exp, scale=-0.5, rd=rstd_r, wr=rstd_r)

    def proj(ps, psr, wcols, xb, xb_r, m=128):
        for kc in range(DC):
            P.op("pe", "matmul", ps[0:m, :], w_in[:, kc, wcols:wcols + m], xb[:, kc, :],
                 start=(kc == 0), stop=(kc == DC - 1), rd=w_in_r + xb_r, wr=[psr])

    load_xb(0)
    for qb in range(NQ):
        xb, xb_r = xb_l[0]
        for j in range(TQ // TT):
            tix = qb * (TQ // TT) + j
            xsj, xsj_r = xs_l[j]
            P.dma("sp", xsj[:, :, :], srcv[:, :, tix * TT:(tix + 1) * TT], rd=[k.dr(("x", id(src), tix))], wr=xsj_r)
        blk = slice(qb * TQ, (qb + 1) * TQ)
        p6, p7 = pb[6], pb[7]
        for c2 in range(2):
            ps, psr = (p6, pr[6]) if c2 == 0 else (p7, pr[7])
            proj(ps, psr, c2 * 128, xb, xb_r)
            P.op("act", "activation", cqf[:, c2, :], ps, AF.Copy, rd=[psr], wr=[cqf_r[c2]])
            P.op("act", "activation", sq[:, c2, :], cqf[:, c2, :], AF.Square, rd=[cqf_r[c2]], wr=[sq_r[c2]])
        for c2 in range(2):
            P.op("pe", "matmul", p6, c["ones256"], sq[:, c2, :], start=(c2 == 0), stop=(c2 == 1),
                 rd=[sq_r[c2], c["r_ones"]], wr=[pr[6]])
        rstd_from(p6, pr[6])
        for c2 in range(2):
            P.op("dve", "scalar_tensor_tensor", out=cqn[:, c2, :], in0=cqf[:, c2, :], scalar=k.vec(p + "mla_q_norm", c2),
                 in1=rstd, op0=ALU.mult, op1=ALU.mult, rd=[cqf_r[c2], vr] + rstd_r, wr=[cqn_r[c2]])
        proj(p7, pr[7], 256, xb, xb_r)
        P.op("act", "activation", ckvf, p7, AF.Copy, rd=[pr[7]], wr=ckvf_r)
        P.op("act", "activation", sqkv, ckvf, AF.Square, rd=ckvf_r, wr=sqkv_r)
        P.op("pe", "matmul", p6, c["ones128"], sqkv, start=True, stop=True, rd=sqkv_r + [c["r_ones"]], wr=[pr[6]])
        rstd_from(p6, pr[6])
        P.op("dve", "scalar_tensor_tensor", out=latT[:, blk], in0=ckvf, scalar=k.vec(p + "mla_kv_norm", 0),
             in1=rstd, op0=ALU.mult, op1=ALU.mult, rd=ckvf_r + [vr] + rstd_r, wr=[latT_r[qb]])
        P.dma("sp", posi[0:64, :], ctx["pos_d"][0:1, blk].partition_broadcast(64), wr=posi_r)
        P.op("dve", "tensor_copy", posf[0:64, :], posi[0:64, :], rd=posi_r, wr=posf_r)
        P.op("dve", "tensor_scalar_mul", ang[0:64, :], posf[0:64, :], invf, rd=posf_r + [c["r_cst"]], wr=ang_r)
        MAGIC = 12582912.0
        C1 = 6.28125
        C2 = float(2.0 * np.pi - 6.28125)
        for (dstt, dstr, off) in ((sinf, sinf_r, 0.0), (cosf, cosf_r, 0.5 * PI)):
            if off != 0.0:
                P.op("dve", "tensor_scalar_add", a0[0:64, :], ang[0:64, :], off, rd=ang_r, wr=a0_r)
                aa, aa_r = a0, a0_r
            else:
                aa, aa_r = ang, ang_r
            P.op("dve", "tensor_scalar", out=a1[0:64, :], in0=aa[0:64, :], scalar1=float(1.0 / (2.0 * np.pi)),
                 scalar2=MAGIC, op0=ALU.mult, op1=ALU.add, rd=aa_r, wr=a1_r)
            P.op("dve", "tensor_scalar_add", a1[0:64, :], a1[0:64, :], -MAGIC, rd=a1_r, wr=a1_r)
            P.op("dve", "scalar_tensor_tensor", out=a2[0:64, :], in0=a1[0:64, :], scalar=-C1, in1=aa[0:64, :],
                 op0=ALU.mult, op1=ALU.add, rd=a1_r + aa_r, wr=a2_r)
            P.op("dve", "scalar_tensor_tensor", out=a2[0:64, :], in0=a1[0:64, :], scalar=-C2, in1=a2[0:64, :],
                 op0=ALU.mult, op1=ALU.add, rd=a1_r + a2_r, wr=a2_r)
            P.op("dve", "tensor_scalar", out=a2[0:64, :], in0=a2[0:64, :], scalar1=-3.1415925, scalar2=3.1415925,
                 op0=ALU.max, op1=ALU.min, rd=a2_r, wr=a2_r)
            P.op("act", "activation", dstt[0:64, :], a2[0:64, :], AF.Sin, rd=a2_r, wr=dstr)
        P.op("pool", "tensor_scalar_mul", coss[0:64, :], cosf[0:64, :], SC, rd=cosf_r, wr=coss_r)
        P.op("pool", "tensor_scalar_mul", sins[0:64, :], sinf[0:64, :], SC, rd=sinf_r, wr=sins_r)
        proj(p6, pr[6], 384, xb, xb_r, m=64)
        proj(p7, pr[7], 448, xb, xb_r, m=64)
        if qb + 1 < NQ:
            load_xb(qb + 1)
        P.op("dve", "tensor_tensor", out=tm1[0:64, :], in0=p6[0:64, :], in1=cosf[0:64, :], op=ALU.mult,
             rd=[pr[6]] + cosf_r, wr=tm1_r)
        P.op("dve", "tensor_tensor", out=tm2[0:64, :], in0=p7[0:64, :], in1=sinf[0:64, :], op=ALU.mult,
             rd=[pr[7]] + sinf_r, wr=tm2_r)
        P.op("pool", "tensor_tensor", out=krT[0:64, blk], in0=tm1[0:64, :], in1=tm2[0:64, :], op=ALU.add,
             rd=tm1_r + tm2_r, wr=[krT_r[qb]])
        for j in range(4):
            bk = 6 + j % 2
            pst = pb[bk].bitcast(BF16)[:, 0:128]
            P.op("pe", "transpose", pst, latT[:, qb * TQ + j * 128:qb * TQ + (j + 1) * 128], c["ident"],
                 rd=[latT_r[qb], c["r_ones"]], wr=[pr[bk]])
            P.op("dve", "tensor_copy", lat_tok[:, qb * 4 + j, :], pst, rd=[pr[bk]], wr=[lat_tok_r[qb]])
        for h in range(8):
            qn, qn_r = qn_l[h % 2]
            b0 = 4 * (h % 2)
            pA, pB, pC, pD = pb[b0], pb[b0 + 1], pb[b0 + 2], pb[b0 + 3]
            rA, rB, rC, rD = pr[b0], pr[b0 + 1], pr[b0 + 2], pr[b0 + 3]
            for c2 in range(2):
                P.op("pe", "matmul", pA, w_uq[:, c2, h * 192:h * 192 + 128], cqn[:, c2, :], start=(c2 == 0),
                     stop=(c2 == 1), rd=w_uq_r + [cqn_r[c2]], wr=[rA])
            P.op("act", "activation", qn, pA, AF.Copy, rd=[rA], wr=qn_r)
            for c2 in range(2):
                P.op("pe", "matmul", pC[0:64, :], w_uq[:, c2, h * 192 + 128:h * 192 + 192], cqn[:, c2, :],
                     start=(c2 == 0), stop=(c2 == 1), rd=w_uq_r + [cqn_r[c2]], wr=[rC])
            for c2 in range(2):
                P.op("pe", "matmul", pD[0:64, :], w_uq[:, c2, 1536 + h * 64:1536 + (h + 1) * 64], cqn[:, c2, :],
                     start=(c2 == 0), stop=(c2 == 1), rd=w_uq_r + [cqn_r[c2]], wr=[rD])
            P.op("pe", "matmul", pB, wukT[:, h, :], qn, start=True, stop=True, rd=wukT_r + qn_r, wr=[rB])
            P.op("act", "activation", qt[:, h, :], pB, AF.Identity, scale=SC, rd=[rB], wr=[qt_r[h]])
            tA, tA_r = tmq_l[(2 * h) % 4]
            tB, tB_r = tmq_l[(2 * h + 1) % 4]
            P.op("dve", "tensor_tensor", out=tA[0:64, :], in0=pC[0:64, :], in1=coss[0:64, :], op=ALU.mult,
                 rd=[rC] + coss_r, wr=tA_r)
            P.op("dve", "tensor_tensor", out=tB[0:64, :], in0=pD[0:64, :], in1=sins[0:64, :], op=ALU.mult,
                 rd=[rD] + sins_r, wr=tB_r)
            P.op("pool", "tensor_tensor", out=qr[0:64, h, :], in0=tA[0:64, :], in1=tB[0:64, :], op=ALU.add,
                 rd=tA_r + tB_r, wr=[qr_r[h]])
        nkt = 4 * (qb + 1)
        iters = [(h, kt) for h in range(8) for kt in range(nkt)]

        def emit_S(n):
            h, kt = iters[n]
            Sb = n % 2
            S = pb[Sb]
            PT, PT_r = PT_l[n % 3]
            diag = kt >= 4 * qb
            ks = slice(kt * 128, (kt + 1) * 128)
            P.op("pe", "matmul", S, latT[:, ks], qt[:, h, :], start=True, stop=False,
                 rd=[latT_r[kt // 4], qt_r[h]], wr=[pr[Sb]])
            P.op("pe", "matmul", S, krT[0:64, ks], qr[0:64, h, :], start=False, stop=(not diag),
                 rd=[krT_r[kt // 4], qr_r[h]], wr=[pr[Sb]])
            if diag:
                P.op("pe", "matmul", S, c["ident"], maskb[:, kt - 4 * qb, :], start=False, stop=True,
                     rd=maskb_r + [c["r_ones"]], wr=[pr[Sb]])
            P.op("act", "activation", PT, S, AF.Exp, rd=[pr[Sb]], wr=PT_r)

        def emit_OV(n):
            h, kt = iters[n]
            Ob, Lb = 2 + h % 2, 4 + h % 2
            O, L = pb[Ob], pb[Lb]
            PT, PT_r = PT_l[n % 3]
            P.op("pe", "matmul", O, lat_tok[:, kt, :], PT, start=(kt == 0), stop=(kt == nkt - 1),
                 rd=[lat_tok_r[kt // 4]] + PT_r, wr=[pr[Ob]])
            P.op("pe", "matmul", L, c["ones1"], PT, start=(kt == 0), stop=(kt == nkt - 1),
                 rd=[c["r_ones"]] + PT_r, wr=[pr[Lb]])
            if kt == nkt - 1:
                On, On_r = On_l[h % 2]
                P.op("dve", "reciprocal", rL, L, rd=[pr[Lb]], wr=rL_r)
                P.op("dve", "tensor_tensor", out=On, in0=O, in1=rL, op=ALU.mult, rd=[pr[Ob]] + rL_r, wr=On_r)
                ob = 6 + h % 2
                P.op("pe", "matmul", pb[ob], w_ukv[:, h * 256 + 128:h * 256 + 256], On, start=True, stop=True,
                     rd=w_ukv_r + On_r, wr=[pr[ob]])
                P.op("dve", "tensor_copy", oT[:, h, :], pb[ob], rd=[pr[ob]], wr=[oT_r[h]])

        emit_S(0)
        for n in range(len(iters)):
            if n + 1 < len(iters):
                emit_S(n + 1)
            emit_OV(n)
        for j in range(TQ // TT):
            def ychunk(oc, ps, ps_r, j=j):
                for h in range(8):
                    P.op("pe", "matmul", ps, w_o[:, h, oc * 128:(oc + 1) * 128], oT[:, h, j * TT:(j + 1) * TT],
                         start=(h == 0), stop=(h == 7), rd=w_o_r + [oT_r[h]], wr=[ps_r])
            ps_y = [(pb[6][:, 0:TT], pr[6]), (pb[7][:, 0:TT], pr[7])]
            ps_stat = [(pb[0][:, 0:TT], pr[0]), (pb[1][:, 0:TT], pr[1])]
            tix = qb * (TQ // TT) + j
            xsj, xsj_r = xs_l[j]
            ln_tail(ctx, t, tix, ychunk, xsj, xsj_r[0], 0, p + "ln1_g", p + "ln1_b", dst, ps_y, ps_stat)


def phase_gla(ctx, l, src, dst):
    k, P, M, T, TT, NT = ctx["k"], ctx["P"], ctx["M"], ctx["T"], ctx["TT"], ctx["NT"]
    W, c, pb, pr = ctx["W"], ctx["consts"], ctx["pbanks"], ctx["pres"]
    p = f"l{l}_"
    vr = c["vr"]
    w_in_d, w_a2_d, b_a_d, w_o_d = W[p + "gla_w_in"], W[p + "gla_w_a2"], W[p + "gla_b_a"], W[p + "gla_w_o"]
    NW = 3088
    w_in, w_in_r = M.alloc("g_w_in", [DC, NW], BF16, nres=DC)
    for kc in range(DC):
        load_w(ctx, w_in[:, kc, :], w_in_d[kc * 128:(kc + 1) * 128, :], [w_in_r[kc]])
    w_o, w_o_r = M.alloc("g_w_o", [DC, D], BF16, nres=DC)
    for kc in range(DC):
        load_w(ctx, w_o[:, kc, :], w_o_d[kc * 128:(kc + 1) * 128, :], [w_o_r[kc]])
    w_a2b, w_a2b_r = M.alloc("g_w_a2b", [512], BF16)
    load_w(ctx, w_a2b[0:16, :], w_a2_d[:, :], w_a2b_r)
    load_w(ctx, w_a2b[16:17, :], b_a_d[:, :], w_a2b_r)
    gc3, gc3_r = M.alloc("g_c3", [3, 128], BF16)
    load_w(ctx, gc3[:, :, :], ctx["glac_d"].rearrange("p (a b) -> p a b", a=3), gc3_r)
    triU, triR = gc3[:, 0, :], gc3[:, 1, :]
    maskA, maskA_r = M.alloc("g_maskA", [128], F32)
    P.dma("sp", maskA, ctx["glac_d"][:, 256:384], wr=maskA_r)
    onec, onec_r = M.alloc("g_onec", [2], F32)
    P.op("dve", "memset", onec, 1.0, wr=onec_r)
    alr1, alr1_r = M.alloc("g_alr1", [128], BF16)
    P.op("dve", "memset", alr1[0:32, :], 1.0, wr=alr1_r)
    Sf, Sf_r = M.alloc("g_Sf", [4, 256], F32, nres=4)
    Sb, Sb_r = M.alloc("g_Sb", [4, 256], BF16, nres=4)
    for h in range(4):
        P.op("dve", "memset", Sf[:, h, :], 0.0, wr=[Sf_r[h]])
        P.op("dve", "memset", Sb[:, h, :], 0.0, wr=[Sb_r[h]])
    xs_l = [M.alloc(f"g_xs{i}", [DC, TT], F32) for i in range(2)]
    xb_l = [M.alloc(f"g_xb{i}", [DC, TT], BF16) for i in range(2)]
    Lf, Lf_r = M.alloc("g_Lf", [512], F32)
    Lb, Lb_r = M.alloc("g_Lb", [512], BF16)
    Ef, Ef_r = M.alloc("g_Ef", [4, 128], F32)
    Emf, Emf_r = M.alloc("g_Emf", [4, 128], F32)
    Erf, Erf_r = M.alloc("g_Erf", [512], F32)
    qtl, qtl_r = M.alloc("g_qtl", [4, 128], BF16)
    ktl, ktl_r = M.alloc("g_ktl", [4, 128], BF16)
    kdec, kdec_r = M.alloc("g_kdec", [512], BF16)
    v_bf, v_bf_r = M.alloc("g_vbf", [1024], BF16, nres=2)
    attn, attn_r = M.alloc("g_attn", [4, 128], BF16, nres=4)
    osq, osq_r = M.alloc("g_osq", [2, 4, 128], BF16, nres=2)
    rso, rso_r = M.alloc("g_rso", [4, 128], F32)
    sil, sil_r = M.alloc("g_sil", [8, 128], F32, nres=2)
    tmp_l = [M.alloc(f"g_tmp{i}", [128], F32) for i in range(2)]
    gT, gT_r = M.alloc("g_gT", [DC, TT], BF16, nres=DC)
    t = ln_tail_alloc(ctx)
    srcv = src.rearrange("(c p) t -> p c t", p=128)
    QS = float(128 ** -0.5)

    def load_x(ti, slot):
        xs, xs_r = xs_l[slot]
        xb, xb_r = xb_l[slot]
        rdr = [k.dr(("x", id(src), ti))]
        P.dma("sp", xs[:, :, :], srcv[:, :, ti * TT:(ti + 1) * TT], rd=rdr, wr=xs_r)
        P.dma("pool", xb[:, :, :], srcv[:, :, ti * TT:(ti + 1) * TT], rd=rdr, wr=xb_r)

    def fm_proj(ps, psr, col0, m, xb, xb_r, tsl):
        for kc in range(DC):
            P.op("pe", "matmul", ps, w_in[:, kc, col0:col0 + m], xb[:, kc, tsl], start=(kc == 0), stop=(kc == DC - 1),
                 rd=[w_in_r[kc]] + xb_r, wr=[psr])

    def tm_proj(ps, psr, col0, n, xb, xb_r, tsl):
        for kc in range(DC):
            P.op("pe", "matmul", ps, xb[:, kc, tsl], w_in[:, kc, col0:col0 + n], start=(kc == 0), stop=(kc == DC - 1),
                 rd=[w_in_r[kc]] + xb_r, wr=[psr])

    def hs(h):
        return slice(h * 128, (h + 1) * 128)

    load_x(0, 0)
    for ti in range(NT):
        slot = ti % 2
        if ti + 1 < NT:
            load_x(ti + 1, (ti + 1) % 2)
        xs, xs_r = xs_l[slot]
        xb, xb_r = xb_l[slot]
        for sub in range(TT // 128):
            tsl = slice(sub * 128, (sub + 1) * 128)
            for h in range(4):
                fm_proj(pb[0][:, hs(h)], pr[0], h * 128, 128, xb, xb_r, tsl)
            for h in range(4):
                fm_proj(pb[1][:, hs(h)], pr[1], 512 + h * 128, 128, xb, xb_r, tsl)
            fm_proj(pb[7][0:16, 0:128], pr[7], 2048, 16, xb, xb_r, tsl)
            tm_proj(pb[2], pr[2], 512, 512, xb, xb_r, tsl)
            tm_proj(pb[3], pr[3], 1024, 512, xb, xb_r, tsl)
            tm_proj(pb[4], pr[4], 1536, 512, xb, xb_r, tsl)
            P.op("act", "activation", alr1[0:16, :], pb[7][0:16, 0:128], AF.Copy, rd=[pr[7]], wr=alr1_r)
            P.op("pe", "matmul", pb[5], alr1[0:17, :], w_a2b[0:17, :], start=True, stop=True,
                 rd=alr1_r + w_a2b_r, wr=[pr[5]])
            P.op("act", "activation", Lf, pb[5], AF.Exp, scale=-1.0, rd=[pr[5]], wr=Lf_r)
            P.op("act", "activation", Lb, Lf, AF.Ln, bias=onec[:, 0:1], scale=1.0, rd=Lf_r + onec_r, wr=Lb_r)
            for h in range(4):
                P.op("pe", "matmul", pb[6][:, hs(h)], Lb[:, hs(h)], triU, start=True, stop=True,
                     rd=Lb_r + gc3_r, wr=[pr[6]])
            P.op("pe", "matmul", pb[7], triR, Lb, start=True, stop=True, rd=Lb_r + gc3_r, wr=[pr[7]])
            Ef2 = Ef.rearrange("p a b -> p (a b)")
            Emf2 = Emf.rearrange("p a b -> p (a b)")
            P.op("act", "activation", Ef2, pb[6], AF.Exp, rd=[pr[6]], wr=Ef_r)
            P.op("act", "activation", Emf2, pb[6], AF.Exp, scale=-1.0, rd=[pr[6]], wr=Emf_r)
            P.op("act", "activation", Erf, pb[7], AF.Exp, rd=[pr[7]], wr=Erf_r)
            P.op("dve", "scalar_tensor_tensor", out=qtl.rearrange("p a b -> p (a b)"), in0=pb[0], scalar=QS, in1=Ef2,
                 op0=ALU.mult, op1=ALU.mult, rd=[pr[0]] + Ef_r, wr=qtl_r)
            P.op("dve", "tensor_tensor", out=ktl.rearrange("p a b -> p (a b)"), in0=pb[1], in1=Emf2, op=ALU.mult,
                 rd=[pr[1]] + Emf_r, wr=ktl_r)
            P.op("dve", "tensor_tensor", out=kdec, in0=pb[2], in1=Erf, op=ALU.mult, rd=[pr[2]] + Erf_r, wr=kdec_r)
            P.op("act", "activation", v_bf[:, 0:512], pb[3], AF.Copy, rd=[pr[3]], wr=[v_bf_r[0]])
            P.op("dve", "tensor_copy", v_bf[:, 512:1024], pb[4], rd=[pr[4]], wr=[v_bf_r[1]])
            for h in range(4):
                P.op("pe", "matmul", pb[2][:, hs(h)], ktl[:, h, :], qtl[:, h, :], start=True, stop=True,
                     rd=ktl_r + qtl_r, wr=[pr[2]])
                P.op("dve", "tensor_tensor", out=attn[:, h, :], in0=pb[2][:, hs(h)], in1=maskA, op=ALU.mult,
                     rd=[pr[2]] + maskA_r, wr=[attn_r[h]])
            dsi = 0
            for h in range(4):
                vr_h = [v_bf_r[(h * 256) // 512]]
                for eh in range(2):
                    P.op("pe", "matmul", pb[3 + eh][:, hs(h)], v_bf[:, h * 256 + eh * 128:h * 256 + (eh + 1) * 128],
                         attn[:, h, :], start=True, stop=False, rd=vr_h + [attn_r[h]], wr=[pr[3 + eh]])
                for ci in range(2):
                    cs = slice(ci * 64, (ci + 1) * 64)
                    for eh in range(2):
                        P.op("pe", "matmul", pb[3 + eh][:, h * 128 + ci * 64:h * 128 + (ci + 1) * 64],
                             Sb[:, h, eh * 128:(eh + 1) * 128], qtl[:, h, cs], start=False, stop=(ci == 1),
                             skip_group_check=True, rd=[Sb_r[h]] + qtl_r, wr=[pr[3 + eh]])
                    db = 5 if dsi % 2 == 0 else 7
                    dsi += 1
                    dps = pb[db][:, 0:256]
                    P.op("pe", "matmul", dps, kdec[cs, hs(h)], v_bf[cs, h * 256:(h + 1) * 256], start=True, stop=True,
                         rd=kdec_r + vr_h, wr=[pr[db]])
                    P.op("dve", "scalar_tensor_tensor", out=Sf[:, h, :], in0=Sf[:, h, :],
                         scalar=Ef[:, h, ci * 64 + 63:ci * 64 + 64], in1=dps, op0=ALU.mult, op1=ALU.add,
                         rd=[Sf_r[h], pr[db]] + Ef_r, wr=[Sf_r[h]])
                    P.op("pool", "tensor_copy", Sb[:, h, :], Sf[:, h, :], rd=[Sf_r[h]], wr=[Sb_r[h]])
            for eh in range(2):
                P.op("act", "activation", osq[:, eh, :, :].rearrange("p a b -> p (a b)"), pb[3 + eh], AF.Square,
                     rd=[pr[3 + eh]], wr=[osq_r[eh]])
            for h in range(4):
                for eh in range(2):
                    P.op("pe", "matmul", pb[6][:, hs(h)], c["ones256"], osq[:, eh, h, :], start=(eh == 0), stop=(eh == 1),
                         rd=[osq_r[eh], c["r_ones"]], wr=[pr[6]])
            rso2 = rso.rearrange("p a b -> p (a b)")
            P.op("dve", "tensor_scalar_add", rso2, pb[6], EPS, rd=[pr[6]], wr=rso_r)
            P.op("act", "activation", rso2, rso2, AF.Ln, rd=rso_r, wr=rso_r)
            P.op("act", "activation", rso2, rso2, AF.Exp, scale=-0.5, rd=rso_r, wr=rso_r)
            for cidx in range(8):
                bk = cidx // 4
                fm_proj(pb[bk][:, hs(cidx % 4)], pr[bk], 2064 + cidx * 128, 128, xb, xb_r, tsl)
            for bk in range(2):
                P.op("act", "activation", sil[:, bk * 4:(bk + 1) * 4, :].rearrange("p a b -> p (a b)"), pb[bk], AF.Silu,
                     rd=[pr[bk]], wr=[sil_r[bk]])
            for h in range(4):
                for eh in range(2):
                    cidx = h * 2 + eh
                    tmp, tmp_r = tmp_l[cidx % 2]
                    P.op("dve", "scalar_tensor_tensor", out=tmp, in0=pb[3 + eh][:, hs(h)],
                         scalar=k.vec(p + "gla_out_norm", eh), in1=rso[:, h, :], op0=ALU.mult, op1=ALU.mult,
                         rd=[pr[3 + eh], vr] + rso_r, wr=tmp_r)
                    P.op("pool", "tensor_tensor", out=gT[:, cidx, tsl], in0=tmp, in1=sil[:, cidx, :], op=ALU.mult,
                         rd=tmp_r + [sil_r[cidx // 4]], wr=[gT_r[cidx]])

        def ychunk(oc, ps, ps_r):
            for cc in range(DC):
                P.op("pe", "matmul", ps, w_o[:, cc, oc * 128:(oc + 1) * 128], gT[:, cc, :],
                     start=(cc == 0), stop=(cc == DC - 1), rd=[w_o_r[cc], gT_r[cc]], wr=[ps_r])
        ps_y = [(pb[0][:, 0:TT], pr[0]), (pb[1][:, 0:TT], pr[1])]
        ps_stat = [(pb[2][:, 0:TT], pr[2]), (pb[5][:, 0:TT], pr[5])]
        ln_tail(ctx, t, ti, ychunk, xs, xs_r[0], 0, p + "ln1_g", p + "ln1_b", dst, ps_y, ps_stat)


def phase_conv(ctx, l, src, dst):
    k, P, M, T, TT, NT = ctx["k"], ctx["P"], ctx["M"], ctx["T"], ctx["TT"], ctx["NT"]
    W, c, pb, pr = ctx["W"], ctx["consts"], ctx["pbanks"], ctx["pres"]
    p = f"l{l}_"
    H = 30
    w_in_d, w_o_d = W[p + "conv_w_in"], W[p + "conv_w_o"]
    w_in, w_in_r = M.alloc("cv_w_in", [DC, 2 * D], BF16, nres=DC)
    w_o, w_o_r = M.alloc("cv_w_o", [DC, D], BF16, nres=DC)
    for kc in range(DC):
        load_w(ctx, w_in[:, kc, :], w_in_d[kc * 128:(kc + 1) * 128, :], [w_in_r[kc]])
    for kc in range(DC):
        load_w(ctx, w_o[:, kc, :], w_o_d[kc * 128:(kc + 1) * 128, :], [w_o_r[kc]])
    NB = 2
    xs_l = [M.alloc(f"cxs{i}", [DC, TT], F32) for i in range(NB)]
    xb_l = [M.alloc(f"cxb{i}", [DC, TT + H], BF16) for i in range(NB)]
    sg_l = [M.alloc(f"csg{i}", [TT + H], F32) for i in range(2)]
    u_l = [M.alloc(f"cu{i}", [TT + H], F32) for i in range(2)]
    aD_l = [M.alloc(f"caD{i}", [TT], F32) for i in range(2)]
    aP_l = [M.alloc(f"caP{i}", [TT], F32) for i in range(2)]
    tm_l = [M.alloc(f"ctm{i}", [TT], F32) for i in range(2)]
    gs, gs_r = M.alloc("cgs", [DC, TT], BF16, nres=DC)
    t2 = ln_tail_alloc(ctx)
    t = ln_tail_alloc(ctx)
    srcv = src.rearrange("(c p) t -> p c t", p=128)

    def load_x(ti, slot):
        xs, xs_r = xs_l[slot]
        xb, xb_r = xb_l[slot]
        t0 = ti * TT
        rdr = [k.dr(("x", id(src), ti))]
        P.dma("sp", xs[:, :, :], srcv[:, :, t0:t0 + TT], rd=rdr, wr=xs_r)
        if ti == 0:
            P.op("pool", "memset", xb[:, :, 0:H], 0.0, wr=xb_r)
            P.dma("pool", xb[:, :, H:TT + H], srcv[:, :, 0:TT], rd=rdr, wr=xb_r)
        else:
            rdr.append(k.dr(("x", id(src), ti - 1)))
            P.dma("pool", xb[:, :, :], srcv[:, :, t0 - H:t0 + TT], rd=rdr, wr=xb_r)

    load_x(0, 0)
    for ti in range(NT):
        slot = ti % NB
        if ti + 1 < NT:
            load_x(ti + 1, (ti + 1) % NB)
        xs, xs_r = xs_l[slot]
        xb, xb_r = xb_l[slot]
        for cc in range(DC):
            ba, bg = (cc % 2) * 2, (cc % 2) * 2 + 1
            psa, psg = pb[ba][:, 0:TT + H], pb[bg][:, 0:TT + H]
            for kc in range(DC):
                P.op("pe", "matmul", psa, w_in[:, kc, cc * 128:(cc + 1) * 128], xb[:, kc, :],
                     start=(kc == 0), stop=(kc == DC - 1), rd=[w_in_r[kc]] + xb_r, wr=[pr[ba]])
            for kc in range(DC):
                P.op("pe", "matmul", psg, w_in[:, kc, D + cc * 128:D + (cc + 1) * 128], xb[:, kc, :],
                     start=(kc == 0), stop=(kc == DC - 1), rd=[w_in_r[kc]] + xb_r, wr=[pr[bg]])
            sg, sg_r = sg_l[cc % 2]
            u, u_r = u_l[cc % 2]
            aD, aD_r = aD_l[cc % 2]
            aP, aP_r = aP_l[cc % 2]
            P.op("act", "activation", sg, psg, AF.Sigmoid, bias=k.vec(p + "conv_b_in", DC + cc), scale=1.0,
                 rd=[pr[bg], c["vr"]], wr=sg_r)
            P.op("dve", "scalar_tensor_tensor", out=u, in0=psa, scalar=k.vec(p + "conv_b_in", cc), in1=sg,
                 op0=ALU.add, op1=ALU.mult, rd=[pr[ba], c["vr"]] + sg_r, wr=u_r)
            if ti == 0:
                P.op("pool", "memset", u[:, 0:H], 0.0, rd=u_r, wr=u_r)
            P.op("act", "activation", aD, u[:, 0:TT], AF.Identity, bias=k.vec(p + "conv_dw_b", cc),
                 scale=k.vec(p + "conv_dw0", cc), rd=u_r + [c["vr"]], wr=aD_r)
            ND = 18
            for j in range(1, ND + 1):
                P.op("dve", "scalar_tensor_tensor", out=aD, in0=u[:, j:j + TT], scalar=k.vec(p + f"conv_dw{j}", cc),
                     in1=aD, op0=ALU.mult, op1=ALU.add, rd=u_r + aD_r + [c["vr"]], wr=aD_r)
            P.op("act", "activation", aP, u[:, ND + 1:ND + 1 + TT], AF.Identity, scale=k.vec(p + f"conv_dw{ND + 1}", cc),
                 rd=u_r + [c["vr"]], wr=aP_r)
            for j in range(ND + 2, 31):
                tm, tm_r = tm_l[j % 2]
                P.op("act", "activation", tm, u[:, j:j + TT], AF.Identity, scale=k.vec(p + f"conv_dw{j}", cc),
                     rd=u_r + [c["vr"]], wr=tm_r)
                P.op("pool", "tensor_tensor", out=aP, in0=aP, in1=tm, op=ALU.add, rd=aP_r + tm_r, wr=aP_r)
            ho = t2["r"][:, cc, :]
            P.op("dve", "tensor_tensor", out=ho, in0=aD, in1=aP, op=ALU.add, rd=aD_r + aP_r, wr=[t2["r_r"][cc]])
            P.op("pool", "tensor_copy", t2["rb"][:, cc, :], ho, rd=[t2["r_r"][cc]], wr=[t2["rb_r"][cc]])
            P.op("act", "activation", t2["rsq"][:, cc, :], ho, AF.Square, rd=[t2["r_r"][cc]], wr=[t2["rsq_r"][cc]])
        t2["ps_stat"] = [(pb[4][:, 0:TT], pr[4]), (pb[5][:, 0:TT], pr[5])]

        def fin(oc, ro, rres):
            P.op("act", "activation", gs[:, oc, :], ro, AF.Silu, rd=[rres], wr=[gs_r[oc]])
        ln_core(ctx, t2, p + "conv_ln_g", p + "conv_ln_b", fin)

        def ychunk(oc, ps, ps_r):
            for cc in range(DC):
                P.op("pe", "matmul", ps, w_o[:, cc, oc * 128:(oc + 1) * 128], gs[:, cc, :],
                     start=(cc == 0), stop=(cc == DC - 1), rd=[w_o_r[cc], gs_r[cc]], wr=[ps_r])
        ps_y = [(pb[6][:, 0:TT], pr[6]), (pb[7][:, 0:TT], pr[7])]
        ps_stat = [(pb[4][:, 0:TT], pr[4]), (pb[5][:, 0:TT], pr[5])]
        ln_tail(ctx, t, ti, ychunk, xs, xs_r[0], 0, p + "ln1_g", p + "ln1_b", dst, ps_y, ps_stat,
                bias_name=p + "conv_b_o")


WNAMES = {
    0: ["mla_w_in", "mla_w_uq", "mla_w_ukv", "mla_w_o"],
    1: ["gla_w_in", "gla_w_a2", "gla_b_a", "gla_w_o"],
    2: ["conv_w_in", "conv_w_o"],
}


def make_consts():
    cst = np.zeros((128, 160), np.float32)
    maskb = np.zeros((128, 2048), np.float32)
    cst[:, 0:128] = np.eye(128, dtype=np.float32)
    half = 32
    inv = (10000.0 ** (-np.arange(half, dtype=np.float32) / half)).astype(np.float32)
    cst[0:32, 128] = inv
    cst[32:64, 128] = inv
    cst[64:96, 128] = inv
    cst[96:128, 128] = inv
    qq = np.arange(512)[None, :]
    for d in range(4):
        kk = (d * 128 + np.arange(128))[:, None]
        maskb[:, d * 512:(d + 1) * 512] = np.where(kk <= qq, 0.0, NEG)
    return cst, maskb


def make_glac():
    g = np.zeros((128, 384), np.float32)
    j = np.arange(128)[:, None]
    i = np.arange(128)[None, :]
    same = (j // 64) == (i // 64)
    g[:, 0:128] = np.where(same & (j <= i), -1.0 / 16.0, 0.0)
    g[:, 128:256] = np.where(same & (j > i), -1.0 / 16.0, 0.0)
    g[:, 256:384] = np.where(same & (j <= i), 1.0, 0.0)
    return g


def kernel(**inputs):
    inp = {kk: np.asarray(v) for kk, v in inputs.items()}
    x = inp["x"]
    B, S, _ = x.shape
    vp = build_vecpack(inp)
    vecs = vp.pack()
    layers = [(l, True, True) for l in range(DEPTH)]
    shapes = {}
    wmaps = {}
    for l in range(DEPTH):
        p = f"l{l}_"
        names = WNAMES[layer_kind(l)] + ["ffn_w_in", "ffn_w_out"]
        for nm in names:
            a = np.ascontiguousarray(inp[p + nm], dtype=np.float32)
            if a.ndim == 1:
                a = a.reshape(1, -1)
            shapes[p + nm] = a.shape
            wmaps[p + nm] = a
    nc, _ = build_program(S, shapes, vp.off, vp.n, layers)
    cst, maskb = make_consts()
    in_maps = []
    for b in range(B):
        m = dict(wmaps)
        m["xT"] = np.ascontiguousarray(x[b].T)
        m["vecs"] = vecs
        m["pos"] = np.ascontiguousarray(inp["positions"][b].reshape(1, S).astype(np.int32))
        m["cst"] = cst
        m["maskb"] = maskb
        m["glac"] = make_glac()
        in_maps.append(m)
    res = run_bass_kernel_spmd(nc, in_maps, core_ids=list(range(B)))
    out = np.stack([np.ascontiguousarray(res.results[b]["outT"].T) for b in range(B)], axis=0)
    return out.astype(np.float32)
```

```python
import numpy as np
import concourse.bass as bass
import concourse.mybir as mybir
from concourse.bass_utils import run_bass_kernel_spmd

F32 = mybir.dt.float32
BF16 = mybir.dt.bfloat16
I32 = mybir.dt.int32
AF = mybir.ActivationFunctionType
ALU = mybir.AluOpType
AX = mybir.AxisListType

D = 1024
DC = 8
FF = 2816
FC = 22
DEPTH = 4
ALPHA = float((2.0 * DEPTH) ** 0.25)
EPS = 1e-5
SAME_ENG_SYNC = False
NEG = -30000.0


class Res:
    __slots__ = ("name", "w", "rs", "pred")

    def __init__(self, name):
        self.name = name
        self.w = None
        self.rs = []
        self.pred = []


class Op:
    __slots__ = ("eng", "fn", "deps", "inc", "tick", "dma", "sem", "idx")

    def __init__(self, eng, fn, dma):
        self.eng = eng
        self.fn = fn
        self.deps = []
        self.inc = False
        self.tick = 0
        self.dma = dma
        self.sem = None
        self.idx = 0


ENGS = ("pe", "act", "dve", "pool", "sp")
NDMASEM = 8


class Prog:
    def __init__(self):
        self.streams = {e: [] for e in ENGS}
        self.ndma = {e: 0 for e in ENGS}
        self.dma_hist = {e: [] for e in ENGS}
        self.n = 0

    def _deps(self, op, rd, wr):
        deps = op.deps
        for r in rd:
            if r.pred:
                deps.extend(r.pred)
            if r.w is not None:
                deps.append(r.w)
        for r in wr:
            if r.pred:
                deps.extend(r.pred)
            if r.w is not None:
                deps.append(r.w)
            deps.extend(r.rs)
        for r in rd:
            r.rs.append(op)
            r.pred = []
        for r in wr:
            r.w = op
            r.rs = []
            r.pred = []
        for d in deps:
            if d.dma or d.eng != op.eng or (SAME_ENG_SYNC and op.eng != "pe"):
                d.inc = True

    def op(self, eng, meth, *args, rd=(), wr=(), **kw):
        fn = (lambda e, meth=meth, args=args, kw=kw: getattr(e, meth)(*args, **kw))
        o = Op(eng, fn, False)
        o.idx = self.n
        self.n += 1
        self._deps(o, rd, wr)
        self.streams[eng].append(o)
        return o

    def dma(self, eng, out, in_, rd=(), wr=()):
        o = Op(eng, (lambda e, out=out, in_=in_: e.dma_start(out=out, in_=in_)), True)
        o.idx = self.n
        self.n += 1
        k = self.ndma[eng]
        self.ndma[eng] = k + 1
        o.sem = (eng, k % NDMASEM)
        o.tick = 16 * (k // NDMASEM + 1)
        hist = self.dma_hist[eng]
        if k >= NDMASEM:
            o.deps.append(hist[k - NDMASEM])
        hist.append(o)
        self._deps(o, rd, wr)
        o.inc = True
        self.streams[eng].append(o)
        return o

    def retire(self, resources):
        out = []
        for r in resources:
            if r.w is not None:
                out.append(r.w)
            out.extend(r.rs)
            out.extend(r.pred)
        last = {}
        res = []
        for o in out:
            if o.dma:
                res.append(o)
            else:
                if o.eng not in last or last[o.eng].idx < o.idx:
                    last[o.eng] = o
        res.extend(last.values())
        for o in res:
            o.inc = True
        return res

    def emit(self, nc, block, sems, dsems):
        for e in ENGS:
            t = 0
            for o in self.streams[e]:
                if not o.dma and o.inc:
                    t += 1
                    o.tick = t
        streams = self.streams

        def run(eng_name, e):
            seen = {}
            for o in streams[eng_name]:
                need = {}
                for d in o.deps:
                    if d.dma:
                        key = d.sem
                    else:
                        if d.eng == eng_name and (eng_name == "pe" or not SAME_ENG_SYNC):
                            continue
                        key = d.eng
                    if need.get(key, 0) < d.tick:
                        need[key] = d.tick
                for key, val in need.items():
                    if seen.get(key, 0) >= val:
                        continue
                    seen[key] = val
                    s = dsems[key] if isinstance(key, tuple) else sems[key]
                    e.wait_ge(s, val)
                ins = o.fn(e)
                if o.inc:
                    if o.dma:
                        ins.then_inc(dsems[o.sem], 16)
                    else:
                        ins.then_inc(sems[eng_name], 1)

        @block.tensor
        def _(e):
            run("pe", e)

        @block.scalar
        def _(e):
            run("act", e)

        @block.vector
        def _(e):
            run("dve", e)

        @block.gpsimd
        def _(e):
            run("pool", e)

        @block.sync
        def _(e):
            run("sp", e)


class Mem:
    def __init__(self, P, big, words):
        self.P = P
        self.big = big
        self.words = words
        self.top = 0
        self.live = []

    def mark(self):
        return self.top

    def release(self, mark):
        self.top = mark

    def alloc(self, name, shape, dtype, nres=1):
        n = 1
        for s in shape:
            n *= s
        if dtype == BF16:
            w = (n + 1) // 2
        else:
            w = n
        a = self.top
        b = a + w
        assert b <= self.words, f"SBUF overflow allocating {name}: {b} > {self.words}"
        self.top = b
        pred = []
        keep = []
        for (s, e, rl) in self.live:
            if s < b and a < e:
                pred.extend(self.P.retire(rl))
            else:
                keep.append((s, e, rl))
        self.live = keep
        rl = [Res(f"{name}{i}") for i in range(nres)]
        for r in rl:
            r.pred = list(pred)
        self.live.append((a, b, rl))
        ap = self.big[:, a:b]
        if dtype == BF16:
            ap = ap.bitcast(BF16)[:, 0:n]
        elif dtype == I32:
            ap = ap.bitcast(I32)
        if len(shape) == 2:
            ap = ap.rearrange("p (a b) -> p a b", a=shape[0])
        elif len(shape) == 3:
            ap = ap.rearrange("p (a b c) -> p a b c", a=shape[0], b=shape[1])
        return ap, rl


def fm(v):
    v = np.asarray(v, np.float32)
    return np.ascontiguousarray(v.reshape(-1, 128).T)


class VecPack:
    def __init__(self):
        self.cols = []
        self.off = {}
        self.n = 0

    def add(self, name, arr2d):
        arr2d = np.asarray(arr2d, np.float32)
        assert arr2d.shape[0] == 128
        self.off[name] = (self.n, arr2d.shape[1])
        self.cols.append(arr2d)
        self.n += arr2d.shape[1]

    def pack(self):
        return np.ascontiguousarray(np.concatenate(self.cols, axis=1))


def layer_kind(i):
    return i % 3


def build_vecpack(inp):
    vp = VecPack()
    for l in range(DEPTH):
        p = f"l{l}_"
        for nm in ("ln1_g", "ln1_b", "ln2_g", "ln2_b"):
            vp.add(p + nm, fm(inp[p + nm]))
        cw = np.asarray(inp[p + "ffn_conv"], np.float32)
        for k in range(3):
            vp.add(p + f"ffn_conv{k}", fm(cw[k]))
        vp.add(p + "ffn_conv_b", fm(inp[p + "ffn_conv_b"]))
        kind = layer_kind(l)
        if kind == 0:
            vp.add(p + "mla_q_norm", fm(inp[p + "mla_q_norm"]))
            vp.add(p + "mla_kv_norm", fm(inp[p + "mla_kv_norm"]))
        elif kind == 1:
            vp.add(p + "gla_b_a", fm(inp[p + "gla_b_a"]))
            vp.add(p + "gla_out_norm", fm(inp[p + "gla_out_norm"]))
        else:
            vp.add(p + "conv_b_in", fm(inp[p + "conv_b_in"]))
            dw = np.asarray(inp[p + "conv_dw"], np.float32)
            for k in range(31):
                vp.add(p + f"conv_dw{k}", fm(dw[k]))
            vp.add(p + "conv_dw_b", fm(inp[p + "conv_dw_b"]))
            vp.add(p + "conv_ln_g", fm(inp[p + "conv_ln_g"]))
            vp.add(p + "conv_ln_b", fm(inp[p + "conv_ln_b"]))
            vp.add(p + "conv_b_o", fm(inp[p + "conv_b_o"]))
    return vp


class K:
    def __init__(self, T, vp_off, nvec, phases, debug_out=None):
        self.T = T
        self.vp_off = vp_off
        self.nvec = nvec
        self.phases = phases
        nc = bass.Bass("TRN2", target_bir_lowering=False)
        self.nc = nc
        self.P = Prog()
        self.dram = {}
        self.dres = {}

    def din(self, name, shape, dtype=F32):
        t = self.nc.dram_tensor(name, list(shape), dtype, kind="ExternalInput").ap()
        self.dram[name] = t
        return t

    def dout(self, name, shape, dtype=F32):
        t = self.nc.dram_tensor(name, list(shape), dtype, kind="ExternalOutput").ap()
        self.dram[name] = t
        return t

    def dscr(self, name, shape, dtype=F32):
        t = self.nc.dram_tensor(name, list(shape), dtype, kind="Internal").ap()
        self.dram[name] = t
        return t

    def dr(self, key):
        r = self.dres.get(key)
        if r is None:
            r = Res(str(key))
            self.dres[key] = r
        return r

    def vec(self, name, j=0, n=1):
        o, w = self.vp_off[name]
        return self.vecs[:, o + j:o + j + n]


def tiles_order(n):
    return list(range(n))


def build_program(T, inp_shapes, vp_off, nvec, layers, TT=256):
    k = K(T, vp_off, nvec, None)
    nc = k.nc
    P = k.P
    NT = T // TT

    xT_in = k.din("xT", [D, T])
    vecs_d = k.din("vecs", [128, nvec])
    pos_d = k.din("pos", [1, T], I32)
    cst_d = k.din("cst", [128, 160])
    mask_d = k.din("maskb", [128, 2048])
    glac_d = k.din("glac", [128, 384])
    W = {}
    for name, shp in inp_shapes.items():
        W[name] = k.din(name, shp)
    outT = k.dout("outT", [D, T])
    nph = sum(int(a) + int(b) for (_, a, b) in layers)
    xs_d = [xT_in]
    for i in range(nph - 1):
        xs_d.append(k.dscr(f"xs{i}", [D, T]))
    xs_d.append(outT)

    WORDS = 53000
    import contextlib
    es = contextlib.ExitStack()
    with es:
        big_h = es.enter_context(nc.sbuf_tensor("big", [128, WORDS], F32))
        big = big_h[:, :]
        pbanks = []
        for i in range(8):
            ph = es.enter_context(nc.psum_tensor(f"ps{i}", [128, 512], F32))
            pbanks.append(ph[:, :])
        sems = {e: es.enter_context(nc.semaphore(f"s_{e}")) for e in ENGS}
        dsems = {}
        for e in ("sp", "pool", "act"):
            for i in range(NDMASEM):
                dsems[(e, i)] = es.enter_context(nc.semaphore(f"d_{e}{i}"))
        M = Mem(P, big, WORDS)
        pres = [Res(f"psum{i}") for i in range(8)]

        vecs, vr = M.alloc("vecs", [nvec], F32)
        k.vecs = vecs
        vr = vr[0]
        P.dma("sp", vecs, vecs_d[:, :], wr=[vr])
        ones1024, r_o = M.alloc("ones1024", [128], BF16)
        r_ones = r_o[0]
        P.op("dve", "memset", ones1024, 1.0 / 1024.0, wr=[r_ones])
        ones256, _r = M.alloc("ones256", [128], BF16)
        P.op("dve", "memset", ones256, 1.0 / 256.0, wr=[r_ones])
        ones128, _r = M.alloc("ones128", [128], BF16)
        P.op("dve", "memset", ones128, 1.0 / 128.0, wr=[r_ones])
        ones1, _r = M.alloc("ones1", [128], BF16)
        P.op("dve", "memset", ones1, 1.0, wr=[r_ones])
        cst, r_c = M.alloc("cst", [160], F32)
        r_cst = r_c[0]
        P.dma("sp", cst, cst_d[:, :], wr=[r_cst])
        ident_b, _r = M.alloc("ident", [128], BF16)
        P.op("dve", "tensor_copy", ident_b, cst[:, 0:128], rd=[r_cst], wr=[r_ones])
        consts = dict(ones1024=ones1024, ones256=ones256, ones128=ones128, ones1=ones1,
                      ident=ident_b, cst=cst, r_ones=r_ones, r_cst=r_cst, vr=vr)
        base_mark = M.mark()

        ctx = dict(k=k, nc=nc, P=P, M=M, pbanks=pbanks, pres=pres, T=T, TT=TT, NT=NT,
                   consts=consts, W=W, pos_d=pos_d, mask_d=mask_d, glac_d=glac_d)

        xi = 0
        for (l, do_mixer, do_ffn) in layers:
            kind = layer_kind(l)
            if do_mixer:
                M.release(base_mark)
                if kind == 0:
                    phase_mla(ctx, l, xs_d[xi], xs_d[xi + 1])
                elif kind == 1:
                    phase_gla(ctx, l, xs_d[xi], xs_d[xi + 1])
                else:
                    phase_conv(ctx, l, xs_d[xi], xs_d[xi + 1])
                xi += 1
            if do_ffn:
                M.release(base_mark)
                phase_ffn(ctx, l, xs_d[xi], xs_d[xi + 1])
                xi += 1
        assert xs_d[xi] is outT or True
        final_src = xs_d[xi]
        fin_deps = [k.dr(("x", id(final_src), i)) for i in range(NT)]
        P.op("sp", "nop", rd=fin_deps)
        with nc.Block() as block:
            P.emit(nc, block, sems, dsems)
    return nc, final_src


def ln_tail_alloc(ctx):
    M, TT = ctx["M"], ctx["TT"]
    t = {}
    t["r"], t["r_r"] = M.alloc("r", [DC, TT], F32, nres=DC)
    t["rb"], t["rb_r"] = M.alloc("rb", [DC, TT], BF16, nres=DC)
    t["rsq"], t["rsq_r"] = M.alloc("rsq", [DC, TT], BF16, nres=DC)
    t["mean"], t["mean_r"] = M.alloc("mean", [TT], F32)
    t["m2"], t["m2_r"] = M.alloc("m2", [TT], F32)
    t["A"], t["A_r"] = M.alloc("A", [TT], F32)
    t["B"], t["B_r"] = M.alloc("B", [TT], F32)
    return t


def ln_core(ctx, t, g_name, b_name, final_fn=None):
    k, P = ctx["k"], ctx["P"]
    c = ctx["consts"]
    r, rb, rsq = t["r"], t["rb"], t["rsq"]
    (pm, pm_r), (pe2, pe2_r) = t["ps_stat"]
    for oc in range(DC):
        P.op("pe", "matmul", pm, c["ones1024"], rb[:, oc, :], start=(oc == 0), stop=(oc == DC - 1),
             rd=[t["rb_r"][oc], c["r_ones"]], wr=[pm_r])
    for oc in range(DC):
        P.op("pe", "matmul", pe2, c["ones1024"], rsq[:, oc, :], start=(oc == 0), stop=(oc == DC - 1),
             rd=[t["rsq_r"][oc], c["r_ones"]], wr=[pe2_r])
    mean, m2, A, B = t["mean"], t["m2"], t["A"], t["B"]
    P.op("act", "activation", mean, pm, AF.Copy, rd=[pm_r], wr=t["mean_r"])
    P.op("pool", "tensor_tensor", out=m2, in0=mean, in1=mean, op=ALU.mult, rd=t["mean_r"], wr=t["m2_r"])
    P.op("dve", "tensor_tensor", out=A, in0=pe2, in1=m2, op=ALU.subtract, rd=[pe2_r] + t["m2_r"], wr=t["A_r"])
    P.op("dve", "tensor_scalar_add", A, A, EPS, rd=t["A_r"], wr=t["A_r"])
    P.op("act", "activation", A, A, AF.Ln, rd=t["A_r"], wr=t["A_r"])
    P.op("act", "activation", A, A, AF.Exp, scale=-0.5, rd=t["A_r"], wr=t["A_r"])
    P.op("dve", "scalar_tensor_tensor", out=B, in0=mean, scalar=-1.0, in1=A, op0=ALU.mult, op1=ALU.mult,
         rd=t["mean_r"] + t["A_r"], wr=t["B_r"])
    for oc in range(DC):
        ro = r[:, oc, :]
        P.op("dve", "tensor_tensor", out=ro, in0=ro, in1=A, op=ALU.mult,
             rd=[t["r_r"][oc]] + t["A_r"], wr=[t["r_r"][oc]])
        P.op("pool", "tensor_tensor", out=ro, in0=ro, in1=B, op=ALU.add,
             rd=[t["r_r"][oc]] + t["B_r"], wr=[t["r_r"][oc]])
        gv, bv = k.vec(g_name, oc), k.vec(b_name, oc)
        P.op("act", "activation", ro, ro, AF.Identity, bias=bv, scale=gv,
             rd=[t["r_r"][oc], c["vr"]], wr=[t["r_r"][oc]])
        if final_fn is not None:
            final_fn(oc, ro, t["r_r"][oc])


def ln_tail(ctx, t, ti, y_chunk_fn, xs, xs_r, xoff, g_name, b_name, dst, ps_y, ps_stat, bias_name=None):
    k, P, TT = ctx["k"], ctx["P"], ctx["TT"]
    c = ctx["consts"]
    r, rb, rsq = t["r"], t["rb"], t["rsq"]
    t["ps_stat"] = ps_stat
    for oc in range(DC):
        ps, ps_r = ps_y[oc % len(ps_y)]
        y_chunk_fn(oc, ps, ps_r)
        xin = xs[:, oc, xoff:xoff + TT]
        ro = r[:, oc, :]
        if bias_name is None:
            P.op("dve", "scalar_tensor_tensor", out=ro, in0=xin, scalar=ALPHA, in1=ps, op0=ALU.mult, op1=ALU.add,
                 rd=[xs_r, ps_r], wr=[t["r_r"][oc]])
        else:
            bv = k.vec(bias_name, oc)
            P.op("act", "activation", ro, ps, AF.Identity, bias=bv, scale=1.0,
                 rd=[ps_r, c["vr"]], wr=[t["r_r"][oc]])
            P.op("dve", "scalar_tensor_tensor", out=ro, in0=xin, scalar=ALPHA, in1=ro, op0=ALU.mult, op1=ALU.add,
                 rd=[xs_r, t["r_r"][oc]], wr=[t["r_r"][oc]])
        P.op("pool", "tensor_copy", rb[:, oc, :], ro, rd=[t["r_r"][oc]], wr=[t["rb_r"][oc]])
        P.op("act", "activation", rsq[:, oc, :], ro, AF.Square, rd=[t["r_r"][oc]], wr=[t["rsq_r"][oc]])
    ln_core(ctx, t, g_name, b_name)
    dview = dst.rearrange("(c p) t -> p c t", p=128)[:, :, ti * TT:(ti + 1) * TT]
    P.dma("sp", dview, r, rd=t["r_r"], wr=[k.dr(("x", id(dst), ti))])


def load_w(ctx, dst_ap, src_ap, res):
    P = ctx["P"]
    P.dma("pool", dst_ap, src_ap, wr=res)


def phase_ffn(ctx, l, src, dst):
    k, P, M, T, TT, NT = ctx["k"], ctx["P"], ctx["M"], ctx["T"], ctx["TT"], ctx["NT"]
    W, c, pb, pr = ctx["W"], ctx["consts"], ctx["pbanks"], ctx["pres"]
    p = f"l{l}_"
    w_in_d, w_out_d = W[p + "ffn_w_in"], W[p + "ffn_w_out"]
    w_in, w_in_r = M.alloc("ffn_w_in", [DC, 2 * FF], BF16, nres=DC * 4)
    w_out, w_out_r = M.alloc("ffn_w_out", [FC, D], BF16, nres=FC)
    for kc in range(DC):
        for q in range(4):
            load_w(ctx, w_in[:, kc, q * 1408:(q + 1) * 1408], w_in_d[kc * 128:(kc + 1) * 128, q * 1408:(q + 1) * 1408],
                   [w_in_r[kc * 4 + q]])
    for fc in range(FC):
        load_w(ctx, w_out[:, fc, :], w_out_d[fc * 128:(fc + 1) * 128, :], [w_out_r[fc]])
    NB = 2
    xs_l, xb_l = [], []
    for i in range(NB):
        xs_l.append(M.alloc(f"xs{i}", [DC, TT + 2], F32))
        xb_l.append(M.alloc(f"xb{i}", [DC, TT + 2], BF16))
    tg_l = [M.alloc(f"tg{i}", [TT], F32) for i in range(2)]
    tv_l = [M.alloc(f"tv{i}", [TT], F32) for i in range(2)]
    gT, gT_r = M.alloc("gT", [FC, TT], BF16, nres=FC)
    t = ln_tail_alloc(ctx)
    srcv = src.rearrange("(c p) t -> p c t", p=128)

    def load_x(ti, slot):
        xs, xs_r = xs_l[slot]
        xb, xb_r = xb_l[slot]
        t0 = ti * TT
        rdr = [k.dr(("x", id(src), ti))]
        if ti == 0:
            P.op("pool", "memset", xs[:, :, 0:2], 0.0, wr=xs_r)
            P.op("pool", "memset", xb[:, :, 0:2], 0.0, wr=xb_r)
            P.dma("sp", xs[:, :, 2:TT + 2], srcv[:, :, 0:TT], rd=rdr, wr=xs_r)
            P.dma("pool", xb[:, :, 2:TT + 2], srcv[:, :, 0:TT], rd=rdr, wr=xb_r)
        else:
            rdr.append(k.dr(("x", id(src), ti - 1)))
            P.dma("sp", xs[:, :, :], srcv[:, :, t0 - 2:t0 + TT], rd=rdr, wr=xs_r)
            P.dma("pool", xb[:, :, :], srcv[:, :, t0 - 2:t0 + TT], rd=rdr, wr=xb_r)

    def fin(gc, tg, tg_r, tv, tv_r):
        P.op("act", "activation", tg, tg, AF.Silu, rd=tg_r, wr=tg_r)
        P.op("pool", "tensor_tensor", out=gT[:, gc, :], in0=tg, in1=tv, op=ALU.mult,
             rd=tg_r + tv_r, wr=[gT_r[gc]])

    load_x(0, 0)
    for ti in range(NT):
        slot = ti % NB
        if ti + 1 < NT:
            load_x(ti + 1, (ti + 1) % NB)
        xs, xs_r = xs_l[slot]
        xb, xb_r = xb_l[slot]
        pend = None
        for gc in range(FC):
            bg, bv_ = (gc % 2) * 2, (gc % 2) * 2 + 1
            psg, psv = pb[bg][:, 0:TT + 2], pb[bv_][:, 0:TT + 2]
            for kc in range(DC):
                P.op("pe", "matmul", psg, w_in[:, kc, gc * 128:(gc + 1) * 128], xb[:, kc, :],
                     start=(kc == 0), stop=(kc == DC - 1),
                     rd=[w_in_r[kc * 4 + (gc * 128) // 1408], w_in_r[kc * 4 + (gc * 128 + 127) // 1408]] + xb_r,
                     wr=[pr[bg]])
            for kc in range(DC):
                c0 = FF + gc * 128
                P.op("pe", "matmul", psv, w_in[:, kc, c0:c0 + 128], xb[:, kc, :],
                     start=(kc == 0), stop=(kc == DC - 1),
                     rd=[w_in_r[kc * 4 + c0 // 1408], w_in_r[kc * 4 + (c0 + 127) // 1408]] + xb_r, wr=[pr[bv_]])
            tg, tg_r = tg_l[gc % 2]
            tv, tv_r = tv_l[gc % 2]
            for (ps, psr, tt, ttr, col) in ((psg, pr[bg], tg, tg_r, gc), (psv, pr[bv_], tv, tv_r, FC + gc)):
                w0, w1, w2 = (k.vec(p + f"ffn_conv{j}", col) for j in range(3))
                cb = k.vec(p + "ffn_conv_b", col)
                P.op("act", "activation", tt, ps[:, 0:TT], AF.Identity, bias=cb, scale=w0,
                     rd=[psr, c["vr"]], wr=ttr)
                P.op("dve", "scalar_tensor_tensor", out=tt, in0=ps[:, 1:TT + 1], scalar=w1, in1=tt,
                     op0=ALU.mult, op1=ALU.add, rd=[psr, c["vr"]] + ttr, wr=ttr)
                P.op("dve", "scalar_tensor_tensor", out=tt, in0=ps[:, 2:TT + 2], scalar=w2, in1=tt,
                     op0=ALU.mult, op1=ALU.add, rd=[psr, c["vr"]] + ttr, wr=ttr)
            if pend is not None:
                fin(*pend)
            pend = (gc, tg, tg_r, tv, tv_r)
        fin(*pend)

        def ychunk(oc, ps, ps_r):
            for fc in range(FC):
                P.op("pe", "matmul", ps, w_out[:, fc, oc * 128:(oc + 1) * 128], gT[:, fc, :],
                     start=(fc == 0), stop=(fc == FC - 1), rd=[w_out_r[fc], gT_r[fc]], wr=[ps_r])
        ps_y = [(pb[4][:, 0:TT], pr[4]), (pb[5][:, 0:TT], pr[5])]
        ps_stat = [(pb[6][:, 0:TT], pr[6]), (pb[7][:, 0:TT], pr[7])]
        ln_tail(ctx, t, ti, ychunk, xs, xs_r[0], 2, p + "ln2_g", p + "ln2_b", dst, ps_y, ps_stat)


def phase_mla(ctx, l, src, dst):
    k, P, M, T, TT = ctx["k"], ctx["P"], ctx["M"], ctx["T"], ctx["TT"]
    W, c, pb, pr = ctx["W"], ctx["consts"], ctx["pbanks"], ctx["pres"]
    p = f"l{l}_"
    TQ = 512
    NQ = T // TQ
    NKT = T // 128
    SC = float(192 ** -0.5)
    PI = float(np.pi)
    vr = c["vr"]
    w_in_d, w_uq_d, w_ukv_d, w_o_d = W[p + "mla_w_in"], W[p + "mla_w_uq"], W[p + "mla_w_ukv"], W[p + "mla_w_o"]
    w_in, w_in_r = M.alloc("m_w_in", [DC, 512], BF16)
    for kc in range(DC):
        load_w(ctx, w_in[:, kc, 0:448], w_in_d[kc * 128:(kc + 1) * 128, :], w_in_r)
    P.op("dve", "tensor_scalar_mul", w_in[:, :, 448:480], w_in[:, :, 416:448], -1.0, rd=w_in_r, wr=w_in_r)
    P.op("dve", "tensor_copy", w_in[:, :, 480:512], w_in[:, :, 384:416], rd=w_in_r, wr=w_in_r)
    w_uq, w_uq_r = M.alloc("m_w_uq", [2, 2048], BF16)
    for c2 in range(2):
        load_w(ctx, w_uq[:, c2, 0:1536], w_uq_d[c2 * 128:(c2 + 1) * 128, :], w_uq_r)
    for c2 in range(2):
        srcv_ = w_uq[:, c2, 0:1536].rearrange("p (h d) -> p h d", h=8)
        dstv_ = w_uq[:, c2, 1536:2048].rearrange("p (h d) -> p h d", h=8)
        P.op("dve", "tensor_scalar_mul", dstv_[:, :, 0:32], srcv_[:, :, 160:192], -1.0, rd=w_uq_r, wr=w_uq_r)
        P.op("dve", "tensor_copy", dstv_[:, :, 32:64], srcv_[:, :, 128:160], rd=w_uq_r, wr=w_uq_r)
    w_ukv, w_ukv_r = M.alloc("m_w_ukv", [2048], BF16)
    load_w(ctx, w_ukv[:, :], w_ukv_d[:, :], w_ukv_r)
    w_o, w_o_r = M.alloc("m_w_o", [8, D], BF16)
    for h in range(8):
        load_w(ctx, w_o[:, h, :], w_o_d[h * 128:(h + 1) * 128, :], w_o_r)
    maskb, maskb_r = M.alloc("m_mask", [4, 512], BF16)
    load_w(ctx, maskb[:, :, :], ctx["mask_d"].rearrange("p (a b) -> p a b", a=4), maskb_r)
    wukT, wukT_r = M.alloc("m_wukT", [8, 128], BF16)
    for h in range(8):
        bk = 6 + h % 2
        pst = pb[bk].bitcast(BF16)[:, 0:128]
        P.op("pe", "transpose", pst, w_ukv[:, h * 256:h * 256 + 128], c["ident"], rd=w_ukv_r + [c["r_ones"]], wr=[pr[bk]])
        P.op("dve", "tensor_copy", wukT[:, h, :], pst, rd=[pr[bk]], wr=wukT_r)
    negpi, negpi_r = M.alloc("m_negpi", [2], F32)
    P.op("dve", "memset", negpi, -PI, wr=negpi_r)
    latT, latT_r = M.alloc("m_latT", [T], BF16, nres=NQ)
    krT, krT_r = M.alloc("m_krT", [T], BF16, nres=NQ)
    lat_tok, lat_tok_r = M.alloc("m_lat_tok", [NKT, 128], BF16, nres=NQ)
    xb_l = [M.alloc(f"m_xb{i}", [DC, TQ], BF16) for i in range(1)]
    xs_l = [M.alloc(f"m_xs{i}", [DC, TT], F32) for i in range(TQ // TT)]
    cqf, cqf_r = M.alloc("m_cqf", [2, TQ], F32, nres=2)
    sq, sq_r = M.alloc("m_sq", [2, TQ], BF16, nres=2)
    cqn, cqn_r = M.alloc("m_cqn", [2, TQ], BF16, nres=2)
    rstd, rstd_r = M.alloc("m_rstd", [TQ], F32)
    ckvf, ckvf_r = cqf[:, 0, :], [cqf_r[0]]
    sqkv, sqkv_r = M.alloc("m_sqkv", [TQ], BF16)
    posi, posi_r = M.alloc("m_posi", [TQ], I32)
    posf, posf_r = M.alloc("m_posf", [TQ], F32)
    a2, a2_r = M.alloc("m_a2", [TQ], F32)
    ang, ang_r = posf, posf_r
    cosf, cosf_r = M.alloc("m_cos", [TQ], F32)
    sinf, sinf_r = M.alloc("m_sin", [TQ], F32)
    coss, coss_r = M.alloc("m_coss", [TQ], F32)
    sins, sins_r = M.alloc("m_sins", [TQ], F32)
    tm1, tm1_r = M.alloc("m_tm1", [TQ], F32)
    tm2, tm2_r = M.alloc("m_tm2", [TQ], F32)
    tmq_l = [(tm1, tm1_r), (tm2, tm2_r), M.alloc("m_tm3", [TQ], F32), M.alloc("m_tm4", [TQ], F32)]
    a0, a0_r = tmq_l[2]
    a1, a1_r = tmq_l[3]
    qn_l = [M.alloc(f"m_qn{i}", [TQ], BF16) for i in range(2)]
    qt, qt_r = M.alloc("m_qt", [8, TQ], BF16, nres=8)
    qr, qr_r = M.alloc("m_qr", [8, TQ], BF16, nres=8)
    PT_l = [M.alloc(f"m_PT{i}", [TQ], BF16) for i in range(3)]
    rL, rL_r = M.alloc("m_rL", [TQ], F32)
    On_l = [M.alloc(f"m_On{i}", [TQ], BF16) for i in range(2)]
    oT, oT_r = M.alloc("m_oT", [8, TQ], BF16, nres=8)
    t = ln_tail_alloc(ctx)
    srcv = src.rearrange("(c p) t -> p c t", p=128)
    invf = c["cst"][0:64, 128:129]

    def load_xb(qb):
        xb, xb_r = xb_l[0]
        rdr = [k.dr(("x", id(src), qb * 2)), k.dr(("x", id(src), qb * 2 + 1))]
        P.dma("pool", xb[:, :, :], srcv[:, :, qb * TQ:(qb + 1) * TQ], rd=rdr, wr=xb_r)

    def rstd_from(ps, psr):
        P.op("dve", "tensor_scalar_add", rstd, ps, EPS, rd=[psr], wr=rstd_r)
        P.op("act", "activation", rstd, rstd, AF.Ln, rd=rstd_r, wr=rstd_r)
        P.op("act", "activation", rstd, rstd, AF.Exp, scale=-0.5, rd=rstd_r, wr=rstd_r)

    def proj(ps, psr, wcols, xb, xb_r, m=128):
        for kc in range(DC):
            P.op("pe", "matmul", ps[0:m, :], w_in[:, kc, wcols:wcols + m], xb[:, kc, :],
                 start=(kc == 0), stop=(kc == DC - 1), rd=w_in_r + xb_r, wr=[psr])

    load_xb(0)
    for qb in range(NQ):
        xb, xb_r = xb_l[0]
        for j in range(TQ // TT):
            tix = qb * (TQ // TT) + j
            xsj, xsj_r = xs_l[j]
            P.dma("sp", xsj[:, :, :], srcv[:, :, tix * TT:(tix + 1) * TT], rd=[k.dr(("x", id(src), tix))], wr=xsj_r)
        blk = slice(qb * TQ, (qb + 1) * TQ)
        p6, p7 = pb[6], pb[7]
        for c2 in range(2):
            ps, psr = (p6, pr[6]) if c2 == 0 else (p7, pr[7])
            proj(ps, psr, c2 * 128, xb, xb_r)
            P.op("act", "activation", cqf[:, c2, :], ps, AF.Copy, rd=[psr], wr=[cqf_r[c2]])
            P.op("act", "activation", sq[:, c2, :], cqf[:, c2, :], AF.Square, rd=[cqf_r[c2]], wr=[sq_r[c2]])
        for c2 in range(2):
            P.op("pe", "matmul", p6, c["ones256"], sq[:, c2, :], start=(c2 == 0), stop=(c2 == 1),
                 rd=[sq_r[c2], c["r_ones"]], wr=[pr[6]])
        rstd_from(p6, pr[6])
        for c2 in range(2):
            P.op("dve", "scalar_tensor_tensor", out=cqn[:, c2, :], in0=cqf[:, c2, :], scalar=k.vec(p + "mla_q_norm", c2),
                 in1=rstd, op0=ALU.mult, op1=ALU.mult, rd=[cqf_r[c2], vr] + rstd_r, wr=[cqn_r[c2]])
        proj(p7, pr[7], 256, xb, xb_r)
        P.op("act", "activation", ckvf, p7, AF.Copy, rd=[pr[7]], wr=ckvf_r)
        P.op("act", "activation", sqkv, ckvf, AF.Square, rd=ckvf_r, wr=sqkv_r)
        P.op("pe", "matmul", p6, c["ones128"], sqkv, start=True, stop=True, rd=sqkv_r + [c["r_ones"]], wr=[pr[6]])
        rstd_from(p6, pr[6])
        P.op("dve", "scalar_tensor_tensor", out=latT[:, blk], in0=ckvf, scalar=k.vec(p + "mla_kv_norm", 0),
             in1=rstd, op0=ALU.mult, op1=ALU.mult, rd=ckvf_r + [vr] + rstd_r, wr=[latT_r[qb]])
        P.dma("sp", posi[0:64, :], ctx["pos_d"][0:1, blk].partition_broadcast(64), wr=posi_r)
        P.op("dve", "tensor_copy", posf[0:64, :], posi[0:64, :], rd=posi_r, wr=posf_r)
        P.op("dve", "tensor_scalar_mul", ang[0:64, :], posf[0:64, :], invf, rd=posf_r + [c["r_cst"]], wr=ang_r)
        MAGIC = 12582912.0
        C1 = 6.28125
        C2 = float(2.0 * np.pi - 6.28125)
        for (dstt, dstr, off) in ((sinf, sinf_r, 0.0), (cosf, cosf_r, 0.5 * PI)):
            if off != 0.0:
                P.op("dve", "tensor_scalar_add", a0[0:64, :], ang[0:64, :], off, rd=ang_r, wr=a0_r)
                aa, aa_r = a0, a0_r
            else:
                aa, aa_r = ang, ang_r
            P.op("dve", "tensor_scalar", out=a1[0:64, :], in0=aa[0:64, :], scalar1=float(1.0 / (2.0 * np.pi)),
                 scalar2=MAGIC, op0=ALU.mult, op1=ALU.add, rd=aa_r, wr=a1_r)
            P.op("dve", "tensor_scalar_add", a1[0:64, :], a1[0:64, :], -MAGIC, rd=a1_r, wr=a1_r)
            P.op("dve", "scalar_tensor_tensor", out=a2[0:64, :], in0=a1[0:64, :], scalar=-C1, in1=aa[0:64, :],
                 op0=ALU.mult, op1=ALU.add, rd=a1_r + aa_r, wr=a2_r)
            P.op("dve", "scalar_tensor_tensor", out=a2[0:64, :], in0=a1[0:64, :], scalar=-C2, in1=a2[0:64, :],
                 op0=ALU.mult, op1=ALU.add, rd=a1_r + a2_r, wr=a2_r)
            P.op("dve", "tensor_scalar", out=a2[0:64, :], in0=a2[0:64, :], scalar1=-3.1415925, scalar2=3.1415925,
                 op0=ALU.max, op1=ALU.min, rd=a2_r, wr=a2_r)
            P.op("act", "activation", dstt[0:64, :], a2[0:64, :], AF.Sin, rd=a2_r, wr=dstr)
        P.op("pool", "tensor_scalar_mul", coss[0:64, :], cosf[0:64, :], SC, rd=cosf_r, wr=coss_r)
        P.op("pool", "tensor_scalar_mul", sins[0:64, :], sinf[0:64, :], SC, rd=sinf_r, wr=sins_r)
        proj(p6, pr[6], 384, xb, xb_r, m=64)
        proj(p7, pr[7], 448, xb, xb_r, m=64)
        if qb + 1 < NQ:
            load_xb(qb + 1)
        P.op("dve", "tensor_tensor", out=tm1[0:64, :], in0=p6[0:64, :], in1=cosf[0:64, :], op=ALU.mult,
             rd=[pr[6]] + cosf_r, wr=tm1_r)
        P.op("dve", "tensor_tensor", out=tm2[0:64, :], in0=p7[0:64, :], in1=sinf[0:64, :], op=ALU.mult,
             rd=[pr[7]] + sinf_r, wr=tm2_r)
        P.op("pool", "tensor_tensor", out=krT[0:64, blk], in0=tm1[0:64, :], in1=tm2[0:64, :], op=ALU.add,
             rd=tm1_r + tm2_r, wr=[krT_r[qb]])
        for j in range(4):
            bk = 6 + j % 2
            pst = pb[bk].bitcast(BF16)[:, 0:128]
            P.op("pe", "transpose", pst, latT[:, qb * TQ + j * 128:qb * TQ + (j + 1) * 128], c["ident"],
                 rd=[latT_r[qb], c["r_ones"]], wr=[pr[bk]])
            P.op("dve", "tensor_copy", lat_tok[:, qb * 4 + j, :], pst, rd=[pr[bk]], wr=[lat_tok_r[qb]])
        for h in range(8):
            qn, qn_r = qn_l[h % 2]
            b0 = 4 * (h % 2)
            pA, pB, pC, pD = pb[b0], pb[b0 + 1], pb[b0 + 2], pb[b0 + 3]
            rA, rB, rC, rD = pr[b0], pr[b0 + 1], pr[b0 + 2], pr[b0 + 3]
            for c2 in range(2):
                P.op("pe", "matmul", pA, w_uq[:, c2, h * 192:h * 192 + 128], cqn[:, c2, :], start=(c2 == 0),
                     stop=(c2 == 1), rd=w_uq_r + [cqn_r[c2]], wr=[rA])
            P.op("act", "activation", qn, pA, AF.Copy, rd=[rA], wr=qn_r)
            for c2 in range(2):
                P.op("pe", "matmul", pC[0:64, :], w_uq[:, c2, h * 192 + 128:h * 192 + 192], cqn[:, c2, :],
                     start=(c2 == 0), stop=(c2 == 1), rd=w_uq_r + [cqn_r[c2]], wr=[rC])
            for c2 in range(2):
                P.op("pe", "matmul", pD[0:64, :], w_uq[:, c2, 1536 + h * 64:1536 + (h + 1) * 64], cqn[:, c2, :],
                     start=(c2 == 0), stop=(c2 == 1), rd=w_uq_r + [cqn_r[c2]], wr=[rD])
            P.op("pe", "matmul", pB, wukT[:, h, :], qn, start=True, stop=True, rd=wukT_r + qn_r, wr=[rB])
            P.op("act", "activation", qt[:, h, :], pB, AF.Identity, scale=SC, rd=[rB], wr=[qt_r[h]])
            tA, tA_r = tmq_l[(2 * h) % 4]
            tB, tB_r = tmq_l[(2 * h + 1) % 4]
            P.op("dve", "tensor_tensor", out=tA[0:64, :], in0=pC[0:64, :], in1=coss[0:64, :], op=ALU.mult,
                 rd=[rC] + coss_r, wr=tA_r)
            P.op("dve", "tensor_tensor", out=tB[0:64, :], in0=pD[0:64, :], in1=sins[0:64, :], op=ALU.mult,
                 rd=[rD] + sins_r, wr=tB_r)
            P.op("pool", "tensor_tensor", out=qr[0:64, h, :], in0=tA[0:64, :], in1=tB[0:64, :], op=ALU.add,
                 rd=tA_r + tB_r, wr=[qr_r[h]])
        nkt = 4 * (qb + 1)
        iters = [(h, kt) for h in range(8) for kt in range(nkt)]

        def emit_S(n):
            h, kt = iters[n]
            Sb = n % 2
            S = pb[Sb]
            PT, PT_r = PT_l[n % 3]
            diag = kt >= 4 * qb
            ks = slice(kt * 128, (kt + 1) * 128)
            P.op("pe", "matmul", S, latT[:, ks], qt[:, h, :], start=True, stop=False,
                 rd=[latT_r[kt // 4], qt_r[h]], wr=[pr[Sb]])
            P.op("pe", "matmul", S, krT[0:64, ks], qr[0:64, h, :], start=False, stop=(not diag),
                 rd=[krT_r[kt // 4], qr_r[h]], wr=[pr[Sb]])
            if diag:
                P.op("pe", "matmul", S, c["ident"], maskb[:, kt - 4 * qb, :], start=False, stop=True,
                     rd=maskb_r + [c["r_ones"]], wr=[pr[Sb]])
            P.op("act", "activation", PT, S, AF.Exp, rd=[pr[Sb]], wr=PT_r)

        def emit_OV(n):
            h, kt = iters[n]
            Ob, Lb = 2 + h % 2, 4 + h % 2
            O, L = pb[Ob], pb[Lb]
            PT, PT_r = PT_l[n % 3]
            P.op("pe", "matmul", O, lat_tok[:, kt, :], PT, start=(kt == 0), stop=(kt == nkt - 1),
                 rd=[lat_tok_r[kt // 4]] + PT_r, wr=[pr[Ob]])
            P.op("pe", "matmul", L, c["ones1"], PT, start=(kt == 0), stop=(kt == nkt - 1),
                 rd=[c["r_ones"]] + PT_r, wr=[pr[Lb]])
            if kt == nkt - 1:
                On, On_r = On_l[h % 2]
                P.op("dve", "reciprocal", rL, L, rd=[pr[Lb]], wr=rL_r)
                P.op("dve", "tensor_tensor", out=On, in0=O, in1=rL, op=ALU.mult, rd=[pr[Ob]] + rL_r, wr=On_r)
                ob = 6 + h % 2
                P.op("pe", "matmul", pb[ob], w_ukv[:, h * 256 + 128:h * 256 + 256], On, start=True, stop=True,
                     rd=w_ukv_r + On_r, wr=[pr[ob]])
                P.op("dve", "tensor_copy", oT[:, h, :], pb[ob], rd=[pr[ob]], wr=[oT_r[h]])

        emit_S(0)
        for n in range(len(iters)):
            if n + 1 < len(iters):
                emit_S(n + 1)
            emit_OV(n)
        for j in range(TQ // TT):
            def ychunk(oc, ps, ps_r, j=j):
                for h in range(8):
                    P.op("pe", "matmul", ps, w_o[:, h, oc * 128:(oc + 1) * 128], oT[:, h, j * TT:(j + 1) * TT],
                         start=(h == 0), stop=(h == 7), rd=w_o_r + [oT_r[h]], wr=[ps_r])
            ps_y = [(pb[6][:, 0:TT], pr[6]), (pb[7][:, 0:TT], pr[7])]
            ps_stat = [(pb[0][:, 0:TT], pr[0]), (pb[1][:, 0:TT], pr[1])]
            tix = qb * (TQ // TT) + j
            xsj, xsj_r = xs_l[j]
            ln_tail(ctx, t, tix, ychunk, xsj, xsj_r[0], 0, p + "ln1_g", p + "ln1_b", dst, ps_y, ps_stat)


def phase_gla(ctx, l, src, dst):
    k, P, M, T, TT, NT = ctx["k"], ctx["P"], ctx["M"], ctx["T"], ctx["TT"], ctx["NT"]
    W, c, pb, pr = ctx["W"], ctx["consts"], ctx["pbanks"], ctx["pres"]
    p = f"l{l}_"
    vr = c["vr"]
    w_in_d, w_a2_d, b_a_d, w_o_d = W[p + "gla_w_in"], W[p + "gla_w_a2"], W[p + "gla_b_a"], W[p + "gla_w_o"]
    NW = 3088
    w_in, w_in_r = M.alloc("g_w_in", [DC, NW], BF16, nres=DC)
    for kc in range(DC):
        load_w(ctx, w_in[:, kc, :], w_in_d[kc * 128:(kc + 1) * 128, :], [w_in_r[kc]])
    w_o, w_o_r = M.alloc("g_w_o", [DC, D], BF16, nres=DC)
    for kc in range(DC):
        load_w(ctx, w_o[:, kc, :], w_o_d[kc * 128:(kc + 1) * 128, :], [w_o_r[kc]])
    w_a2b, w_a2b_r = M.alloc("g_w_a2b", [512], BF16)
    load_w(ctx, w_a2b[0:16, :], w_a2_d[:, :], w_a2b_r)
    load_w(ctx, w_a2b[16:17, :], b_a_d[:, :], w_a2b_r)
    gc3, gc3_r = M.alloc("g_c3", [3, 128], BF16)
    load_w(ctx, gc3[:, :, :], ctx["glac_d"].rearrange("p (a b) -> p a b", a=3), gc3_r)
    triU, triR = gc3[:, 0, :], gc3[:, 1, :]
    maskA, maskA_r = M.alloc("g_maskA", [128], F32)
    P.dma("sp", maskA, ctx["glac_d"][:, 256:384], wr=maskA_r)
    onec, onec_r = M.alloc("g_onec", [2], F32)
    P.op("dve", "memset", onec, 1.0, wr=onec_r)
    alr1, alr1_r = M.alloc("g_alr1", [128], BF16)
    P.op("dve", "memset", alr1[0:32, :], 1.0, wr=alr1_r)
    Sf, Sf_r = M.alloc("g_Sf", [4, 256], F32, nres=4)
    Sb, Sb_r = M.alloc("g_Sb", [4, 256], BF16, nres=4)
    for h in range(4):
        P.op("dve", "memset", Sf[:, h, :], 0.0, wr=[Sf_r[h]])
        P.op("dve", "memset", Sb[:, h, :], 0.0, wr=[Sb_r[h]])
    xs_l = [M.alloc(f"g_xs{i}", [DC, TT], F32) for i in range(2)]
    xb_l = [M.alloc(f"g_xb{i}", [DC, TT], BF16) for i in range(2)]
    Lf, Lf_r = M.alloc("g_Lf", [512], F32)
    Lb, Lb_r = M.alloc("g_Lb", [512], BF16)
    Ef, Ef_r = M.alloc("g_Ef", [4, 128], F32)
    Emf, Emf_r = M.alloc("g_Emf", [4, 128], F32)
    Erf, Erf_r = M.alloc("g_Erf", [512], F32)
    qtl, qtl_r = M.alloc("g_qtl", [4, 128], BF16)
    ktl, ktl_r = M.alloc("g_ktl", [4, 128], BF16)
    kdec, kdec_r = M.alloc("g_kdec", [512], BF16)
    v_bf, v_bf_r = M.alloc("g_vbf", [1024], BF16, nres=2)
    attn, attn_r = M.alloc("g_attn", [4, 128], BF16, nres=4)
    osq, osq_r = M.alloc("g_osq", [2, 4, 128], BF16, nres=2)
    rso, rso_r = M.alloc("g_rso", [4, 128], F32)
    sil, sil_r = M.alloc("g_sil", [8, 128], F32, nres=2)
    tmp_l = [M.alloc(f"g_tmp{i}", [128], F32) for i in range(2)]
    gT, gT_r = M.alloc("g_gT", [DC, TT], BF16, nres=DC)
    t = ln_tail_alloc(ctx)
    srcv = src.rearrange("(c p) t -> p c t", p=128)
    QS = float(128 ** -0.5)

    def load_x(ti, slot):
        xs, xs_r = xs_l[slot]
        xb, xb_r = xb_l[slot]
        rdr = [k.dr(("x", id(src), ti))]
        P.dma("sp", xs[:, :, :], srcv[:, :, ti * TT:(ti + 1) * TT], rd=rdr, wr=xs_r)
        P.dma("pool", xb[:, :, :], srcv[:, :, ti * TT:(ti + 1) * TT], rd=rdr, wr=xb_r)

    def fm_proj(ps, psr, col0, m, xb, xb_r, tsl):
        for kc in range(DC):
            P.op("pe", "matmul", ps, w_in[:, kc, col0:col0 + m], xb[:, kc, tsl], start=(kc == 0), stop=(kc == DC - 1),
                 rd=[w_in_r[kc]] + xb_r, wr=[psr])

    def tm_proj(ps, psr, col0, n, xb, xb_r, tsl):
        for kc in range(DC):
            P.op("pe", "matmul", ps, xb[:, kc, tsl], w_in[:, kc, col0:col0 + n], start=(kc == 0), stop=(kc == DC - 1),
                 rd=[w_in_r[kc]] + xb_r, wr=[psr])

    def hs(h):
        return slice(h * 128, (h + 1) * 128)

    load_x(0, 0)
    for ti in range(NT):
        slot = ti % 2
        if ti + 1 < NT:
            load_x(ti + 1, (ti + 1) % 2)
        xs, xs_r = xs_l[slot]
        xb, xb_r = xb_l[slot]
        for sub in range(TT // 128):
            tsl = slice(sub * 128, (sub + 1) * 128)
            for h in range(4):
                fm_proj(pb[0][:, hs(h)], pr[0], h * 128, 128, xb, xb_r, tsl)
            for h in range(4):
                fm_proj(pb[1][:, hs(h)], pr[1], 512 + h * 128, 128, xb, xb_r, tsl)
            fm_proj(pb[7][0:16, 0:128], pr[7], 2048, 16, xb, xb_r, tsl)
            tm_proj(pb[2], pr[2], 512, 512, xb, xb_r, tsl)
            tm_proj(pb[3], pr[3], 1024, 512, xb, xb_r, tsl)
            tm_proj(pb[4], pr[4], 1536, 512, xb, xb_r, tsl)
            P.op("act", "activation", alr1[0:16, :], pb[7][0:16, 0:128], AF.Copy, rd=[pr[7]], wr=alr1_r)
            P.op("pe", "matmul", pb[5], alr1[0:17, :], w_a2b[0:17, :], start=True, stop=True,
                 rd=alr1_r + w_a2b_r, wr=[pr[5]])
            P.op("act", "activation", Lf, pb[5], AF.Exp, scale=-1.0, rd=[pr[5]], wr=Lf_r)
            P.op("act", "activation", Lb, Lf, AF.Ln, bias=onec[:, 0:1], scale=1.0, rd=Lf_r + onec_r, wr=Lb_r)
            for h in range(4):
                P.op("pe", "matmul", pb[6][:, hs(h)], Lb[:, hs(h)], triU, start=True, stop=True,
                     rd=Lb_r + gc3_r, wr=[pr[6]])
            P.op("pe", "matmul", pb[7], triR, Lb, start=True, stop=True, rd=Lb_r + gc3_r, wr=[pr[7]])
            Ef2 = Ef.rearrange("p a b -> p (a b)")
            Emf2 = Emf.rearrange("p a b -> p (a b)")
            P.op("act", "activation", Ef2, pb[6], AF.Exp, rd=[pr[6]], wr=Ef_r)
            P.op("act", "activation", Emf2, pb[6], AF.Exp, scale=-1.0, rd=[pr[6]], wr=Emf_r)
            P.op("act", "activation", Erf, pb[7], AF.Exp, rd=[pr[7]], wr=Erf_r)
            P.op("dve", "scalar_tensor_tensor", out=qtl.rearrange("p a b -> p (a b)"), in0=pb[0], scalar=QS, in1=Ef2,
                 op0=ALU.mult, op1=ALU.mult, rd=[pr[0]] + Ef_r, wr=qtl_r)
            P.op("dve", "tensor_tensor", out=ktl.rearrange("p a b -> p (a b)"), in0=pb[1], in1=Emf2, op=ALU.mult,
                 rd=[pr[1]] + Emf_r, wr=ktl_r)
            P.op("dve", "tensor_tensor", out=kdec, in0=pb[2], in1=Erf, op=ALU.mult, rd=[pr[2]] + Erf_r, wr=kdec_r)
            P.op("act", "activation", v_bf[:, 0:512], pb[3], AF.Copy, rd=[pr[3]], wr=[v_bf_r[0]])
            P.op("dve", "tensor_copy", v_bf[:, 512:1024], pb[4], rd=[pr[4]], wr=[v_bf_r[1]])
            for h in range(4):
                P.op("pe", "matmul", pb[2][:, hs(h)], ktl[:, h, :], qtl[:, h, :], start=True, stop=True,
                     rd=ktl_r + qtl_r, wr=[pr[2]])
                P.op("dve", "tensor_tensor", out=attn[:, h, :], in0=pb[2][:, hs(h)], in1=maskA, op=ALU.mult,
                     rd=[pr[2]] + maskA_r, wr=[attn_r[h]])
            dsi = 0
            for h in range(4):
                vr_h = [v_bf_r[(h * 256) // 512]]
                for eh in range(2):
                    P.op("pe", "matmul", pb[3 + eh][:, hs(h)], v_bf[:, h * 256 + eh * 128:h * 256 + (eh + 1) * 128],
                         attn[:, h, :], start=True, stop=False, rd=vr_h + [attn_r[h]], wr=[pr[3 + eh]])
                for ci in range(2):
                    cs = slice(ci * 64, (ci + 1) * 64)
                    for eh in range(2):
                        P.op("pe", "matmul", pb[3 + eh][:, h * 128 + ci * 64:h * 128 + (ci + 1) * 64],
                             Sb[:, h, eh * 128:(eh + 1) * 128], qtl[:, h, cs], start=False, stop=(ci == 1),
                             skip_group_check=True, rd=[Sb_r[h]] + qtl_r, wr=[pr[3 + eh]])
                    db = 5 if dsi % 2 == 0 else 7
                    dsi += 1
                    dps = pb[db][:, 0:256]
                    P.op("pe", "matmul", dps, kdec[cs, hs(h)], v_bf[cs, h * 256:(h + 1) * 256], start=True, stop=True,
                         rd=kdec_r + vr_h, wr=[pr[db]])
                    P.op("dve", "scalar_tensor_tensor", out=Sf[:, h, :], in0=Sf[:, h, :],
                         scalar=Ef[:, h, ci * 64 + 63:ci * 64 + 64], in1=dps, op0=ALU.mult, op1=ALU.add,
                         rd=[Sf_r[h], pr[db]] + Ef_r, wr=[Sf_r[h]])
                    P.op("pool", "tensor_copy", Sb[:, h, :], Sf[:, h, :], rd=[Sf_r[h]], wr=[Sb_r[h]])
            for eh in range(2):
                P.op("act", "activation", osq[:, eh, :, :].rearrange("p a b -> p (a b)"), pb[3 + eh], AF.Square,
                     rd=[pr[3 + eh]], wr=[osq_r[eh]])
            for h in range(4):
                for eh in range(2):
                    P.op("pe", "matmul", pb[6][:, hs(h)], c["ones256"], osq[:, eh, h, :], start=(eh == 0), stop=(eh == 1),
                         rd=[osq_r[eh], c["r_ones"]], wr=[pr[6]])
            rso2 = rso.rearrange("p a b -> p (a b)")
            P.op("dve", "tensor_scalar_add", rso2, pb[6], EPS, rd=[pr[6]], wr=rso_r)
            P.op("act", "activation", rso2, rso2, AF.Ln, rd=rso_r, wr=rso_r)
            P.op("act", "activation", rso2, rso2, AF.Exp, scale=-0.5, rd=rso_r, wr=rso_r)
            for cidx in range(8):
                bk = cidx // 4
                fm_proj(pb[bk][:, hs(cidx % 4)], pr[bk], 2064 + cidx * 128, 128, xb, xb_r, tsl)
            for bk in range(2):
                P.op("act", "activation", sil[:, bk * 4:(bk + 1) * 4, :].rearrange("p a b -> p (a b)"), pb[bk], AF.Silu,
                     rd=[pr[bk]], wr=[sil_r[bk]])
            for h in range(4):
                for eh in range(2):
                    cidx = h * 2 + eh
                    tmp, tmp_r = tmp_l[cidx % 2]
                    P.op("dve", "scalar_tensor_tensor", out=tmp, in0=pb[3 + eh][:, hs(h)],
                         scalar=k.vec(p + "gla_out_norm", eh), in1=rso[:, h, :], op0=ALU.mult, op1=ALU.mult,
                         rd=[pr[3 + eh], vr] + rso_r, wr=tmp_r)
                    P.op("pool", "tensor_tensor", out=gT[:, cidx, tsl], in0=tmp, in1=sil[:, cidx, :], op=ALU.mult,
                         rd=tmp_r + [sil_r[cidx // 4]], wr=[gT_r[cidx]])

        def ychunk(oc, ps, ps_r):
            for cc in range(DC):
                P.op("pe", "matmul", ps, w_o[:, cc, oc * 128:(oc + 1) * 128], gT[:, cc, :],
                     start=(cc == 0), stop=(cc == DC - 1), rd=[w_o_r[cc], gT_r[cc]], wr=[ps_r])
        ps_y = [(pb[0][:, 0:TT], pr[0]), (pb[1][:, 0:TT], pr[1])]
        ps_stat = [(pb[2][:, 0:TT], pr[2]), (pb[5][:, 0:TT], pr[5])]
        ln_tail(ctx, t, ti, ychunk, xs, xs_r[0], 0, p + "ln1_g", p + "ln1_b", dst, ps_y, ps_stat)


def phase_conv(ctx, l, src, dst):
    k, P, M, T, TT, NT = ctx["k"], ctx["P"], ctx["M"], ctx["T"], ctx["TT"], ctx["NT"]
    W, c, pb, pr = ctx["W"], ctx["consts"], ctx["pbanks"], ctx["pres"]
    p = f"l{l}_"
    H = 30
    w_in_d, w_o_d = W[p + "conv_w_in"], W[p + "conv_w_o"]
    w_in, w_in_r = M.alloc("cv_w_in", [DC, 2 * D], BF16, nres=DC)
    w_o, w_o_r = M.alloc("cv_w_o", [DC, D], BF16, nres=DC)
    for kc in range(DC):
        load_w(ctx, w_in[:, kc, :], w_in_d[kc * 128:(kc + 1) * 128, :], [w_in_r[kc]])
    for kc in range(DC):
        load_w(ctx, w_o[:, kc, :], w_o_d[kc * 128:(kc + 1) * 128, :], [w_o_r[kc]])
    NB = 2
    xs_l = [M.alloc(f"cxs{i}", [DC, TT], F32) for i in range(NB)]
    xb_l = [M.alloc(f"cxb{i}", [DC, TT + H], BF16) for i in range(NB)]
    sg_l = [M.alloc(f"csg{i}", [TT + H], F32) for i in range(2)]
    u_l = [M.alloc(f"cu{i}", [TT + H], F32) for i in range(2)]
    aD_l = [M.alloc(f"caD{i}", [TT], F32) for i in range(2)]
    aP_l = [M.alloc(f"caP{i}", [TT], F32) for i in range(2)]
    tm_l = [M.alloc(f"ctm{i}", [TT], F32) for i in range(2)]
    gs, gs_r = M.alloc("cgs", [DC, TT], BF16, nres=DC)
    t2 = ln_tail_alloc(ctx)
    t = ln_tail_alloc(ctx)
    srcv = src.rearrange("(c p) t -> p c t", p=128)

    def load_x(ti, slot):
        xs, xs_r = xs_l[slot]
        xb, xb_r = xb_l[slot]
        t0 = ti * TT
        rdr = [k.dr(("x", id(src), ti))]
        P.dma("sp", xs[:, :, :], srcv[:, :, t0:t0 + TT], rd=rdr, wr=xs_r)
        if ti == 0:
            P.op("pool", "memset", xb[:, :, 0:H], 0.0, wr=xb_r)
            P.dma("pool", xb[:, :, H:TT + H], srcv[:, :, 0:TT], rd=rdr, wr=xb_r)
        else:
            rdr.append(k.dr(("x", id(src), ti - 1)))
            P.dma("pool", xb[:, :, :], srcv[:, :, t0 - H:t0 + TT], rd=rdr, wr=xb_r)

    load_x(0, 0)
    for ti in range(NT):
        slot = ti % NB
        if ti + 1 < NT:
            load_x(ti + 1, (ti + 1) % NB)
        xs, xs_r = xs_l[slot]
        xb, xb_r = xb_l[slot]
        for cc in range(DC):
            ba, bg = (cc % 2) * 2, (cc % 2) * 2 + 1
            psa, psg = pb[ba][:, 0:TT + H], pb[bg][:, 0:TT + H]
            for kc in range(DC):
                P.op("pe", "matmul", psa, w_in[:, kc, cc * 128:(cc + 1) * 128], xb[:, kc, :],
                     start=(kc == 0), stop=(kc == DC - 1), rd=[w_in_r[kc]] + xb_r, wr=[pr[ba]])
            for kc in range(DC):
                P.op("pe", "matmul", psg, w_in[:, kc, D + cc * 128:D + (cc + 1) * 128], xb[:, kc, :],
                     start=(kc == 0), stop=(kc == DC - 1), rd=[w_in_r[kc]] + xb_r, wr=[pr[bg]])
            sg, sg_r = sg_l[cc % 2]
            u, u_r = u_l[cc % 2]
            aD, aD_r = aD_l[cc % 2]
            aP, aP_r = aP_l[cc % 2]
            P.op("act", "activation", sg, psg, AF.Sigmoid, bias=k.vec(p + "conv_b_in", DC + cc), scale=1.0,
                 rd=[pr[bg], c["vr"]], wr=sg_r)
            P.op("dve", "scalar_tensor_tensor", out=u, in0=psa, scalar=k.vec(p + "conv_b_in", cc), in1=sg,
                 op0=ALU.add, op1=ALU.mult, rd=[pr[ba], c["vr"]] + sg_r, wr=u_r)
            if ti == 0:
                P.op("pool", "memset", u[:, 0:H], 0.0, rd=u_r, wr=u_r)
            P.op("act", "activation", aD, u[:, 0:TT], AF.Identity, bias=k.vec(p + "conv_dw_b", cc),
                 scale=k.vec(p + "conv_dw0", cc), rd=u_r + [c["vr"]], wr=aD_r)
            ND = 24
            for j in range(1, ND + 1):
                P.op("dve", "scalar_tensor_tensor", out=aD, in0=u[:, j:j + TT], scalar=k.vec(p + f"conv_dw{j}", cc),
                     in1=aD, op0=ALU.mult, op1=ALU.add, rd=u_r + aD_r + [c["vr"]], wr=aD_r)
            P.op("act", "activation", aP, u[:, ND + 1:ND + 1 + TT], AF.Identity, scale=k.vec(p + f"conv_dw{ND + 1}", cc),
                 rd=u_r + [c["vr"]], wr=aP_r)
            for j in range(ND + 2, 31):
                tm, tm_r = tm_l[j % 2]
                P.op("act", "activation", tm, u[:, j:j + TT], AF.Identity, scale=k.vec(p + f"conv_dw{j}", cc),
                     rd=u_r + [c["vr"]], wr=tm_r)
                P.op("pool", "tensor_tensor", out=aP, in0=aP, in1=tm, op=ALU.add, rd=aP_r + tm_r, wr=aP_r)
            ho = t2["r"][:, cc, :]
            P.op("dve", "tensor_tensor", out=ho, in0=aD, in1=aP, op=ALU.add, rd=aD_r + aP_r, wr=[t2["r_r"][cc]])
            P.op("pool", "tensor_copy", t2["rb"][:, cc, :], ho, rd=[t2["r_r"][cc]], wr=[t2["rb_r"][cc]])
            P.op("act", "activation", t2["rsq"][:, cc, :], ho, AF.Square, rd=[t2["r_r"][cc]], wr=[t2["rsq_r"][cc]])
        t2["ps_stat"] = [(pb[4][:, 0:TT], pr[4]), (pb[5][:, 0:TT], pr[5])]

        def fin(oc, ro, rres):
            P.op("act", "activation", gs[:, oc, :], ro, AF.Silu, rd=[rres], wr=[gs_r[oc]])
        ln_core(ctx, t2, p + "conv_ln_g", p + "conv_ln_b", fin)

        def ychunk(oc, ps, ps_r):
            for cc in range(DC):
                P.op("pe", "matmul", ps, w_o[:, cc, oc * 128:(oc + 1) * 128], gs[:, cc, :],
                     start=(cc == 0), stop=(cc == DC - 1), rd=[w_o_r[cc], gs_r[cc]], wr=[ps_r])
        ps_y = [(pb[6][:, 0:TT], pr[6]), (pb[7][:, 0:TT], pr[7])]
        ps_stat = [(pb[4][:, 0:TT], pr[4]), (pb[5][:, 0:TT], pr[5])]
        ln_tail(ctx, t, ti, ychunk, xs, xs_r[0], 0, p + "ln1_g", p + "ln1_b", dst, ps_y, ps_stat,
                bias_name=p + "conv_b_o")


WNAMES = {
    0: ["mla_w_in", "mla_w_uq", "mla_w_ukv", "mla_w_o"],
    1: ["gla_w_in", "gla_w_a2", "gla_b_a", "gla_w_o"],
    2: ["conv_w_in", "conv_w_o"],
}


def make_consts():
    cst = np.zeros((128, 160), np.float32)
    maskb = np.zeros((128, 2048), np.float32)
    cst[:, 0:128] = np.eye(128, dtype=np.float32)
    half = 32
    inv = (10000.0 ** (-np.arange(half, dtype=np.float32) / half)).astype(np.float32)
    cst[0:32, 128] = inv
    cst[32:64, 128] = inv
    cst[64:96, 128] = inv
    cst[96:128, 128] = inv
    qq = np.arange(512)[None, :]
    for d in range(4):
        kk = (d * 128 + np.arange(128))[:, None]
        maskb[:, d * 512:(d + 1) * 512] = np.where(kk <= qq, 0.0, NEG)
    return cst, maskb


def make_glac():
    g = np.zeros((128, 384), np.float32)
    j = np.arange(128)[:, None]
    i = np.arange(128)[None, :]
    same = (j // 64) == (i // 64)
    g[:, 0:128] = np.where(same & (j <= i), -1.0 / 16.0, 0.0)
    g[:, 128:256] = np.where(same & (j > i), -1.0 / 16.0, 0.0)
    g[:, 256:384] = np.where(same & (j <= i), 1.0, 0.0)
    return g


def kernel(**inputs):
    inp = {kk: np.asarray(v) for kk, v in inputs.items()}
    x = inp["x"]
    B, S, _ = x.shape
    vp = build_vecpack(inp)
    vecs = vp.pack()
    layers = [(l, True, True) for l in range(DEPTH)]
    shapes = {}
    wmaps = {}
    for l in range(DEPTH):
        p = f"l{l}_"
        names = WNAMES[layer_kind(l)] + ["ffn_w_in", "ffn_w_out"]
        for nm in names:
            a = np.ascontiguousarray(inp[p + nm], dtype=np.float32)
            if a.ndim == 1:
                a = a.reshape(1, -1)
            shapes[p + nm] = a.shape
            wmaps[p + nm] = a
    nc, _ = build_program(S, shapes, vp.off, vp.n, layers)
    cst, maskb = make_consts()
    in_maps = []
    for b in range(B):
        m = dict(wmaps)
        m["xT"] = np.ascontiguousarray(x[b].T)
        m["vecs"] = vecs
        m["pos"] = np.ascontiguousarray(inp["positions"][b].reshape(1, S).astype(np.int32))
        m["cst"] = cst
        m["maskb"] = maskb
        m["glac"] = make_glac()
        in_maps.append(m)
    res = run_bass_kernel_spmd(nc, in_maps, core_ids=list(range(B)))
    out = np.stack([np.ascontiguousarray(res.results[b]["outT"].T) for b in range(B)], axis=0)
    return out.astype(np.float32)
```
